# Optimizing a Trainium2 kernel written in Bass

```python
import jax, jax.numpy as jnp
from jax import lax
import numpy as np

D_MODEL = 2048
BATCH = 8
SEQ = 2048
DEPTH = 1

CHUNK = 64
RWKV_WIDTH = 1024
RWKV_HEAD = 64
RWKV_HEADS = RWKV_WIDTH // RWKV_HEAD
DECAY_LORA = 64
AAA_LORA = 64
GATE_LORA = 160
RWKV_GN_EPS = 64e-5
HGRN_WIDTH = 1024
HGRN_EXPAND = 128
HGRN_HEADS = HGRN_WIDTH // HGRN_EXPAND
HGRN_DK = HGRN_EXPAND
HGRN_DV = HGRN_WIDTH // HGRN_HEADS
GLA_BLOCK = CHUNK // 4
N_EXPERTS = 32
TOP_K = 4
D_EXPERT = 2048
SWIGLU_ALPHA = 1.702
SWIGLU_LIMIT = 7.0
MOE_BLOCK = 128
NORM_EPS = 1e-5
RWKV_COLS = 3 * RWKV_WIDTH + DECAY_LORA + AAA_LORA + GATE_LORA
HGRN_COLS = 4 * HGRN_WIDTH
GATE_COLS = 2 * D_MODEL
IN_COLS = RWKV_COLS + HGRN_COLS + GATE_COLS

kernel_name = 'hybrid_rwkv7_hgrn2_moe_block'


def _rmsnorm(x, g, eps=NORM_EPS):
    xf = x.astype(jnp.float32)
    y = xf * lax.rsqrt(jnp.mean(xf * xf, axis=-1, keepdims=True) + eps)
    return (y * g.astype(jnp.float32)).astype(x.dtype)


def _token_shift(p, mu):
    prev = jnp.pad(p, ((0, 0), (1, 0), (0, 0)))[:, :-1, :]
    return p + (prev - p) * mu


def _rwkv7_mix(p, w0, w2, a0, a2, g2, k_k, k_a, r_k, gn_g, gn_b):
    B, S, _ = p.shape
    C, H, N = RWKV_WIDTH, RWKV_HEADS, RWKV_HEAD
    f32 = jnp.float32
    p = p.astype(f32)
    r, k, v, wd, ad, gd = jnp.split(p, [C, 2 * C, 3 * C, 3 * C + DECAY_LORA, 3 * C + DECAY_LORA + AAA_LORA], axis=-1)
    w_log = -jax.nn.softplus(-(w0 + jnp.tanh(wd) @ w2)) - 0.5
    decay = jnp.exp(-jnp.exp(w_log))
    a = jax.nn.sigmoid(a0 + ad @ a2)
    g = jax.nn.sigmoid(gd) @ g2
    heads = lambda t: t.reshape(B, S, H, N)
    kk = heads(k * k_k)
    kk = kk / jnp.maximum(jnp.sqrt(jnp.sum(kk * kk, axis=-1, keepdims=True)), 1e-12)
    k = k * (1.0 + (a - 1.0) * k_a)
    r, k, v, a, decay = heads(r), heads(k), heads(v), heads(a), heads(decay)
    tm = lambda t: jnp.moveaxis(t, 1, 0)

    def step(state, inp):
        r_t, w_t, k_t, v_t, kk_t, a_t = inp
        sa = jnp.einsum('bhvk,bhk->bhv', state, -kk_t)
        state = (state * w_t[:, :, None, :]
                 + sa[..., None] * (kk_t * a_t)[:, :, None, :]
                 + v_t[..., None] * k_t[:, :, None, :])
        return state, jnp.einsum('bhvk,bhk->bhv', state, r_t)

    _, y = lax.scan(step, jnp.zeros((B, H, N, N), f32),
                    (tm(r), tm(decay), tm(k), tm(v), tm(kk), tm(a)))
    y = jnp.moveaxis(y, 0, 1)
    mu = jnp.mean(y, axis=-1, keepdims=True)
    var = jnp.mean(jnp.square(y - mu), axis=-1, keepdims=True)
    y = ((y - mu) * lax.rsqrt(var + RWKV_GN_EPS)).reshape(B, S, C) * gn_g + gn_b
    bonus = jnp.sum(r * k * r_k, axis=-1, keepdims=True) * v
    return (y + bonus.reshape(B, S, C)) * g


def _hgrn2_mix(p, lb, gn_g):
    B, S, _ = p.shape
    H, DK, DV, L = HGRN_HEADS, HGRN_DK, HGRN_DV, GLA_BLOCK
    nC = S // L
    f32 = jnp.float32
    q, f, i, og = jnp.split(p.astype(f32), 4, axis=-1)
    q = jax.nn.silu(q)
    forget = lb + (1.0 - lb) * jax.nn.sigmoid(f)
    log_f = jnp.log(forget)
    k = 1.0 - forget
    blocks = lambda t, d: t.reshape(B, nC, L, H, d).transpose(0, 3, 1, 2, 4)
    q, k, log_f, v = blocks(q, DK), blocks(k, DK), blocks(log_f, DK), blocks(i, DV)
    b = jnp.cumsum(log_f, axis=3)
    q_dec = q * jnp.exp(b)
    k_dec = k * jnp.exp(-b)
    causal = jnp.tril(jnp.ones((L, L), dtype=bool))
    attn = jnp.where(causal, jnp.einsum('bhcld,bhcmd->bhclm', q_dec, k_dec), 0.0)
    o_intra = jnp.einsum('bhclm,bhcme->bhcle', attn, v)
    b_last = b[:, :, :, -1:, :]
    k_end = k * jnp.exp(b_last - b)
    decay_end = jnp.exp(b_last[:, :, :, 0, :])

    def step(state, inp):
        q_c, k_c, v_c, d_c = inp
        o = jnp.einsum('bhld,bhde->bhle', q_c, state)
        state = state * d_c[..., None] + jnp.einsum('bhld,bhle->bhde', k_c, v_c)
        return state, o

    cm = lambda t: jnp.moveaxis(t, 2, 0)
    _, o_inter = lax.scan(step, jnp.zeros((B, H, DK, DV), f32),
                          (cm(q_dec), cm(k_end), cm(v), cm(decay_end)))
    o = o_intra + jnp.moveaxis(o_inter, 0, 2)
    o = o.transpose(0, 2, 3, 1, 4).reshape(B, S, H, DV)
    o = o * lax.rsqrt(jnp.mean(o * o, axis=-1, keepdims=True) + NORM_EPS) * gn_g
    o = o * jax.nn.silu(og.reshape(B, S, H, DV))
    return o.reshape(B, S, HGRN_WIDTH)


def _token_mixer(h, w_in, mu, w0, w2, a0, a2, g2, k_k, k_a, r_k, rgn_g, rgn_b, lb, hgn_g, proj_a, proj_b, w_out):
    proj = h @ w_in
    p_rwkv, p_hgrn, p_gate = jnp.split(proj, [RWKV_COLS, RWKV_COLS + HGRN_COLS], axis=-1)
    y_a = _rwkv7_mix(_token_shift(p_rwkv, mu), w0, w2, a0, a2, g2, k_k, k_a, r_k, rgn_g, rgn_b)
    y_b = _hgrn2_mix(p_hgrn, lb, hgn_g)
    gate_a, gate_b = jnp.split(p_gate.astype(jnp.float32), 2, axis=-1)
    merged = (jax.nn.sigmoid(gate_a) * (y_a @ proj_a.astype(jnp.float32))
              + jax.nn.sigmoid(gate_b) * (y_b @ proj_b.astype(jnp.float32)))
    return (merged.astype(h.dtype) @ w_out)


def _moe(h, router_w, router_b, w_gate_up, b_gate_up, w_down, b_down):
    B, S, D = h.shape
    T = B * S
    A = T * TOP_K
    hf = h.reshape(T, D)
    logits = hf.astype(jnp.float32) @ router_w.astype(jnp.float32) + router_b.astype(jnp.float32)
    top_vals, top_idx = lax.top_k(logits, TOP_K)
    gates = jax.nn.softmax(top_vals, axis=-1)
    flat_e = top_idx.reshape(A)
    flat_tok = jnp.repeat(jnp.arange(T, dtype=jnp.int32), TOP_K)
    flat_w = gates.reshape(A)
    order = jnp.argsort(flat_e)
    se, stok, sw = flat_e[order], flat_tok[order], flat_w[order]
    counts = jnp.bincount(flat_e, length=N_EXPERTS)
    padded = ((counts + MOE_BLOCK - 1) // MOE_BLOCK) * MOE_BLOCK
    pad_end = jnp.cumsum(padded)
    pad_start = pad_end - padded
    start = jnp.cumsum(counts) - counts
    dest = pad_start[se] + jnp.arange(A) - start[se]
    NB = -(-A // MOE_BLOCK) + N_EXPERTS
    tok_buf = jnp.zeros((NB * MOE_BLOCK,), jnp.int32).at[dest].set(stok)
    w_buf = jnp.zeros((NB * MOE_BLOCK,), h.dtype).at[dest].set(sw.astype(h.dtype))
    block_expert = jnp.clip(jnp.searchsorted(pad_end, jnp.arange(NB) * MOE_BLOCK, side='right'), 0, N_EXPERTS - 1)

    def expert_block(args):
        tok, wt, e = args
        xb = hf[tok]
        gu = xb @ w_gate_up[e] + b_gate_up[e]
        x_glu, x_lin = jnp.split(gu, 2, axis=-1)
        x_glu = jnp.minimum(x_glu, SWIGLU_LIMIT)
        x_lin = jnp.clip(x_lin, -SWIGLU_LIMIT, SWIGLU_LIMIT)
        act = x_glu * jax.nn.sigmoid(SWIGLU_ALPHA * x_glu) * (x_lin + 1.0)
        return (act @ w_down[e] + b_down[e]) * wt[:, None]

    ys = lax.map(expert_block, (tok_buf.reshape(NB, MOE_BLOCK), w_buf.reshape(NB, MOE_BLOCK), block_expert))
    out = jnp.zeros((T, D), ys.dtype).at[tok_buf].add(ys.reshape(NB * MOE_BLOCK, D))
    return out.reshape(B, S, D).astype(h.dtype)


def setup_inputs(seed: int = 0) -> dict:
    key = jax.random.key(seed)
    ks = jax.random.split(key, 32)
    f32 = jnp.float32
    nrm = lambda k, shape, s: jax.random.normal(k, shape, f32) * s
    Dp = DEPTH
    return {
        'x': nrm(ks[0], (BATCH, SEQ, D_MODEL), 1.0),
        'c': nrm(ks[1], (BATCH, D_MODEL), 1.0),
        'ada_w': nrm(ks[2], (Dp, D_MODEL, 6 * D_MODEL), D_MODEL ** -0.5),
        'ada_b': nrm(ks[3], (Dp, 6 * D_MODEL), 0.01),
        'norm1_g': 1.0 + nrm(ks[4], (Dp, D_MODEL), 0.01),
        'norm2_g': 1.0 + nrm(ks[5], (Dp, D_MODEL), 0.01),
        'w_in': nrm(ks[6], (Dp, D_MODEL, IN_COLS), D_MODEL ** -0.5),
        'rwkv_mu': jax.random.uniform(ks[7], (Dp, RWKV_COLS), f32),
        'rwkv_w0': nrm(ks[8], (Dp, RWKV_WIDTH), 0.5),
        'rwkv_w2': nrm(ks[9], (Dp, DECAY_LORA, RWKV_WIDTH), DECAY_LORA ** -0.5),
        'rwkv_a0': nrm(ks[10], (Dp, RWKV_WIDTH), 0.1),
        'rwkv_a2': nrm(ks[11], (Dp, AAA_LORA, RWKV_WIDTH), AAA_LORA ** -0.5),
        'rwkv_g2': nrm(ks[12], (Dp, GATE_LORA, RWKV_WIDTH), GATE_LORA ** -0.5),
        'rwkv_k_k': 1.0 + nrm(ks[13], (Dp, RWKV_WIDTH), 0.1),
        'rwkv_k_a': 1.0 + nrm(ks[14], (Dp, RWKV_WIDTH), 0.1),
        'rwkv_r_k': nrm(ks[15], (Dp, RWKV_HEADS, RWKV_HEAD), 0.1),
        'rwkv_gn_g': 1.0 + nrm(ks[16], (Dp, RWKV_WIDTH), 0.01),
        'rwkv_gn_b': nrm(ks[17], (Dp, RWKV_WIDTH), 0.01),
        'hgrn_lb_logits': nrm(ks[18], (DEPTH + 1, HGRN_WIDTH), 0.1),
        'hgrn_gn_g': 1.0 + nrm(ks[19], (Dp, HGRN_DV), 0.01),
        'proj_a': nrm(ks[20], (Dp, RWKV_WIDTH, D_MODEL), RWKV_WIDTH ** -0.5),
        'proj_b': nrm(ks[21], (Dp, HGRN_WIDTH, D_MODEL), HGRN_WIDTH ** -0.5),
        'w_out': nrm(ks[22], (Dp, D_MODEL, D_MODEL), D_MODEL ** -0.5),
        'router_w': nrm(ks[23], (Dp, D_MODEL, N_EXPERTS), D_MODEL ** -0.5),
        'router_b': nrm(ks[24], (Dp, N_EXPERTS), 0.01),
        'exp_w_gate_up': nrm(ks[25], (Dp, N_EXPERTS, D_MODEL, 2 * D_EXPERT), D_MODEL ** -0.5),
        'exp_b_gate_up': nrm(ks[26], (Dp, N_EXPERTS, 2 * D_EXPERT), 0.01),
        'exp_w_down': nrm(ks[27], (Dp, N_EXPERTS, D_EXPERT, D_MODEL), D_EXPERT ** -0.5),
        'exp_b_down': nrm(ks[28], (Dp, N_EXPERTS, D_MODEL), 0.01),
        'final_norm_g': 1.0 + nrm(ks[29], (D_MODEL,), 0.01),
    }


def reference(x, c, ada_w, ada_b, norm1_g, norm2_g, w_in, rwkv_mu, rwkv_w0, rwkv_w2, rwkv_a0, rwkv_a2,
              rwkv_g2, rwkv_k_k, rwkv_k_a, rwkv_r_k, rwkv_gn_g, rwkv_gn_b, hgrn_lb_logits, hgrn_gn_g,
              proj_a, proj_b, w_out, router_w, router_b, exp_w_gate_up, exp_b_gate_up, exp_w_down,
              exp_b_down, final_norm_g):
    lower_bounds = jnp.cumsum(jax.nn.softmax(hgrn_lb_logits.astype(jnp.float32), axis=0), axis=0)
    c_act = jax.nn.silu(c)
    for l in range(DEPTH):
        mod = c_act @ ada_w[l] + ada_b[l]
        sh1, sc1, g1, sh2, sc2, g2 = [m[:, None, :] for m in jnp.split(mod, 6, axis=-1)]
        h = _rmsnorm(x, norm1_g[l]) * (1.0 + sc1) + sh1
        mix = _token_mixer(h, w_in[l], rwkv_mu[l], rwkv_w0[l], rwkv_w2[l], rwkv_a0[l], rwkv_a2[l], rwkv_g2[l],
                           rwkv_k_k[l], rwkv_k_a[l], rwkv_r_k[l], rwkv_gn_g[l], rwkv_gn_b[l],
                           lower_bounds[l], hgrn_gn_g[l], proj_a[l], proj_b[l], w_out[l])
        x = x + g1 * mix
        h = _rmsnorm(x, norm2_g[l]) * (1.0 + sc2) + sh2
        ffn = _moe(h, router_w[l], router_b[l], exp_w_gate_up[l], exp_b_gate_up[l], exp_w_down[l], exp_b_down[l])
        x = x + g2 * ffn
    return _rmsnorm(x, final_norm_g)
```

```python
from contextlib import ExitStack
import numpy as np
import concourse.bass as bass
import concourse.mybir as mybir
from concourse.bass_utils import run_bass_kernel_spmd

F32 = mybir.dt.float32
BF16 = mybir.dt.bfloat16
I32 = mybir.dt.int32
U32 = mybir.dt.uint32
AF = mybir.ActivationFunctionType
ALU = mybir.AluOpType
AX = mybir.AxisListType

D = 2048
T = 2048
NT = 16
NB = 4
NE = 32
CAP = 2048
SUB = 512
NA = SUB // 128
RW = 1024
RWKV_COLS = 3360
HG0 = 3360
GT0 = 3360 + 4096
NDS = 32


class Prog:
    CE = ('pe', 'act', 'dve', 'pool')

    def __init__(self, nc, stack):
        self.nc = nc
        self.eng = {'pe': nc.tensor, 'act': nc.scalar, 'dve': nc.vector, 'pool': nc.gpsimd, 'sp': nc.sync}
        self.esem = {e: stack.enter_context(nc.semaphore('se_' + e)) for e in self.CE}
        self.dsem = [stack.enter_context(nc.semaphore('sd%d' % i)) for i in range(NDS)]
        self.duse = [0] * NDS
        self.dnext = 0
        self.seq = {e: 0 for e in self.CE}
        self.seen = {e: {} for e in self.eng}
        self.res = {}
        self.ops = {e: [] for e in self.eng}
        self.nops = 0

    def _need(self, e, ev, waits):
        if ev is None:
            return
        sem, val, src = ev
        if src == e and e == 'pe':
            return
        k = id(sem)
        if self.seen[e].get(k, 0) >= val:
            return
        self.seen[e][k] = val
        waits.append((sem, val))

    def _deps(self, e, reads, writes):
        waits = []
        for r in reads:
            st = self.res.get(r)
            if st:
                self._need(e, st['w'], waits)
        for w in writes:
            st = self.res.get(w)
            if st:
                self._need(e, st['w'], waits)
                for ev in st['r'].values():
                    self._need(e, ev, waits)
        return waits

    def _commit(self, e, ev, reads, writes):
        for r in reads:
            st = self.res.setdefault(r, {'w': None, 'r': {}})
            st['r'][(e, id(ev[0]))] = ev
        for w in writes:
            self.res[w] = {'w': ev, 'r': {}}

    def op(self, e, fn, reads=(), writes=()):
        if self.nops >= OPLIMIT[0]:
            return
        waits = self._deps(e, reads, writes)
        self.seq[e] += 1
        ev = (self.esem[e], self.seq[e], e)
        self.ops[e].append((waits, fn, (self.esem[e], 1)))
        self._commit(e, ev, reads, writes)
        self.nops += 1

    def dma(self, q, fn, reads=(), writes=()):
        if self.nops >= OPLIMIT[0]:
            return
        waits = self._deps(q, reads, writes)
        i = self.dnext
        self.dnext = (self.dnext + 1) % NDS
        sem = self.dsem[i]
        if self.duse[i] > 0:
            self._need(q, (sem, 16 * self.duse[i], 'dma'), waits)
        self.duse[i] += 1
        ev = (sem, 16 * self.duse[i], 'dma')
        self.ops[q].append((waits, fn, (sem, 16)))
        self._commit(q, ev, reads, writes)
        self.nops += 1

    def barrier(self):
        for e in self.eng:
            waits = []
            for c in self.CE:
                if self.seq[c] > 0:
                    self._need(e, (self.esem[c], self.seq[c], 'x'), waits)
            for i in range(NDS):
                if self.duse[i] > 0:
                    self._need(e, (self.dsem[i], 16 * self.duse[i], 'dma'), waits)
            if waits:
                self.ops[e].append((waits, None, None))
        self.res = {}

    def flush(self):
        nc = self.nc
        ops = self.ops
        self.ops = {e: [] for e in self.eng}
        with nc.Block() as block:
            def emit(name):
                def body(engine):
                    for waits, fn, inc in ops[name]:
                        for sem, val in waits:
                            engine.wait_ge(sem, val)
                        if fn is not None:
                            fn(engine).then_inc(inc[0], inc[1])
                return body
            block.tensor(emit('pe'))
            block.scalar(emit('act'))
            block.vector(emit('dve'))
            block.gpsimd(emit('pool'))
            block.sync(emit('sp'))


def build(debug=None, stages=99):
    nc = bass.Bass("TRN2", target_bir_lowering=False)
    dbg = {}
    with ExitStack() as top:
        P = Prog(nc, top)

        def din(name, shape, dt=F32):
            return nc.dram_tensor(name, list(shape), dt, kind="ExternalInput").ap()

        def dout(name, shape, dt=F32):
            return nc.dram_tensor(name, list(shape), dt, kind="ExternalOutput").ap()

        def dscratch(name, shape, dt=F32):
            return nc.dram_tensor(name, list(shape), dt, kind="Internal").ap()

        sbn = [0]

        def sb(st, name, shape, dt=F32):
            sbn[0] += 1
            return st.enter_context(nc.sbuf_tensor('sb%d_%s' % (sbn[0], name), list(shape), dt))

        xT = din('xT', [D, T])
        x_in = din('x', [T, D])
        cT = din('cT', [128, 16])
        ada_w = din('ada_w', [24, 128, 16, 512])
        adab_col = din('adab_col', [128, 32])
        adab_row = din('adab_row', [1, 8192])
        n1g_col = din('n1g_col', [128, 16])
        w_in_l = din('w_in_l', [NCH, 128, 16, 128])
        cols_d = din('cols', [128, NCOL])
        cst_d = din('cst', [128, NCST])
        w2p_d = din('w2p', [128, 1024])
        a2p_d = din('a2p', [128, 1024])
        g2a_d = din('g2a', [128, 1024])
        g2b_d = din('g2b', [128, 1024])
        pa_l = din('pa_l', [16, 128, 8, 128])
        pb_l = din('pb_l', [16, 128, 8, 128])
        wout_l = din('wout_l', [16, 128, 16, 128])
        rows_d = din('rows', [1, 2 * D + 32])
        rw_l = din('rw_l', [128, 16, 32])
        if stages >= 5:
            wgu_l = din('wgu_l', [NE, 8, 128, 16, 512])
            wdn_l = din('wdn_l', [NE, 4, 128, 16, 512])
        bgu_col = din('bgu_col', [128, NE * 32])
        bdn_row = din('bdn_row', [1, NE * D])
        out = dout('out', [T, D])
        modrow = dscratch('modrow', [1, 8192])
        x1s = dscratch('x1s', [T, D])
        Xg = dscratch('Xg', [NE * CAP, D], BF16)
        Ys = dscratch('Ys', [NE * CAP, D], BF16)

        if debug:
            for nm, (shp, dt_) in debug.items():
                dbg[nm] = dout('dbg_' + nm, shp, dt_)

        def tap(name, ap, key):
            if debug and name in debug:
                P.dma('sp', lambda e: e.dma_start(out=dbg[name], in_=ap), reads=[key])

        ps = [top.enter_context(nc.psum_tensor('ps%d' % i, [128, 512], F32)) for i in range(8)]
        psn = [0]

        def nps():
            i = psn[0]
            psn[0] = (i + 1) % 8
            return i

        ones_bf = sb(top, 'ones_bf', [128, 128], BF16)
        P.op('pool', lambda e: e.memset(ones_bf[:], 1.0), writes=['ones_bf'])
        A1 = sb(top, 'A1', [128, 16])
        SH1 = sb(top, 'SH1', [128, 16])
        cst = sb(top, 'cst', [128, NCST])
        cols = sb(top, 'colsb', [128, NCOL])
        P.dma('sp', lambda e: e.dma_start(out=cst[:], in_=cst_d), writes=['cst'])
        P.dma('sp', lambda e: e.dma_start(out=cols[:], in_=cols_d), writes=['cols'])
        mask4 = cst[:, 0:512]
        ident = cst[:, 512:640]
        strictL = cst[:, 640:768]
        inclU = cst[:, 768:896]
        bd64 = cst[:, 896:1024]
        zeros = cst[:, 1024:1152]
        ecap1 = cst[:, 1152:1184]
        strictU = cst[:, 1184:1312]
        idx4_all = sb(top, 'idx4_all', [128, NT, 4], I32)
        g4_all = sb(top, 'g4_all', [128, NT, 4])

        def rsqrt(apf, key):
            P.op('act', lambda e: e.activation(out=apf(), in_=apf(), func=AF.Sqrt), reads=[key], writes=[key])
            P.op('dve', lambda e: e.reciprocal(out=apf(), in_=apf()), reads=[key], writes=[key])

        def col(name, j=0, n=1):
            o = COLOFF[name] + j
            return cols[:, o:o + n]

        with ExitStack() as st:
            ct = sb(st, 'ct', [128, 16])
            cact = sb(st, 'cact', [128, 16], BF16)
            abc = sb(st, 'abc', [128, 32])
            n1g = sb(st, 'n1g', [128, 16])
            abr = sb(st, 'abr', [1, 8192])
            mrow = sb(st, 'mrow', [1, 8192])
            mcol = sb(st, 'mcol', [128, 32])
            wst = [sb(st, 'wst%d' % i, [128, 16, 512]) for i in range(2)]
            wbf = [sb(st, 'wbf%d' % i, [128, 16, 512], BF16) for i in range(2)]
            P.dma('sp', lambda e: e.dma_start(out=ct[:], in_=cT), writes=['ct'])
            P.dma('sp', lambda e: e.dma_start(out=abc[:], in_=adab_col), writes=['abc'])
            P.dma('sp', lambda e: e.dma_start(out=n1g[:], in_=n1g_col), writes=['n1g'])
            P.dma('sp', lambda e: e.dma_start(out=abr[:], in_=adab_row), writes=['abr'])
            P.op('act', lambda e: e.activation(out=cact[:], in_=ct[:], func=AF.Silu), reads=['ct'], writes=['cact'])
            for s in range(24):
                b = s % 2
                P.dma('sp', lambda e, s=s, b=b: e.dma_start(out=wst[b][:], in_=ada_w[s]), writes=[('wst', b)])
                P.op('dve', lambda e, b=b: e.tensor_copy(out=wbf[b][:, 0:8, :], in_=wst[b][:, 0:8, :]),
                     reads=[('wst', b)], writes=[('wbf', b, 0)])
                P.op('act', lambda e, b=b: e.copy(out=wbf[b][:, 8:16, :], in_=wst[b][:, 8:16, :]),
                     reads=[('wst', b)], writes=[('wbf', b, 1)])
                rd = [('wbf', b, 0), ('wbf', b, 1), 'cact']
                if s < 8:
                    for j in range(4):
                        cc = 4 * s + j

                        def mmf(e, b=b, j=j, cc=cc):
                            for k in range(16):
                                r = e.matmul(ps[0][:, cc:cc + 1], lhsT=wbf[b][:, k, j * 128:(j + 1) * 128],
                                             rhs=cact[:, k:k + 1], start=(k == 0), stop=(k == 15))
                            return r
                        P.op('pe', mmf, reads=rd, writes=[('ps', 0)])
                else:
                    pb = 1 + (s % 2)

                    def mmf(e, b=b, pb=pb):
                        for k in range(16):
                            r = e.matmul(ps[pb][0:1, :], lhsT=cact[:, k:k + 1], rhs=wbf[b][:, k, :],
                                         start=(k == 0), stop=(k == 15))
                        return r
                    P.op('pe', mmf, reads=rd, writes=[('ps', pb)])
                    c0 = (s - 8) * 512
                    P.op('dve', lambda e, pb=pb, c0=c0: e.tensor_tensor(
                        out=mrow[0:1, c0:c0 + 512], in0=ps[pb][0:1, :], in1=abr[0:1, c0:c0 + 512], op=ALU.add),
                        reads=[('ps', pb), 'abr'], writes=[('mrow', s)])
            P.op('dve', lambda e: e.tensor_tensor(out=mcol[:], in0=ps[0][:, 0:32], in1=abc[:], op=ALU.add),
                 reads=[('ps', 0), 'abc'], writes=['mcol'])
            P.op('dve', lambda e: e.scalar_tensor_tensor(out=A1[:], in0=mcol[:, 16:32], scalar=1.0, in1=n1g[:],
                                                         op0=ALU.add, op1=ALU.mult),
                 reads=['mcol', 'n1g'], writes=['A1'])
            P.op('dve', lambda e: e.tensor_copy(out=SH1[:], in_=mcol[:, 0:16]), reads=['mcol'], writes=['SH1'])
            n2r = sb(st, 'n2r', [1, D])
            P.dma('sp', lambda e: e.dma_start(out=n2r[:], in_=rows_d[0:1, 0:D]), writes=['n2r'])
            P.op('dve', lambda e: e.scalar_tensor_tensor(out=mrow[0:1, 2 * D:3 * D], in0=mrow[0:1, 2 * D:3 * D], scalar=1.0, in1=n2r[:],
                                                         op0=ALU.add, op1=ALU.mult),
                 reads=[('mrow', s) for s in range(16, 20)] + ['n2r'], writes=[('mrow', s) for s in range(16, 20)])
            P.dma('sp', lambda e: e.dma_start(out=modrow, in_=mrow[:]),
                  reads=[('mrow', s) for s in range(8, 24)], writes=['modrow'])
            P.barrier()
            P.flush()

        with ExitStack() as mx:
            rbbc = sb(mx, 'rbbc', [128, 32])
            rwt = sb(mx, 'rwt', [128, 16, 32])
            P.dma('sp', lambda e: e.dma_start(out=rbbc[:], in_=rows_d[0:1, 2 * D:2 * D + 32].to_broadcast([128, 32])),
                  writes=['rbbc'])
            P.dma('sp', lambda e: e.dma_start(out=rwt[:], in_=rw_l), writes=['rwt'])
            S_rw = sb(mx, 'S_rw', [128, 8, 128])
            S_hg = sb(mx, 'S_hg', [128, 8, 128])
            carry = sb(mx, 'carry', [128, 32])
            lbc = sb(mx, 'lbc', [128, 8])
            omlc = sb(mx, 'omlc', [128, 8])
            ommc = sb(mx, 'ommc', [128, 27])
            w2p = sb(mx, 'w2p', [128, 1024])
            a2p = sb(mx, 'a2p', [128, 1024])
            g2a = sb(mx, 'g2a', [128, 1024])
            g2b = sb(mx, 'g2b', [128, 1024])
            maskb_all = sb(mx, 'maskb_all', [128, NT, 32], BF16)
            triS_bf = sb(mx, 'triS_bf', [128, 128], BF16)
            P.op('pool', lambda e: e.memset(S_rw[:], 0.0), writes=['S_rw'])
            P.op('pool', lambda e: e.memset(S_hg[:], 0.0), writes=['S_hg'])
            P.op('pool', lambda e: e.memset(carry[:], 0.0), writes=['carry'])
            P.dma('sp', lambda e: e.dma_start(out=w2p[:], in_=w2p_d), writes=['w2p'])
            P.dma('sp', lambda e: e.dma_start(out=a2p[:], in_=a2p_d), writes=['a2p'])
            P.dma('sp', lambda e: e.dma_start(out=g2a[:], in_=g2a_d), writes=['g2a'])
            P.dma('sp', lambda e: e.dma_start(out=g2b[:], in_=g2b_d), writes=['g2b'])
            P.op('dve', lambda e: e.tensor_copy(out=triS_bf[:], in_=strictU), reads=['cst'], writes=['triS_bf'])
            P.op('dve', lambda e: e.tensor_tensor(out=lbc[:], in0=col('lb0', 0, 8), in1=col('lb1', 0, 8), op=ALU.subtract),
                 reads=['cols'], writes=['lbc'])
            P.op('act', lambda e: e.activation(out=lbc[:], in_=lbc[:], func=AF.Sigmoid), reads=['lbc'], writes=['lbc'])
            P.op('dve', lambda e: e.tensor_scalar(out=omlc[:], in0=lbc[:], scalar1=-1.0, scalar2=1.0, op0=ALU.mult, op1=ALU.add),
                 reads=['lbc'], writes=['omlc'])
            P.op('dve', lambda e: e.tensor_scalar(out=ommc[:], in0=col('mu', 0, 27), scalar1=-1.0, scalar2=1.0,
                                                  op0=ALU.mult, op1=ALU.add), reads=['cols'], writes=['ommc'])

            NSTG, NBFB = 2, 2
            stg = [sb(mx, 'stg%d' % i, [128, 16, 128]) for i in range(NSTG)]
            wcb = [sb(mx, 'wcb%d' % i, [128, 16, 128], BF16) for i in range(NBFB)]
            cln = [0]

            def load_chunk(src, kdim=16):
                n = cln[0]
                cln[0] += 1
                s_, b_ = n % NSTG, n % NBFB
                P.dma('sp', lambda e: e.dma_start(out=stg[s_][:, 0:kdim, :], in_=src), writes=[('stg', s_)])
                if n % 2 == 0:
                    P.op('act', lambda e: e.copy(out=wcb[b_][:, 0:kdim, :], in_=stg[s_][:, 0:kdim, :]),
                         reads=[('stg', s_)], writes=[('wcb', b_)])
                else:
                    P.op('pool', lambda e: e.tensor_copy(out=wcb[b_][:, 0:kdim, :], in_=stg[s_][:, 0:kdim, :]),
                         reads=[('stg', s_)], writes=[('wcb', b_)])
                return wcb[b_], ('wcb', b_)

            for tb in range(min(NB, NBLK_DBG[0]) if stages >= 1.5 else 0):
                t0_ = tb * 512
                with ExitStack() as bk:
                    hT = sb(bk, 'hT', [128, 16, 512], BF16)
                    yaT = sb(bk, 'yaT', [128, 8, 512], BF16)
                    ybT = sb(bk, 'ybT', [128, 8, 512], BF16)
                    praw = [sb(bk, 'praw%d' % i, [128, 513]) for i in range(2)]
                    shtmp = sb(bk, 'shtmp', [128, 512])
                    prn = [0]

                    with ExitStack() as sB:
                        xt = sb(sB, 'xt', [128, 16, 512])
                        sq = sb(sB, 'sq', [128, 16, 512], BF16)
                        rstd = sb(sB, 'rstd', [128, 1, 512])
                        P.dma('sp', lambda e: e.dma_start(
                            out=xt[:], in_=xT.rearrange("(k p) t -> p k t", p=128)[:, :, t0_:t0_ + 512]), writes=['xt'])
                        P.op('act', lambda e: e.activation(out=sq[:], in_=xt[:], func=AF.Square), reads=['xt'], writes=['sq'])
                        pb = nps()

                        def mmf(e, pb=pb):
                            for k in range(16):
                                r = e.matmul(ps[pb][:, :], lhsT=ones_bf[:], rhs=sq[:, k, :], start=(k == 0), stop=(k == 15))
                            return r
                        P.op('pe', mmf, reads=['sq', 'ones_bf'], writes=[('ps', pb)])
                        P.op('dve', lambda e, pb=pb: e.tensor_scalar(out=rstd[:, 0, :], in0=ps[pb][:, :], scalar1=1.0 / D, scalar2=1e-5,
                                                                     op0=ALU.mult, op1=ALU.add), reads=[('ps', pb)], writes=['rstd'])
                        rsqrt(lambda: rstd[:, 0, :], 'rstd')
                        P.op('dve', lambda e: e.tensor_tensor(out=xt[:], in0=xt[:], in1=rstd[:].to_broadcast([128, 16, 512]),
                                                              op=ALU.mult), reads=['xt', 'rstd'], writes=['xt'])
                        for k in range(16):
                            P.op('act', lambda e, k=k: e.activation(out=hT[:, k, :], in_=xt[:, k, :], func=AF.Identity,
                                                                    scale=A1[:, k:k + 1], bias=SH1[:, k:k + 1]),
                                 reads=['xt', 'A1', 'SH1'], writes=['hT'])
                        if tb == 0:
                            tap('hT', hT[:], 'hT')
                        P.barrier()
                        P.flush()

                    if stages < 2:
                        continue

                    def proj_fm(ci, M=128):
                        w, wk = load_chunk(w_in_l[ci])
                        pb = nps()

                        def f(e):
                            for k in range(16):
                                r = e.matmul(ps[pb][0:M, :], lhsT=w[:, k, 0:M], rhs=hT[:, k, :], start=(k == 0), stop=(k == 15))
                            return r
                        P.op('pe', f, reads=[wk, 'hT'], writes=[('ps', pb)])
                        return pb

                    def shift(pb, ci, out_ap, okey, M=128):
                        i = prn[0] % 2
                        prn[0] += 1
                        pr, pk = praw[i], ('praw', i)
                        P.op('act', lambda e: e.copy(out=pr[0:M, 1:513], in_=ps[pb][0:M, :]), reads=[('ps', pb)], writes=[pk])
                        P.op('dve', lambda e: e.tensor_copy(out=pr[0:M, 0:1], in_=carry[0:M, ci:ci + 1]), reads=['carry'], writes=[pk])
                        P.op('pool', lambda e: e.tensor_scalar(out=shtmp[0:M, :], in0=pr[0:M, 0:512], scalar1=col('mu', ci)[0:M, :],
                                                               scalar2=None, op0=ALU.mult), reads=[pk, 'cols'], writes=['shtmp'])
                        P.op('dve', lambda e: e.scalar_tensor_tensor(out=out_ap, in0=pr[0:M, 1:513], scalar=ommc[0:M, ci:ci + 1],
                                                                     in1=shtmp[0:M, :], op0=ALU.mult, op1=ALU.add),
                             reads=[pk, 'shtmp', 'ommc'], writes=[okey])
                        P.op('dve', lambda e: e.tensor_copy(out=carry[0:M, ci:ci + 1], in_=pr[0:M, 512:513]), reads=[pk], writes=['carry'])

                    with ExitStack() as sC:
                        was = sb(sC, 'was', [128, 512])
                        thw = sb(sC, 'thw', [128, 512])
                        sg0 = sb(sC, 'sg0', [128, 512])
                        sg1 = sb(sC, 'sg1', [128, 512])
                        P.op('pool', lambda e: e.memset(sg1[:], 0.0), writes=['sg1'])
                        pb = proj_fm(CI_WA)
                        shift(pb, 24, was[:], 'was')
                        P.op('act', lambda e: e.activation(out=thw[:], in_=was[:, :], func=AF.Tanh), reads=['was'], writes=['thw'])
                        pb = proj_fm(CI_GD0)
                        shift(pb, 25, sg0[:], 'sg0')
                        P.op('act', lambda e: e.activation(out=sg0[:], in_=sg0[:], func=AF.Sigmoid), reads=['sg0'], writes=['sg0'])
                        pb = proj_fm(CI_GD1, M=32)
                        shift(pb, 26, sg1[0:32, :], 'sg1', M=32)
                        P.op('act', lambda e: e.activation(out=sg1[0:32, :], in_=sg1[0:32, :], func=AF.Sigmoid), reads=['sg1'], writes=['sg1'])

                        names = ['r_t', 'k_t', 'v_t', 'a_t', 'lw', 'g_t', 'cum', 'c_t', 'E1', 'E2', 'kk', 'km', 'b_t', 'bon', 'Bt', 'Kt', 'tmpA', 'tmpB', 'Bt0', 'Bt1', 'Kt0', 'Kt1']
                        Tt = {n: sb(sC, n, [128, 4, 128]) for n in names}
                        KR = sb(sC, 'KR', [128, 4, 2, 128])
                        BKV = sb(sC, 'BKV', [128, 4, 3, 128])
                        eref = sb(sC, 'eref', [128, 4])
                        ecl = sb(sC, 'ecl', [128, 4])
                        Msc = [sb(sC, 'Msc%d' % u, [128, 512]) for u in range(8)]
                        Xb = [[sb(sC, 'X%d_%d' % (u, v), [128, 128]) for v in range(2)] for u in range(8)]
                        XTb = [[sb(sC, 'XT%d_%d' % (u, v), [128, 128]) for v in range(2)] for u in range(8)]
                        Tm = [sb(sC, 'Tm%d' % u, [128, 128]) for u in range(8)]
                        S0p = sb(sC, 'S0p', [128, 128])
                        nG = sb(sC, 'nG', [128, 128])
                        Ut = sb(sC, 'Ut', [128, 128])
                        ytm = sb(sC, 'ytm', [128, 4, 128])
                        ysq = Tt['tmpA']
                        st8 = sb(sC, 'st8', [128, 4, 8])

                        def F2(n):
                            return Tt[n][:].rearrange("p a b -> p (a b)")

                        for j in range(8):
                            jc = slice(j * 128, (j + 1) * 128)
                            pb = nps()
                            P.op('pe', lambda e, pb=pb, jc=jc: e.matmul(ps[pb][:, :], lhsT=a2p[:, jc], rhs=was[:, :], start=True, stop=True),
                                 reads=['a2p', 'was'], writes=[('ps', pb)])
                            P.op('act', lambda e, pb=pb, j=j: e.activation(out=F2('a_t'), in_=ps[pb][:, :], func=AF.Sigmoid, bias=col('a0', j)),
                                 reads=[('ps', pb), 'cols'], writes=['a_t'])
                            pb = nps()
                            P.op('pe', lambda e, pb=pb, jc=jc: e.matmul(ps[pb][:, :], lhsT=w2p[:, jc], rhs=thw[:, :], start=True, stop=True),
                                 reads=['w2p', 'thw'], writes=[('ps', pb)])
                            P.op('act', lambda e, pb=pb, j=j: e.activation(out=F2('lw'), in_=ps[pb][:, :], func=AF.Sigmoid, bias=col('w0', j)),
                                 reads=[('ps', pb), 'cols'], writes=['lw'])
                            P.op('pool', lambda e: e.tensor_scalar(out=F2('lw'), in0=F2('lw'), scalar1=-0.6065306597126334, scalar2=None, op0=ALU.mult),
                                 reads=['lw'], writes=['lw'])
                            pb = nps()

                            def gmm(e, pb=pb, jc=jc):
                                e.matmul(ps[pb][:, :], lhsT=g2a[:, jc], rhs=sg0[:, :], start=True, stop=False)
                                return e.matmul(ps[pb][:, :], lhsT=g2b[:, jc], rhs=sg1[:, :], start=False, stop=True)
                            P.op('pe', gmm, reads=['g2a', 'g2b', 'sg0', 'sg1'], writes=[('ps', pb)])
                            P.op('act', lambda e, pb=pb: e.copy(out=F2('g_t'), in_=ps[pb][:, :]), reads=[('ps', pb)], writes=['g_t'])
                            for nm, ci in (('r_t', j), ('k_t', 8 + j), ('v_t', 16 + j)):
                                pb = proj_fm(ci)
                                shift(pb, ci, F2(nm), nm)
                            if tb == 0 and j == 0:
                                tap('r0', F2('r_t'), 'r_t')
                                tap('a0', F2('a_t'), 'a_t')
                                tap('lw0', F2('lw'), 'lw')
                            for i in range(4):
                                P.op('dve', lambda e, i=i: e.tensor_tensor_scan(out=Tt['cum'][:, i, :], data0=Tt['lw'][:, i, :], data1=zeros,
                                                                               initial=0.0, op0=ALU.add, op1=ALU.add),
                                     reads=['lw', 'cst'], writes=['cum'])
                            for i in range(4):
                                P.op('dve', lambda e, i=i: e.tensor_scalar(out=Tt['c_t'][:, i, :], in0=Tt['cum'][:, i, :], scalar1=Tt['cum'][:, i, 63:64],
                                                                          scalar2=None, op0=ALU.subtract), reads=['cum'], writes=['c_t'])
                            P.op('act', lambda e: e.activation(out=F2('E1'), in_=F2('c_t'), func=AF.Exp, scale=-1.0), reads=['c_t'], writes=['E1'])
                            P.op('act', lambda e: e.activation(out=F2('E2'), in_=F2('c_t'), func=AF.Exp), reads=['c_t'], writes=['E2'])
                            P.op('pool', lambda e: e.tensor_tensor(out=F2('tmpA'), in0=F2('c_t'), in1=F2('lw'), op=ALU.subtract),
                                 reads=['c_t', 'lw'], writes=['tmpA'])
                            P.op('act', lambda e: e.activation(out=F2('tmpA'), in_=F2('tmpA'), func=AF.Exp), reads=['tmpA'], writes=['tmpA'])
                            P.op('act', lambda e: e.activation(out=eref[:], in_=Tt['cum'][:, :, 63], func=AF.Exp), reads=['cum'], writes=['eref'])
                            P.op('act', lambda e: e.activation(out=ecl[:], in_=Tt['c_t'][:, :, 127], func=AF.Exp), reads=['c_t'], writes=['ecl'])
                            P.op('dve', lambda e, j=j: e.tensor_scalar(out=F2('kk'), in0=F2('k_t'), scalar1=col('k_k', j), scalar2=None, op0=ALU.mult),
                                 reads=['k_t', 'cols'], writes=['kk'])
                            P.op('pool', lambda e: e.tensor_tensor(out=F2('tmpB'), in0=F2('kk'), in1=F2('kk'), op=ALU.mult), reads=['kk'], writes=['tmpB'])
                            pb = nps()
                            P.op('pe', lambda e, pb=pb: e.matmul(ps[pb][:, :], lhsT=bd64, rhs=F2('tmpB'), start=True, stop=True),
                                 reads=['cst', 'tmpB'], writes=[('ps', pb)])
                            P.op('dve', lambda e, pb=pb: e.tensor_scalar(out=F2('tmpB'), in0=ps[pb][:, :], scalar1=1e-24, scalar2=None, op0=ALU.max),
                                 reads=[('ps', pb)], writes=['tmpB'])
                            rsqrt(lambda: F2('tmpB'), 'tmpB')
                            P.op('dve', lambda e: e.tensor_tensor(out=F2('kk'), in0=F2('kk'), in1=F2('tmpB'), op=ALU.mult), reads=['kk', 'tmpB'], writes=['kk'])
                            P.op('dve', lambda e, j=j: e.tensor_scalar(out=F2('km'), in0=F2('a_t'), scalar1=-1.0, scalar2=col('k_a', j), op0=ALU.add, op1=ALU.mult),
                                 reads=['a_t', 'cols'], writes=['km'])
                            P.op('dve', lambda e: e.scalar_tensor_tensor(out=F2('km'), in0=F2('km'), scalar=1.0, in1=F2('k_t'), op0=ALU.add, op1=ALU.mult),
                                 reads=['km', 'k_t'], writes=['km'])
                            P.op('pool', lambda e: e.tensor_tensor(out=F2('b_t'), in0=F2('kk'), in1=F2('a_t'), op=ALU.mult), reads=['kk', 'a_t'], writes=['b_t'])
                            P.op('dve', lambda e, j=j: e.scalar_tensor_tensor(out=F2('tmpB'), in0=F2('r_t'), scalar=col('r_k', j), in1=F2('km'), op0=ALU.mult, op1=ALU.mult),
                                 reads=['r_t', 'km', 'cols'], writes=['tmpB'])
                            pb = nps()
                            P.op('pe', lambda e, pb=pb: e.matmul(ps[pb][:, :], lhsT=bd64, rhs=F2('tmpB'), start=True, stop=True),
                                 reads=['cst', 'tmpB'], writes=[('ps', pb)])
                            P.op('dve', lambda e, pb=pb: e.tensor_tensor(out=F2('bon'), in0=ps[pb][:, :], in1=F2('v_t'), op=ALU.mult),
                                 reads=[('ps', pb), 'v_t'], writes=['bon'])
                            P.op('dve', lambda e: e.tensor_tensor(out=F2('Bt'), in0=F2('b_t'), in1=F2('E1'), op=ALU.mult), reads=['b_t', 'E1'], writes=['Bt'])
                            P.op('pool', lambda e: e.tensor_tensor(out=F2('Kt'), in0=F2('km'), in1=F2('E1'), op=ALU.mult), reads=['km', 'E1'], writes=['Kt'])
                            for h2 in range(2):
                                hm = bd64[:, h2 * 64:h2 * 64 + 1]
                                P.op('dve', lambda e, h2=h2, hm=hm: e.tensor_scalar(out=F2('Bt%d' % h2), in0=F2('Bt'), scalar1=hm, scalar2=None, op0=ALU.mult),
                                     reads=['Bt', 'cst'], writes=['Bt%d' % h2])
                                P.op('pool', lambda e, h2=h2, hm=hm: e.tensor_scalar(out=F2('Kt%d' % h2), in0=F2('Kt'), scalar1=hm, scalar2=None, op0=ALU.mult),
                                     reads=['Kt', 'cst'], writes=['Kt%d' % h2])
                            P.op('dve', lambda e: e.tensor_tensor(out=KR[:, :, 0, :], in0=Tt['kk'][:], in1=Tt['tmpA'][:], op=ALU.mult), reads=['kk', 'tmpA'], writes=['KR'])
                            P.op('pool', lambda e: e.tensor_tensor(out=KR[:, :, 1, :], in0=Tt['r_t'][:], in1=Tt['E2'][:], op=ALU.mult), reads=['r_t', 'E2'], writes=['KR'])
                            for i in range(4):
                                pb = nps()

                                def trf(e, pb=pb, i=i):
                                    e.transpose(ps[pb][:, 0:128], Tt['Bt'][:, i, :], ident)
                                    e.transpose(ps[pb][:, 128:256], Tt['Kt'][:, i, :], ident)
                                    return e.transpose(ps[pb][:, 256:384], Tt['v_t'][:, i, :], ident)
                                P.op('pe', trf, reads=['Bt', 'Kt', 'v_t', 'cst'], writes=[('ps', pb)])
                                P.op('act', lambda e, pb=pb, i=i: e.copy(out=BKV[:, i, :, :].rearrange("p a b -> p (a b)"), in_=ps[pb][:, 0:384]),
                                     reads=[('ps', pb)], writes=[('BKV', i)])
                            for i in range(4):
                                for h2 in range(2):
                                    u = i * 2 + h2
                                    hs = slice(h2 * 64, h2 * 64 + 64)
                                    pb = nps()

                                    def scf(e, pb=pb, i=i, h2=h2):
                                        krr = KR[:, i, :, :].rearrange("p a b -> p (a b)")
                                        e.matmul(ps[pb][:, 0:256], lhsT=Tt['Bt%d' % h2][:, i, :], rhs=krr, start=True, stop=True)
                                        return e.matmul(ps[pb][:, 256:512], lhsT=Tt['Kt%d' % h2][:, i, :], rhs=krr, start=True, stop=True)
                                    P.op('pe', scf, reads=['Bt%d' % h2, 'Kt%d' % h2, 'KR'], writes=[('ps', pb)])
                                    P.op('dve', lambda e, pb=pb, u=u: e.tensor_tensor(out=Msc[u][:], in0=ps[pb][:, :], in1=mask4, op=ALU.mult),
                                         reads=[('ps', pb), 'cst'], writes=[('Msc', u)])
                                    pb = nps()
                                    P.op('pe', lambda e, pb=pb, i=i, h2=h2: e.matmul(ps[pb][:, 0:128], lhsT=KR[:, i, 0, :], rhs=Tt['Bt%d' % h2][:, i, :], start=True, stop=True),
                                         reads=['Bt%d' % h2, 'KR'], writes=[('ps', pb)])
                                    P.op('dve', lambda e, pb=pb, u=u: e.tensor_tensor(out=XTb[u][0][:], in0=ps[pb][:, 0:128], in1=strictL, op=ALU.mult),
                                         reads=[('ps', pb), 'cst'], writes=[('XT', u, 0)])
                                    P.op('pool', lambda e, u=u: e.tensor_copy(out=Xb[u][0][:], in_=Msc[u][:, 0:128]), reads=[('Msc', u)], writes=[('X', u, 0)])
                                    P.op('pool', lambda e, u=u: e.tensor_tensor(out=Tm[u][:], in0=ident, in1=Msc[u][:, 0:128], op=ALU.subtract),
                                         reads=[('Msc', u), 'cst'], writes=[('Tm', u)])
                            for lvl in range(6):
                                a_, b_ = lvl % 2, (lvl + 1) % 2
                                for u in range(8):
                                    pb = nps()

                                    def sqf(e, pb=pb, u=u, a_=a_, last=(lvl == 5)):
                                        r = e.matmul(ps[pb][:, 128:256], lhsT=Xb[u][a_][:], rhs=XTb[u][a_][:], start=True, stop=True)
                                        if not last:
                                            r = e.matmul(ps[pb][:, 0:128], lhsT=XTb[u][a_][:], rhs=Xb[u][a_][:], start=True, stop=True)
                                        return r
                                    P.op('pe', sqf, reads=[('X', u, a_), ('XT', u, a_)], writes=[('ps', pb)])
                                    P.op('dve', lambda e, pb=pb, u=u, b_=b_: e.tensor_copy(out=XTb[u][b_][:], in_=ps[pb][:, 128:256]),
                                         reads=[('ps', pb)], writes=[('XT', u, b_)])
                                    if lvl < 5:
                                        P.op('dve', lambda e, pb=pb, u=u, b_=b_: e.tensor_copy(out=Xb[u][b_][:], in_=ps[pb][:, 0:128]),
                                             reads=[('ps', pb)], writes=[('X', u, b_)])
                                for u in range(8):
                                    pb = nps()
                                    P.op('pe', lambda e, pb=pb, u=u, b_=b_: e.matmul(ps[pb][:, 0:128], lhsT=XTb[u][b_][:], rhs=Tm[u][:], start=True, stop=True),
                                         reads=[('XT', u, b_), ('Tm', u)], writes=[('ps', pb)])
                                    P.op('dve', lambda e, pb=pb, u=u: e.tensor_tensor(out=Tm[u][:], in0=ps[pb][:, 0:128], in1=Tm[u][:], op=ALU.add),
                                         reads=[('ps', pb), ('Tm', u)], writes=[('Tm', u)])
                            Sj = S_rw[:, j, :]
                            for i in range(4):
                                P.op('dve', lambda e, i=i, Sj=Sj: e.tensor_scalar(out=S0p[:], in0=Sj, scalar1=eref[:, i:i + 1], scalar2=None, op0=ALU.mult),
                                     reads=['S_rw', 'eref'], writes=['S0p'])
                                pb = nps()

                                def gf(e, pb=pb, i=i):
                                    e.matmul(ps[pb][:, 0:128], lhsT=KR[:, i, 0, :], rhs=S0p[:], start=True, stop=False)
                                    for h2 in range(2):
                                        r = e.matmul(ps[pb][:, h2 * 64:h2 * 64 + 64], lhsT=Msc[i * 2 + h2][:, 256:384],
                                                     rhs=BKV[:, i, 2, h2 * 64:h2 * 64 + 64], start=False, stop=(h2 == 1))
                                    return r
                                P.op('pe', gf, reads=['KR', 'S0p', ('Msc', 2 * i), ('Msc', 2 * i + 1), ('BKV', i)], writes=[('ps', pb)])
                                P.op('dve', lambda e, pb=pb: e.tensor_scalar(out=nG[:], in0=ps[pb][:, 0:128], scalar1=-1.0, scalar2=None, op0=ALU.mult),
                                     reads=[('ps', pb)], writes=['nG'])
                                pb = nps()

                                def uf(e, pb=pb, i=i):
                                    for h2 in range(2):
                                        r = e.matmul(ps[pb][:, h2 * 64:h2 * 64 + 64], lhsT=Tm[i * 2 + h2][:], rhs=nG[:, h2 * 64:h2 * 64 + 64], start=True, stop=True)
                                    return r
                                P.op('pe', uf, reads=[('Tm', 2 * i), ('Tm', 2 * i + 1), 'nG'], writes=[('ps', pb)])
                                P.op('act', lambda e, pb=pb: e.copy(out=Ut[:], in_=ps[pb][:, 0:128]), reads=[('ps', pb)], writes=['Ut'])
                                pb = nps()

                                def yf(e, pb=pb, i=i):
                                    e.matmul(ps[pb][:, 0:128], lhsT=KR[:, i, 1, :], rhs=S0p[:], start=True, stop=False)
                                    for h2 in range(2):
                                        cs = slice(h2 * 64, h2 * 64 + 64)
                                        e.matmul(ps[pb][:, cs], lhsT=Msc[i * 2 + h2][:, 128:256], rhs=Ut[:, cs], start=False, stop=False)
                                        r = e.matmul(ps[pb][:, cs], lhsT=Msc[i * 2 + h2][:, 384:512], rhs=BKV[:, i, 2, cs], start=False, stop=(h2 == 1))
                                    return r
                                P.op('pe', yf, reads=['KR', 'S0p', ('Msc', 2 * i), ('Msc', 2 * i + 1), 'Ut', ('BKV', i)], writes=[('ps', pb)])
                                P.op('act', lambda e, pb=pb, i=i: e.copy(out=ytm[:, i, :], in_=ps[pb][:, 0:128]), reads=[('ps', pb)], writes=[('ytm', i)])
                                pb = nps()

                                def sf(e, pb=pb, i=i):
                                    e.matmul(ps[pb][:, 0:128], lhsT=ident, rhs=S0p[:], start=True, stop=False)
                                    e.matmul(ps[pb][:, 0:128], lhsT=BKV[:, i, 0, :], rhs=Ut[:], start=False, stop=False)
                                    return e.matmul(ps[pb][:, 0:128], lhsT=BKV[:, i, 1, :], rhs=BKV[:, i, 2, :], start=False, stop=True)
                                P.op('pe', sf, reads=['cst', 'S0p', ('BKV', i), 'Ut'], writes=[('ps', pb)])
                                P.op('dve', lambda e, pb=pb, i=i, Sj=Sj: e.scalar_tensor_tensor(out=Sj, in0=ps[pb][:, 0:128], scalar=ecl[:, i:i + 1], in1=bd64,
                                                                                             op0=ALU.mult, op1=ALU.mult),
                                     reads=[('ps', pb), 'ecl', 'cst'], writes=['S_rw'])
                            if tb == 0 and j == 0:
                                tap('y0', ytm[:], ('ytm', 3))
                            yk = [('ytm', i) for i in range(4)]
                            yv = ytm[:].rearrange("p a (g n) -> p (a g) n", g=2)
                            P.op('pool', lambda e: e.tensor_tensor(out=ysq[:], in0=ytm[:], in1=ytm[:], op=ALU.mult), reads=yk, writes=['ysq'])
                            P.op('dve', lambda e: e.reduce_sum(out=st8[:, 0, :], in_=yv, axis=AX.X), reads=yk, writes=['st8'])
                            P.op('dve', lambda e: e.reduce_sum(out=st8[:, 1, :], in_=ysq[:].rearrange("p a (g n) -> p (a g) n", g=2), axis=AX.X),
                                 reads=['ysq'], writes=['st8'])
                            P.op('dve', lambda e: e.tensor_scalar(out=st8[:, 2, :], in0=st8[:, 0, :], scalar1=1.0 / 64, scalar2=None, op0=ALU.mult),
                                 reads=['st8'], writes=['st8'])
                            P.op('dve', lambda e: e.tensor_tensor(out=st8[:, 0, :], in0=st8[:, 2, :], in1=st8[:, 2, :], op=ALU.mult), reads=['st8'], writes=['st8'])
                            P.op('dve', lambda e: e.scalar_tensor_tensor(out=st8[:, 3, :], in0=st8[:, 1, :], scalar=1.0 / 64, in1=st8[:, 0, :],
                                                                         op0=ALU.mult, op1=ALU.subtract), reads=['st8'], writes=['st8'])
                            P.op('dve', lambda e: e.tensor_scalar(out=st8[:, 3, :], in0=st8[:, 3, :], scalar1=64e-5, scalar2=None, op0=ALU.add),
                                 reads=['st8'], writes=['st8'])
                            rsqrt(lambda: st8[:, 3, :], 'st8')
                            for g in range(8):
                                P.op('dve', lambda e, g=g: e.tensor_scalar(out=ytm[:, g // 2, (g % 2) * 64:(g % 2) * 64 + 64],
                                                                          in0=ytm[:, g // 2, (g % 2) * 64:(g % 2) * 64 + 64],
                                                                          scalar1=st8[:, 2, g:g + 1], scalar2=st8[:, 3, g:g + 1],
                                                                          op0=ALU.subtract, op1=ALU.mult),
                                     reads=yk + ['st8'], writes=yk)
                            pb = nps()

                            def ytr(e, pb=pb):
                                for i in range(4):
                                    r = e.transpose(ps[pb][:, i * 128:(i + 1) * 128], ytm[:, i, :], ident)
                                return r
                            P.op('pe', ytr, reads=yk + ['cst'], writes=[('ps', pb)])
                            P.op('act', lambda e, pb=pb, j=j: e.activation(out=F2('tmpB'), in_=ps[pb][:, :], func=AF.Identity,
                                                                           scale=col('gn_g', j), bias=col('gn_b', j)),
                                 reads=[('ps', pb), 'cols'], writes=['tmpB'])
                            P.op('dve', lambda e: e.tensor_tensor(out=F2('tmpB'), in0=F2('tmpB'), in1=F2('bon'), op=ALU.add), reads=['tmpB', 'bon'], writes=['tmpB'])
                            P.op('dve', lambda e, j=j: e.tensor_tensor(out=yaT[:, j, :], in0=F2('tmpB'), in1=F2('g_t'), op=ALU.mult),
                                 reads=['tmpB', 'g_t'], writes=['yaT'])
                        if tb == 0:
                            tap('yaT', yaT[:], 'yaT')
                        P.barrier()
                        P.flush()

                    if stages < 3:
                        continue
                    with ExitStack() as sD:
                        names = ['q_t', 'fg', 'lf', 'kf', 'cum', 'c_t', 'Eq', 'Ek', 'qd', 'kd', 'vtm', 'sog', 'otm', 'osq', 'kdtm']
                        Tt = {n: sb(sD, 'h_' + n, [128, 4, 128]) for n in names}
                        eref = sb(sD, 'h_eref', [128, 4])
                        ecl = sb(sD, 'h_ecl', [128, 4])
                        AT = sb(sD, 'h_AT', [128, 128])
                        S0p = sb(sD, 'h_S0p', [128, 128])
                        rs4 = sb(sD, 'h_rs4', [128, 4])

                        def F2(n):
                            return Tt[n][:].rearrange("p a b -> p (a b)")

                        for h in range(8):
                            pb = proj_fm(CI_HQ + h)
                            P.op('act', lambda e, pb=pb: e.activation(out=F2('q_t'), in_=ps[pb][:, :], func=AF.Silu), reads=[('ps', pb)], writes=['q_t'])
                            pb = proj_fm(CI_HF + h)
                            P.op('act', lambda e, pb=pb: e.activation(out=F2('fg'), in_=ps[pb][:, :], func=AF.Sigmoid), reads=[('ps', pb)], writes=['fg'])
                            P.op('dve', lambda e, h=h: e.tensor_scalar(out=F2('fg'), in0=F2('fg'), scalar1=omlc[:, h:h + 1], scalar2=lbc[:, h:h + 1],
                                                                      op0=ALU.mult, op1=ALU.add), reads=['fg', 'omlc', 'lbc'], writes=['fg'])
                            P.op('act', lambda e: e.activation(out=F2('lf'), in_=F2('fg'), func=AF.Ln), reads=['fg'], writes=['lf'])
                            P.op('pool', lambda e: e.tensor_scalar(out=F2('kf'), in0=F2('fg'), scalar1=-1.0, scalar2=1.0, op0=ALU.mult, op1=ALU.add),
                                 reads=['fg'], writes=['kf'])
                            for i in range(4):
                                P.op('dve', lambda e, i=i: e.tensor_tensor_scan(out=Tt['cum'][:, i, :], data0=Tt['lf'][:, i, :], data1=zeros,
                                                                               initial=0.0, op0=ALU.add, op1=ALU.add), reads=['lf', 'cst'], writes=['cum'])
                            for i in range(4):
                                P.op('dve', lambda e, i=i: e.tensor_scalar(out=Tt['c_t'][:, i, :], in0=Tt['cum'][:, i, :], scalar1=Tt['cum'][:, i, 63:64],
                                                                          scalar2=None, op0=ALU.subtract), reads=['cum'], writes=['c_t'])
                            P.op('act', lambda e: e.activation(out=ecl[:], in_=Tt['c_t'][:, :, 127], func=AF.Exp), reads=['c_t'], writes=['h_ecl'])
                            P.op('act', lambda e: e.activation(out=eref[:], in_=Tt['cum'][:, :, 63], func=AF.Exp), reads=['cum'], writes=['h_eref'])
                            P.op('dve', lambda e: e.tensor_scalar(out=F2('c_t'), in0=F2('c_t'), scalar1=-40.0, scalar2=40.0, op0=ALU.max, op1=ALU.min),
                                 reads=['c_t', 'h_ecl'], writes=['c_t'])
                            P.op('act', lambda e: e.activation(out=F2('Eq'), in_=F2('c_t'), func=AF.Exp), reads=['c_t'], writes=['Eq'])
                            P.op('act', lambda e: e.activation(out=F2('Ek'), in_=F2('c_t'), func=AF.Exp, scale=-1.0), reads=['c_t'], writes=['Ek'])
                            P.op('dve', lambda e: e.tensor_tensor(out=F2('qd'), in0=F2('q_t'), in1=F2('Eq'), op=ALU.mult), reads=['q_t', 'Eq'], writes=['qd'])
                            P.op('pool', lambda e: e.tensor_tensor(out=F2('kd'), in0=F2('kf'), in1=F2('Ek'), op=ALU.mult), reads=['kf', 'Ek'], writes=['kd'])
                            for nm, cbase, fn in (('vtm', CI_HI, None), ('sog', CI_HO, AF.Silu)):
                                w, wk = load_chunk(w_in_l[cbase + h])
                                pb = nps()

                                def tmf(e, pb=pb, w=w):
                                    for i in range(4):
                                        for k in range(16):
                                            r = e.matmul(ps[pb][:, i * 128:(i + 1) * 128], lhsT=hT[:, k, i * 128:(i + 1) * 128], rhs=w[:, k, :],
                                                         start=(k == 0), stop=(k == 15))
                                    return r
                                P.op('pe', tmf, reads=[wk, 'hT'], writes=[('ps', pb)])
                                if fn is None:
                                    P.op('act', lambda e, pb=pb, nm=nm: e.copy(out=F2(nm), in_=ps[pb][:, :]), reads=[('ps', pb)], writes=[nm])
                                else:
                                    P.op('act', lambda e, pb=pb, nm=nm, fn=fn: e.activation(out=F2(nm), in_=ps[pb][:, :], func=fn), reads=[('ps', pb)], writes=[nm])
                            pb = nps()

                            def ktr(e, pb=pb):
                                for i in range(4):
                                    r = e.transpose(ps[pb][:, i * 128:(i + 1) * 128], Tt['kd'][:, i, :], ident)
                                return r
                            P.op('pe', ktr, reads=['kd', 'cst'], writes=[('ps', pb)])
                            P.op('act', lambda e, pb=pb: e.copy(out=F2('kdtm'), in_=ps[pb][:, :]), reads=[('ps', pb)], writes=['kdtm'])
                            Sh = S_hg[:, h, :]
                            for i in range(4):
                                pb = nps()
                                P.op('pe', lambda e, pb=pb, i=i: e.matmul(ps[pb][:, 0:128], lhsT=Tt['kd'][:, i, :], rhs=Tt['qd'][:, i, :], start=True, stop=True),
                                     reads=['kd', 'qd'], writes=[('ps', pb)])
                                P.op('dve', lambda e, pb=pb: e.tensor_tensor(out=AT[:], in0=ps[pb][:, 0:128], in1=inclU, op=ALU.mult),
                                     reads=[('ps', pb), 'cst'], writes=['h_AT'])
                                P.op('dve', lambda e, i=i, Sh=Sh: e.tensor_scalar(out=S0p[:], in0=Sh, scalar1=eref[:, i:i + 1], scalar2=None, op0=ALU.mult),
                                     reads=['S_hg', 'h_eref'], writes=['h_S0p'])
                                pb = nps()

                                def of(e, pb=pb, i=i):
                                    e.matmul(ps[pb][:, 0:128], lhsT=AT[:], rhs=Tt['vtm'][:, i, :], start=True, stop=False)
                                    return e.matmul(ps[pb][:, 0:128], lhsT=Tt['qd'][:, i, :], rhs=S0p[:], start=False, stop=True)
                                P.op('pe', of, reads=['h_AT', 'vtm', 'qd', 'h_S0p'], writes=[('ps', pb)])
                                P.op('act', lambda e, pb=pb, i=i: e.copy(out=Tt['otm'][:, i, :], in_=ps[pb][:, 0:128]), reads=[('ps', pb)], writes=['otm'])
                                pb = nps()

                                def sf(e, pb=pb, i=i):
                                    e.matmul(ps[pb][:, 0:128], lhsT=ident, rhs=S0p[:], start=True, stop=False)
                                    return e.matmul(ps[pb][:, 0:128], lhsT=Tt['kdtm'][:, i, :], rhs=Tt['vtm'][:, i, :], start=False, stop=True)
                                P.op('pe', sf, reads=['cst', 'h_S0p', 'kdtm', 'vtm'], writes=[('ps', pb)])
                                P.op('dve', lambda e, pb=pb, i=i, Sh=Sh: e.tensor_scalar(out=Sh, in0=ps[pb][:, 0:128], scalar1=ecl[:, i:i + 1], scalar2=None, op0=ALU.mult),
                                     reads=[('ps', pb), 'h_ecl'], writes=['S_hg'])
                            if tb == 0 and h == 0:
                                tap('o0', Tt['otm'][:], 'otm')
                            P.op('pool', lambda e: e.tensor_tensor(out=F2('osq'), in0=F2('otm'), in1=F2('otm'), op=ALU.mult), reads=['otm'], writes=['osq'])
                            P.op('dve', lambda e: e.reduce_sum(out=rs4[:], in_=Tt['osq'][:], axis=AX.X), reads=['osq'], writes=['h_rs4'])
                            P.op('dve', lambda e: e.tensor_scalar(out=rs4[:], in0=rs4[:], scalar1=1.0 / 128, scalar2=1e-5, op0=ALU.mult, op1=ALU.add),
                                 reads=['h_rs4'], writes=['h_rs4'])
                            rsqrt(lambda: rs4[:], 'h_rs4')
                            for i in range(4):
                                P.op('dve', lambda e, i=i: e.scalar_tensor_tensor(out=Tt['otm'][:, i, :], in0=Tt['otm'][:, i, :], scalar=rs4[:, i:i + 1],
                                                                                 in1=Tt['sog'][:, i, :], op0=ALU.mult, op1=ALU.mult),
                                     reads=['otm', 'h_rs4', 'sog'], writes=['otm'])
                            pb = nps()

                            def otr(e, pb=pb):
                                for i in range(4):
                                    r = e.transpose(ps[pb][:, i * 128:(i + 1) * 128], Tt['otm'][:, i, :], ident)
                                return r
                            P.op('pe', otr, reads=['otm', 'cst'], writes=[('ps', pb)])
                            P.op('act', lambda e, pb=pb, h=h: e.activation(out=ybT[:, h, :], in_=ps[pb][:, :], func=AF.Identity, scale=col('hgn', 0)),
                                 reads=[('ps', pb), 'cols'], writes=['ybT'])
                        if tb == 0:
                            tap('ybT', ybT[:], 'ybT')
                        P.barrier()
                        P.flush()

                    if stages < 4:
                        continue
                    with ExitStack() as sE:
                        mgT = sb(sE, 'mgT', [128, 16, 512], BF16)
                        sga = sb(sE, 'sga', [128, 512])
                        sgb = sb(sE, 'sgb', [128, 512])
                        tE = sb(sE, 'tE', [128, 512])
                        xb = sb(sE, 'xb', [128, 4, D])
                        g1bc = sb(sE, 'g1bc', [128, D])
                        A2bc = sb(sE, 'A2bc', [128, D])
                        sh2bc = sb(sE, 'sh2bc', [128, D])
                        P.dma('sp', lambda e: e.dma_start(out=g1bc[:], in_=modrow[0:1, 0:D].to_broadcast([128, D])), reads=['modrow'], writes=['g1bc'])
                        P.dma('sp', lambda e: e.dma_start(out=sh2bc[:], in_=modrow[0:1, D:2 * D].to_broadcast([128, D])), reads=['modrow'], writes=['sh2bc'])
                        P.dma('sp', lambda e: e.dma_start(out=A2bc[:], in_=modrow[0:1, 2 * D:3 * D].to_broadcast([128, D])), reads=['modrow'], writes=['A2bc'])
                        P.dma('sp', lambda e: e.dma_start(out=xb[:], in_=x_in[t0_:t0_ + 512, :].rearrange("(a p) d -> p a d", p=128)), writes=['xb'])
                        for dc in range(16):
                            pb = proj_fm(CI_GA + dc)
                            P.op('act', lambda e, pb=pb: e.activation(out=sga[:], in_=ps[pb][:, :], func=AF.Sigmoid), reads=[('ps', pb)], writes=['sga'])
                            pb = proj_fm(CI_GB + dc)
                            P.op('act', lambda e, pb=pb: e.activation(out=sgb[:], in_=ps[pb][:, :], func=AF.Sigmoid), reads=[('ps', pb)], writes=['sgb'])
                            for src_l, yT, sgt, first in ((pa_l, yaT, sga, True), (pb_l, ybT, sgb, False)):
                                w, wk = load_chunk(src_l[dc], kdim=8)
                                pb = nps()

                                def zf(e, pb=pb, w=w, yT=yT):
                                    for k in range(8):
                                        r = e.matmul(ps[pb][:, :], lhsT=w[:, k, :], rhs=yT[:, k, :], start=(k == 0), stop=(k == 7))
                                    return r
                                P.op('pe', zf, reads=[wk, 'yaT', 'ybT'], writes=[('ps', pb)])
                                if first:
                                    P.op('dve', lambda e, pb=pb: e.tensor_tensor(out=tE[:], in0=ps[pb][:, :], in1=sga[:], op=ALU.mult),
                                         reads=[('ps', pb), 'sga'], writes=['tE'])
                                else:
                                    P.op('dve', lambda e, pb=pb: e.tensor_tensor(out=sgb[:], in0=ps[pb][:, :], in1=sgb[:], op=ALU.mult),
                                         reads=[('ps', pb), 'sgb'], writes=['sgb'])
                            P.op('pool', lambda e, dc=dc: e.tensor_tensor(out=mgT[:, dc, :], in0=tE[:], in1=sgb[:], op=ALU.add),
                                 reads=['tE', 'sgb'], writes=['mgT'])
                        if tb == 0:
                            tap('mgT', mgT[:], 'mgT')
                        for dc in range(16):
                            w, wk = load_chunk(wout_l[dc])
                            pb = nps()

                            def mf(e, pb=pb, w=w):
                                for i in range(4):
                                    for k in range(16):
                                        r = e.matmul(ps[pb][:, i * 128:(i + 1) * 128], lhsT=mgT[:, k, i * 128:(i + 1) * 128], rhs=w[:, k, :],
                                                     start=(k == 0), stop=(k == 15))
                                return r
                            P.op('pe', mf, reads=[wk, 'mgT'], writes=[('ps', pb)])
                            dsl = slice(dc * 128, (dc + 1) * 128)
                            P.op('dve', lambda e, pb=pb, dsl=dsl: e.tensor_tensor(out=tE[:].rearrange("p (a b) -> p a b", a=4),
                                                                                 in0=ps[pb][:, :].rearrange("p (a b) -> p a b", a=4),
                                                                                 in1=g1bc[:, dsl].rearrange("p (a b) -> p a b", a=1).to_broadcast([128, 4, 128]),
                                                                                 op=ALU.mult),
                                 reads=[('ps', pb), 'g1bc'], writes=['tE'])
                            P.op('pool', lambda e, dsl=dsl: e.tensor_tensor(out=xb[:, :, dsl], in0=xb[:, :, dsl], in1=tE[:].rearrange("p (a b) -> p a b", a=4), op=ALU.add),
                                 reads=['tE', 'xb'], writes=['xb'])
                        P.dma('sp', lambda e: e.dma_start(out=x1s[t0_:t0_ + 512, :].rearrange("(a p) d -> p a d", p=128), in_=xb[:]), reads=['xb'], writes=['x1s'])
                        if tb == 0:
                            tap('x1', xb[:], 'xb')
                        junk = sb(sE, 'junk', [128, D], BF16)
                        h2f = sb(sE, 'h2f', [128, D])
                        h2b = [sb(sE, 'h2b%d' % i, [128, D], BF16) for i in range(2)]
                        h2T = sb(sE, 'h2T', [128, 16, 128])
                        ss1 = sb(sE, 'ss1', [128, 1])
                        lg = sb(sE, 'lg', [128, 32])
                        mx8 = sb(sE, 'mx8', [128, 8])
                        msk = sb(sE, 'msk', [128, 32])
                        ex = sb(sE, 'ex', [128, 32])
                        den = sb(sE, 'den', [128, 1])
                        nmx = sb(sE, 'nmx', [128, 1])
                        vv = sb(sE, 'vv', [128, 32])
                        v8 = sb(sE, 'v8', [128, 8])
                        sel = sb(sE, 'sel', [128, 32])
                        for i in range(4):
                            it = tb * 4 + i
                            xt1 = xb[:, i, :]
                            P.op('act', lambda e, xt1=xt1: e.activation(out=junk[:], in_=xt1, func=AF.Square, accum_out=ss1[:]), reads=['xb'], writes=['junk', 'ss1'])
                            P.op('dve', lambda e: e.tensor_scalar(out=ss1[:], in0=ss1[:], scalar1=1.0 / D, scalar2=1e-5, op0=ALU.mult, op1=ALU.add), reads=['ss1'], writes=['ss1'])
                            rsqrt(lambda: ss1[:], 'ss1')
                            P.op('dve', lambda e, xt1=xt1: e.scalar_tensor_tensor(out=h2f[:], in0=xt1, scalar=ss1[:, 0:1], in1=A2bc[:], op0=ALU.mult, op1=ALU.mult),
                                 reads=['xb', 'ss1', 'A2bc'], writes=['h2f'])
                            P.op('pool', lambda e: e.tensor_tensor(out=h2f[:], in0=h2f[:], in1=sh2bc[:], op=ALU.add), reads=['h2f', 'sh2bc'], writes=['h2f'])
                            hb, hbk = h2b[it % 2], ('h2b', it % 2)
                            P.op('act', lambda e, hb=hb: e.copy(out=hb[:], in_=h2f[:]), reads=['h2f'], writes=[hbk])
                            if it == 0:
                                tap('h2', h2f[:], 'h2f')
                            for q4 in range(4):
                                pb = nps()

                                def htr(e, pb=pb, q4=q4):
                                    for c4 in range(4):
                                        k = q4 * 4 + c4
                                        r = e.transpose(ps[pb][:, c4 * 128:(c4 + 1) * 128], h2f[:, k * 128:(k + 1) * 128], ident)
                                    return r
                                P.op('pe', htr, reads=['h2f', 'cst'], writes=[('ps', pb)])
                                P.op('act', lambda e, pb=pb, q4=q4: e.copy(out=h2T[:, q4 * 4:(q4 + 1) * 4, :].rearrange("p a b -> p (a b)"), in_=ps[pb][:, :]),
                                     reads=[('ps', pb)], writes=[('h2T', q4)])
                            pb = nps()

                            def lgf(e, pb=pb):
                                for k in range(16):
                                    r = e.matmul(ps[pb][:, 0:32], lhsT=h2T[:, k, :], rhs=rwt[:, k, :], start=(k == 0), stop=(k == 15))
                                return r
                            P.op('pe', lgf, reads=[('h2T', q) for q in range(4)] + ['rwt'], writes=[('ps', pb)])
                            P.op('dve', lambda e, pb=pb: e.tensor_tensor(out=lg[:], in0=ps[pb][:, 0:32], in1=rbbc[:], op=ALU.add), reads=[('ps', pb), 'rbbc'], writes=['lg'])
                            if it == 0:
                                tap('lg', lg[:], 'lg')
                            P.op('dve', lambda e: e.max(out=mx8[:], in_=lg[:]), reads=['lg'], writes=['mx8'])
                            P.op('dve', lambda e: e.tensor_scalar(out=msk[:], in0=lg[:], scalar1=mx8[:, 3:4], scalar2=None, op0=ALU.is_ge), reads=['lg', 'mx8'], writes=['msk'])
                            P.op('dve', lambda e: e.tensor_scalar(out=nmx[:], in0=mx8[:, 0:1], scalar1=-1.0, scalar2=None, op0=ALU.mult), reads=['mx8'], writes=['nmx'])
                            P.op('act', lambda e: e.activation(out=ex[:], in_=lg[:], func=AF.Exp, bias=nmx[:, 0:1]), reads=['lg', 'nmx'], writes=['ex'])
                            P.op('dve', lambda e: e.tensor_tensor(out=ex[:], in0=ex[:], in1=msk[:], op=ALU.mult), reads=['ex', 'msk'], writes=['ex'])
                            P.op('dve', lambda e: e.reduce_sum(out=den[:], in_=ex[:], axis=AX.X), reads=['ex'], writes=['den'])
                            P.op('dve', lambda e: e.reciprocal(out=den[:], in_=den[:]), reads=['den'], writes=['den'])
                            P.op('dve', lambda e: e.tensor_scalar(out=ex[:], in0=ex[:], scalar1=den[:, 0:1], scalar2=None, op0=ALU.mult), reads=['ex', 'den'], writes=['ex'])
                            P.op('dve', lambda e, it=it: e.tensor_copy(out=maskb_all[:, it, :], in_=msk[:]), reads=['msk'], writes=[('maskb', it)])
                            pb = nps()

                            def pf(e, pb=pb, it=it):
                                for jt in range(it):
                                    e.matmul(ps[pb][:, 0:32], lhsT=ones_bf[:], rhs=maskb_all[:, jt, :], start=(jt == 0), stop=False)
                                return e.matmul(ps[pb][:, 0:32], lhsT=triS_bf[:], rhs=maskb_all[:, it, :], start=(it == 0), stop=True)
                            P.op('pe', pf, reads=[('maskb', jt) for jt in range(it + 1)] + ['ones_bf', 'triS_bf'], writes=[('ps', pb)])
                            P.op('dve', lambda e, pb=pb: e.tensor_tensor(out=vv[:], in0=ps[pb][:, 0:32], in1=ecap1, op=ALU.add), reads=[('ps', pb), 'cst'], writes=['vv'])
                            P.op('dve', lambda e: e.tensor_tensor(out=vv[:], in0=vv[:], in1=msk[:], op=ALU.mult), reads=['vv', 'msk'], writes=['vv'])
                            P.op('dve', lambda e: e.max(out=v8[:], in_=vv[:]), reads=['vv'], writes=['v8'])
                            for k4 in range(4):
                                P.op('dve', lambda e, k4=k4: e.tensor_scalar(out=sel[:], in0=vv[:], scalar1=v8[:, k4:k4 + 1], scalar2=None, op0=ALU.is_equal),
                                     reads=['vv', 'v8'], writes=['sel'])
                                P.op('dve', lambda e: e.tensor_tensor(out=sel[:], in0=sel[:], in1=ex[:], op=ALU.mult), reads=['sel', 'ex'], writes=['sel'])
                                P.op('dve', lambda e, k4=k4, it=it: e.reduce_sum(out=g4_all[:, it, k4:k4 + 1], in_=sel[:], axis=AX.X), reads=['sel'], writes=['g4_all'])
                            P.op('dve', lambda e: e.tensor_scalar(out=v8[:, 0:4], in0=v8[:, 0:4], scalar1=-1.0, scalar2=None, op0=ALU.add), reads=['v8'], writes=['v8'])
                            P.op('dve', lambda e, it=it: e.tensor_copy(out=idx4_all[:, it, :], in_=v8[:, 0:4]), reads=['v8'], writes=['idx4_all'])
                            for k4 in range(4):
                                P.dma('pool', lambda e, hb=hb, it=it, k4=k4: e.indirect_dma_start(
                                    out=Xg, out_offset=bass.IndirectOffsetOnAxis(ap=idx4_all[:, it, k4:k4 + 1].bitcast(U32), axis=0),
                                    in_=hb[:], in_offset=None), reads=[hbk, 'idx4_all'], writes=['Xg'])
                        if tb == 0:
                            tap('idx4', idx4_all[:, 0:4, :], 'idx4_all')
                            tap('g4', g4_all[:, 0:4, :], 'g4_all')
                        P.barrier()
                        P.flush()
            P.barrier()
            P.flush()

        if stages >= 5:
            with ExitStack() as sG:
                bgu = sb(sG, 'bgu', [128, NE * 32])
                bdr = sb(sG, 'bdr', [1, D])
                bdrb = sb(sG, 'bdrb', [1, D], BF16)
                ones1 = sb(sG, 'ones1', [1, 128], BF16)
                identb = sb(sG, 'identb', [128, 128], BF16)
                xg = sb(sG, 'xg', [128, NA, D], BF16)
                XeT = sb(sG, 'XeT', [128, 16, SUB], BF16)
                actT = sb(sG, 'actT', [128, 16, SUB], BF16)
                glu = sb(sG, 'glu', [128, 4, SUB])
                sgl = sb(sG, 'sgl', [128, 4, SUB])
                lin = sb(sG, 'lin', [128, SUB])
                ysb = [sb(sG, 'ysb%d' % i, [128, 512], BF16) for i in range(2)]
                wst = [sb(sG, 'gwst%d' % i, [128, 16, 512]) for i in range(2)]
                wbf = [sb(sG, 'gwbf%d' % i, [128, 16, 512], BF16) for i in range(2)]
                P.dma('sp', lambda e: e.dma_start(out=bgu[:], in_=bgu_col), writes=['bgu'])
                P.op('pool', lambda e: e.memset(ones1[:], 1.0), writes=['ones1'])
                P.op('dve', lambda e: e.tensor_copy(out=identb[:], in_=ident), reads=['cst'], writes=['identb'])
                sln = [0]
                ysn = [0]

                def load_slab(src):
                    n = sln[0]
                    sln[0] += 1
                    b = n % 2
                    P.dma('sp', lambda e: e.dma_start(out=wst[b][:], in_=src), writes=[('gwst', b)])
                    P.op('dve', lambda e: e.tensor_copy(out=wbf[b][:, 0:6, :], in_=wst[b][:, 0:6, :]), reads=[('gwst', b)], writes=[('gwbf', b, 0)])
                    P.op('act', lambda e: e.copy(out=wbf[b][:, 6:11, :], in_=wst[b][:, 6:11, :]), reads=[('gwst', b)], writes=[('gwbf', b, 1)])
                    P.op('pool', lambda e: e.tensor_copy(out=wbf[b][:, 11:16, :], in_=wst[b][:, 11:16, :]), reads=[('gwst', b)], writes=[('gwbf', b, 2)])
                    return wbf[b], [('gwbf', b, 0), ('gwbf', b, 1), ('gwbf', b, 2)]

                for pas in range(NE * (CAP // SUB)):
                    ex_ = pas // (CAP // SUB)
                    row0 = ex_ * CAP + (pas % (CAP // SUB)) * SUB
                    P.dma('sp', lambda e, row0=row0: e.dma_start(out=xg[:], in_=Xg[row0:row0 + SUB, :].rearrange("(a p) d -> p a d", p=128)),
                          reads=['Xg'], writes=['xg'])
                    P.dma('sp', lambda e, ex_=ex_: e.dma_start(out=bdr[:], in_=bdn_row[0:1, ex_ * D:(ex_ + 1) * D]), writes=['bdr'])
                    P.op('dve', lambda e: e.tensor_copy(out=bdrb[:], in_=bdr[:]), reads=['bdr'], writes=['bdrb'])
                    for k in range(16):
                        pb = nps()

                        def xtr(e, pb=pb, k=k):
                            pv = ps[pb][:, 0:SUB // 2].bitcast(BF16)
                            for a in range(NA):
                                r = e.transpose(pv[:, a * 128:(a + 1) * 128], xg[:, a, k * 128:(k + 1) * 128], identb[:])
                            return r
                        P.op('pe', xtr, reads=['xg', 'identb'], writes=[('ps', pb)])
                        cp_eng = 'act' if k % 2 == 0 else 'dve'
                        if cp_eng == 'act':
                            P.op('act', lambda e, pb=pb, k=k: e.copy(out=XeT[:, k, :], in_=ps[pb][:, 0:SUB // 2].bitcast(BF16)), reads=[('ps', pb)], writes=['XeT'])
                        else:
                            P.op('dve', lambda e, pb=pb, k=k: e.tensor_copy(out=XeT[:, k, :], in_=ps[pb][:, 0:SUB // 2].bitcast(BF16)), reads=[('ps', pb)], writes=['XeT'])
                    for gs in range(4):
                        for half in range(2):
                            w, wks = load_slab(wgu_l[ex_, half * 4 + gs])
                            for c4 in range(4):
                                pb = nps()

                                def guf(e, pb=pb, w=w, c4=c4):
                                    for k in range(16):
                                        r = e.matmul(ps[pb][:, 0:SUB], lhsT=w[:, k, c4 * 128:(c4 + 1) * 128], rhs=XeT[:, k, :], start=(k == 0), stop=(k == 15))
                                    return r
                                P.op('pe', guf, reads=wks + ['XeT'], writes=[('ps', pb)])
                                bcol = bgu[:, ex_ * 32 + half * 16 + gs * 4 + c4: ex_ * 32 + half * 16 + gs * 4 + c4 + 1]
                                if half == 0:
                                    P.op('dve', lambda e, pb=pb, c4=c4, bcol=bcol: e.tensor_scalar(out=glu[:, c4, :], in0=ps[pb][:, 0:SUB], scalar1=bcol, scalar2=7.0,
                                                                                                   op0=ALU.add, op1=ALU.min), reads=[('ps', pb), 'bgu'], writes=[('glu', c4)])
                                    P.op('act', lambda e, c4=c4: e.activation(out=sgl[:, c4, :], in_=glu[:, c4, :], func=AF.Sigmoid, scale=1.702), reads=[('glu', c4)], writes=[('sgl', c4)])
                                    P.op('pool', lambda e, c4=c4: e.tensor_tensor(out=glu[:, c4, :], in0=glu[:, c4, :], in1=sgl[:, c4, :], op=ALU.mult),
                                         reads=[('glu', c4), ('sgl', c4)], writes=[('glu', c4)])
                                else:
                                    P.op('dve', lambda e, pb=pb, bcol=bcol: e.tensor_scalar(out=lin[:], in0=ps[pb][:, 0:SUB], scalar1=bcol, scalar2=7.0,
                                                                                            op0=ALU.add, op1=ALU.min), reads=[('ps', pb), 'bgu'], writes=['lin'])
                                    P.op('dve', lambda e: e.tensor_scalar(out=lin[:], in0=lin[:], scalar1=-7.0, scalar2=1.0, op0=ALU.max, op1=ALU.add), reads=['lin'], writes=['lin'])
                                    P.op('dve', lambda e, c4=c4, gs=gs: e.tensor_tensor(out=actT[:, gs * 4 + c4, :], in0=lin[:], in1=glu[:, c4, :], op=ALU.mult),
                                         reads=['lin', ('glu', c4)], writes=['actT'])
                    for db in range(4):
                        w, wks = load_slab(wdn_l[ex_, db])
                        for a in range(NA):
                            pb = nps()

                            def dnf(e, pb=pb, w=w, a=a, db=db):
                                for k in range(16):
                                    e.matmul(ps[pb][:, :], lhsT=actT[:, k, a * 128:(a + 1) * 128], rhs=w[:, k, :], start=(k == 0), stop=False)
                                return e.matmul(ps[pb][:, :], lhsT=ones1[:], rhs=bdrb[0:1, db * 512:(db + 1) * 512], start=False, stop=True)
                            P.op('pe', dnf, reads=wks + ['actT', 'ones1', 'bdrb'], writes=[('ps', pb)])
                            yb_, ybk = ysb[ysn[0] % 2], ('ysb', ysn[0] % 2)
                            ysn[0] += 1
                            P.op('act', lambda e, pb=pb, yb_=yb_: e.copy(out=yb_[:], in_=ps[pb][:, :]), reads=[('ps', pb)], writes=[ybk])
                            r0 = row0 + a * 128
                            P.dma('sp', lambda e, yb_=yb_, r0=r0, db=db: e.dma_start(out=Ys[r0:r0 + 128, db * 512:(db + 1) * 512], in_=yb_[:]), reads=[ybk], writes=['Ys'])
                    if pas % 4 == 3:
                        P.barrier()
                        P.flush()
                P.barrier()
                P.flush()

        if debug and 'idx4full' in debug:
            P.dma('sp', lambda e: e.dma_start(out=dbg['idx4full'], in_=idx4_all[:]), reads=['idx4_all'])
            P.dma('sp', lambda e: e.dma_start(out=dbg['g4full'], in_=g4_all[:]), reads=['g4_all'])
            P.barrier()
            P.flush()
        if debug and 'Xg0' in debug:
            P.dma('sp', lambda e: e.dma_start(out=dbg['Xg0'], in_=Xg[0:128, :]), reads=['Xg'])
            P.dma('sp', lambda e: e.dma_start(out=dbg['Ys0'], in_=Ys[0:128, :]), reads=['Ys'])
            P.dma('sp', lambda e: e.dma_start(out=dbg['x1s'], in_=x1s[0:128, :]), reads=['x1s'])
            P.barrier()
            P.flush()
        if stages >= 6:
            with ExitStack() as sH:
                g2bc = sb(sH, 'g2bc', [128, D])
                fnbc = sb(sH, 'fnbc', [128, D])
                x1t = [sb(sH, 'x1t%d' % i, [128, D]) for i in range(2)]
                yg = [sb(sH, 'yg%d' % i, [128, D], BF16) for i in range(4)]
                acc = sb(sH, 'acc', [128, D])
                junk = sb(sH, 'junkH', [128, D], BF16)
                ssH = sb(sH, 'ssH', [128, 1])
                P.dma('sp', lambda e: e.dma_start(out=g2bc[:], in_=modrow[0:1, 3 * D:4 * D].to_broadcast([128, D])), reads=['modrow'], writes=['g2bc'])
                P.dma('sp', lambda e: e.dma_start(out=fnbc[:], in_=rows_d[0:1, D:2 * D].to_broadcast([128, D])), writes=['fnbc'])
                for it in range(NT):
                    xt_, xk = x1t[it % 2], ('x1t', it % 2)
                    P.dma('sp', lambda e, xt_=xt_, it=it: e.dma_start(out=xt_[:], in_=x1s[it * 128:(it + 1) * 128, :]), reads=['x1s'], writes=[xk])
                    for k4 in range(4):
                        P.dma('pool', lambda e, it=it, k4=k4: e.indirect_dma_start(
                            out=yg[k4][:], out_offset=None, in_=Ys,
                            in_offset=bass.IndirectOffsetOnAxis(ap=idx4_all[:, it, k4:k4 + 1].bitcast(U32), axis=0)),
                            reads=['Ys', 'idx4_all'], writes=[('yg', k4)])
                    P.op('dve', lambda e, it=it: e.tensor_scalar(out=acc[:], in0=yg[0][:], scalar1=g4_all[:, it, 0:1], scalar2=None, op0=ALU.mult),
                         reads=[('yg', 0), 'g4_all'], writes=['acc'])
                    for k4 in range(1, 4):
                        P.op('dve', lambda e, it=it, k4=k4: e.scalar_tensor_tensor(out=acc[:], in0=yg[k4][:], scalar=g4_all[:, it, k4:k4 + 1], in1=acc[:],
                                                                                   op0=ALU.mult, op1=ALU.add), reads=[('yg', k4), 'g4_all', 'acc'], writes=['acc'])
                    P.op('pool', lambda e: e.tensor_tensor(out=acc[:], in0=acc[:], in1=g2bc[:], op=ALU.mult), reads=['acc', 'g2bc'], writes=['acc'])
                    P.op('dve', lambda e, xt_=xt_: e.tensor_tensor(out=xt_[:], in0=xt_[:], in1=acc[:], op=ALU.add), reads=[xk, 'acc'], writes=[xk])
                    P.op('act', lambda e, xt_=xt_: e.activation(out=junk[:], in_=xt_[:], func=AF.Square, accum_out=ssH[:]), reads=[xk], writes=['junkH', 'ssH'])
                    P.op('dve', lambda e: e.tensor_scalar(out=ssH[:], in0=ssH[:], scalar1=1.0 / D, scalar2=1e-5, op0=ALU.mult, op1=ALU.add), reads=['ssH'], writes=['ssH'])
                    rsqrt(lambda: ssH[:], 'ssH')
                    P.op('dve', lambda e, xt_=xt_: e.scalar_tensor_tensor(out=xt_[:], in0=xt_[:], scalar=ssH[:, 0:1], in1=fnbc[:], op0=ALU.mult, op1=ALU.mult),
                         reads=[xk, 'ssH', 'fnbc'], writes=[xk])
                    P.dma('sp', lambda e, xt_=xt_, it=it: e.dma_start(out=out[it * 128:(it + 1) * 128, :], in_=xt_[:]), reads=[xk], writes=['out'])
                P.barrier()
                P.flush()

        P.barrier()
        P.flush()
    return nc


NCH = 91
NBLK_DBG = [4]
OPLIMIT = [10 ** 9]
CI_WA, CI_GD0, CI_GD1 = 24, 25, 26
CI_HQ, CI_HF, CI_HI, CI_HO = 27, 35, 43, 51
CI_GA, CI_GB = 59, 75
COLOFF = {'mu': 0, 'w0': 27, 'a0': 35, 'k_k': 43, 'k_a': 51, 'r_k': 59, 'gn_g': 67, 'gn_b': 75, 'lb0': 83, 'lb1': 91, 'hgn': 99}
NCOL = 100
NCST = 1312


def _f(a):
    return np.ascontiguousarray(np.asarray(a, dtype=np.float32))


def _chunks(w, starts, width=128):
    outl = []
    for c0 in starts:
        blk = np.zeros((2048, 128), np.float32)
        n = min(width, w.shape[1] - c0)
        blk[:, :n] = w[:, c0:c0 + n]
        outl.append(blk.reshape(16, 128, 128).transpose(1, 0, 2))
    return np.ascontiguousarray(np.stack(outl, 0))


def _colv(v, n):
    buf = np.zeros(n * 128, np.float32)
    buf[:v.size] = v.reshape(-1)
    return buf.reshape(n, 128).T


def prep_shared(inputs, experts=True):
    sh = {}
    aw = _f(inputs['ada_w'])[0]
    sh['ada_w'] = np.ascontiguousarray(aw.reshape(16, 128, 24, 512).transpose(2, 1, 0, 3))
    ab = _f(inputs['ada_b'])[0]
    sh['adab_col'] = np.ascontiguousarray(ab[:4096].reshape(32, 128).T)
    sh['adab_row'] = np.ascontiguousarray(ab[4096:].reshape(1, 8192))
    sh['n1g_col'] = np.ascontiguousarray(_f(inputs['norm1_g'])[0].reshape(16, 128).T)
    w_in = _f(inputs['w_in'])[0]
    starts = [i * 128 for i in range(27)] + [3360 + i * 128 for i in range(32)] + [7456 + i * 128 for i in range(32)]
    wl = _chunks(w_in, starts)
    wl[26, :, :, 32:] = 0.0
    sh['w_in_l'] = wl
    cols = np.zeros((128, NCOL), np.float32)
    cols[:, 0:27] = _colv(_f(inputs['rwkv_mu'])[0], 27)
    for nm, key in (('w0', 'rwkv_w0'), ('a0', 'rwkv_a0'), ('k_k', 'rwkv_k_k'), ('k_a', 'rwkv_k_a'), ('r_k', 'rwkv_r_k'),
                    ('gn_g', 'rwkv_gn_g'), ('gn_b', 'rwkv_gn_b')):
        cols[:, COLOFF[nm]:COLOFF[nm] + 8] = _colv(_f(inputs[key])[0], 8)
    lbl = _f(inputs['hgrn_lb_logits'])
    cols[:, 83:91] = _colv(lbl[0], 8)
    cols[:, 91:99] = _colv(lbl[1], 8)
    cols[:, 99:100] = _f(inputs['hgrn_gn_g'])[0].reshape(128, 1)
    sh['cols'] = cols
    p = np.arange(128)[:, None]
    fcol = np.arange(128)[None, :]
    sU = (p < fcol).astype(np.float32)
    iU = (p <= fcol).astype(np.float32)
    sL = (p > fcol).astype(np.float32)
    cst = np.zeros((128, NCST), np.float32)
    cst[:, 0:512] = np.concatenate([sU, iU, sU, iU], 1)
    cst[:, 512:640] = np.eye(128, dtype=np.float32)
    cst[:, 640:768] = sL
    cst[:, 768:896] = iU
    cst[:, 896:1024] = ((p // 64) == (fcol // 64)).astype(np.float32)
    cst[:, 1152:1184] = (np.arange(32) * CAP + 1)[None, :].astype(np.float32)
    cst[:, 1184:1312] = sU
    sh['cst'] = cst
    z64 = np.zeros((64, 1024), np.float32)
    sh['w2p'] = np.ascontiguousarray(np.concatenate([_f(inputs['rwkv_w2'])[0], z64], 0))
    sh['a2p'] = np.ascontiguousarray(np.concatenate([z64, _f(inputs['rwkv_a2'])[0]], 0))
    g2 = _f(inputs['rwkv_g2'])[0]
    sh['g2a'] = np.ascontiguousarray(g2[:128])
    sh['g2b'] = np.ascontiguousarray(np.concatenate([g2[128:160], np.zeros((96, 1024), np.float32)], 0))
    for nm, key in (('pa_l', 'proj_a'), ('pb_l', 'proj_b')):
        w = _f(inputs[key])[0]
        sh[nm] = np.ascontiguousarray(w.reshape(8, 128, 16, 128).transpose(2, 1, 0, 3))
    wo = _f(inputs['w_out'])[0]
    sh['wout_l'] = np.ascontiguousarray(wo.reshape(16, 128, 16, 128).transpose(2, 1, 0, 3))
    sh['rows'] = np.ascontiguousarray(np.concatenate([_f(inputs['norm2_g'])[0], _f(inputs['final_norm_g']),
                                                      _f(inputs['router_b'])[0]]).reshape(1, -1))
    rw = _f(inputs['router_w'])[0]
    sh['rw_l'] = np.ascontiguousarray(rw.reshape(16, 128, 32).transpose(1, 0, 2))
    if experts:
        wgu = np.asarray(inputs['exp_w_gate_up'], dtype=np.float32)[0]
        sh['wgu_l'] = np.ascontiguousarray(wgu.reshape(NE, 16, 128, 8, 512).transpose(0, 3, 2, 1, 4))
        wdn = np.asarray(inputs['exp_w_down'], dtype=np.float32)[0]
        sh['wdn_l'] = np.ascontiguousarray(wdn.reshape(NE, 16, 128, 4, 512).transpose(0, 3, 2, 1, 4))
    bgu = _f(inputs['exp_b_gate_up'])[0]
    sh['bgu_col'] = np.ascontiguousarray(bgu.reshape(NE, 32, 128).transpose(2, 0, 1).reshape(128, NE * 32))
    sh['bdn_row'] = np.ascontiguousarray(_f(inputs['exp_b_down'])[0].reshape(1, NE * D))
    return sh


def prep_core(inputs, b):
    x = np.asarray(inputs['x'], dtype=np.float32)[b]
    c = np.asarray(inputs['c'], dtype=np.float32)[b]
    return {
        'x': np.ascontiguousarray(x),
        'xT': np.ascontiguousarray(x.T),
        'cT': np.ascontiguousarray(c.reshape(16, 128).T),
    }


def kernel(**inputs):
    nc = build()
    sh = prep_shared(inputs)
    in_maps = []
    for b in range(8):
        m = dict(sh)
        m.update(prep_core(inputs, b))
        in_maps.append(m)
    res = run_bass_kernel_spmd(nc, in_maps, core_ids=list(range(8)))
    return np.stack([np.asarray(r['out'], dtype=np.float32) for r in res.results], axis=0)
```

```python
from contextlib import ExitStack
import numpy as np
import concourse.bass as bass
import concourse.mybir as mybir
from concourse.bass_utils import run_bass_kernel_spmd

F32 = mybir.dt.float32
BF16 = mybir.dt.bfloat16
I32 = mybir.dt.int32
U32 = mybir.dt.uint32
AF = mybir.ActivationFunctionType
ALU = mybir.AluOpType
AX = mybir.AxisListType

D = 2048
T = 2048
NT = 16
NB = 4
NE = 32
NPASS = 48
CAP = 2048
SUB = 512
NA = SUB // 128
RW = 1024
RWKV_COLS = 3360
HG0 = 3360
GT0 = 3360 + 4096
NDS = 32


class Prog:
    CE = ('pe', 'act', 'dve', 'pool')

    def __init__(self, nc, stack):
        self.nc = nc
        self.eng = {'pe': nc.tensor, 'act': nc.scalar, 'dve': nc.vector, 'pool': nc.gpsimd, 'sp': nc.sync}
        self.esem = {e: stack.enter_context(nc.semaphore('se_' + e)) for e in self.CE}
        self.dsem = [stack.enter_context(nc.semaphore('sd%d' % i)) for i in range(NDS)]
        self.duse = [0] * NDS
        self.dnext = 0
        self.seq = {e: 0 for e in self.CE}
        self.seen = {e: {} for e in self.eng}
        self.res = {}
        self.ops = {e: [] for e in self.eng}
        self.nops = 0

    def _need(self, e, ev, waits):
        if ev is None:
            return
        sem, val, src = ev
        if src == e and e == 'pe':
            return
        k = id(sem)
        if self.seen[e].get(k, 0) >= val:
            return
        self.seen[e][k] = val
        waits.append((sem, val))

    def _deps(self, e, reads, writes):
        waits = []
        for r in reads:
            st = self.res.get(r)
            if st:
                self._need(e, st['w'], waits)
        for w in writes:
            st = self.res.get(w)
            if st:
                self._need(e, st['w'], waits)
                for ev in st['r'].values():
                    self._need(e, ev, waits)
        return waits

    def _commit(self, e, ev, reads, writes):
        for r in reads:
            st = self.res.setdefault(r, {'w': None, 'r': {}})
            st['r'][(e, id(ev[0]))] = ev
        for w in writes:
            self.res[w] = {'w': ev, 'r': {}}

    def op(self, e, fn, reads=(), writes=()):
        if self.nops >= OPLIMIT[0]:
            return
        waits = self._deps(e, reads, writes)
        self.seq[e] += 1
        ev = (self.esem[e], self.seq[e], e)
        self.ops[e].append((waits, fn, (self.esem[e], 1)))
        self._commit(e, ev, reads, writes)
        self.nops += 1

    def dma(self, q, fn, reads=(), writes=()):
        if self.nops >= OPLIMIT[0]:
            return
        waits = self._deps(q, reads, writes)
        i = self.dnext
        self.dnext = (self.dnext + 1) % NDS
        sem = self.dsem[i]
        if self.duse[i] > 0:
            self._need(q, (sem, 16 * self.duse[i], 'dma'), waits)
        self.duse[i] += 1
        ev = (sem, 16 * self.duse[i], 'dma')
        self.ops[q].append((waits, fn, (sem, 16)))
        self._commit(q, ev, reads, writes)
        self.nops += 1

    def barrier(self):
        for e in self.eng:
            waits = []
            for c in self.CE:
                if self.seq[c] > 0:
                    self._need(e, (self.esem[c], self.seq[c], 'x'), waits)
            for i in range(NDS):
                if self.duse[i] > 0:
                    self._need(e, (self.dsem[i], 16 * self.duse[i], 'dma'), waits)
            if waits:
                self.ops[e].append((waits, None, None))
        self.res = {}

    def flush(self):
        nc = self.nc
        ops = self.ops
        self.ops = {e: [] for e in self.eng}
        with nc.Block() as block:
            def emit(name):
                def body(engine):
                    for waits, fn, inc in ops[name]:
                        for sem, val in waits:
                            engine.wait_ge(sem, val)
                        if fn is not None:
                            fn(engine).then_inc(inc[0], inc[1])
                return body
            block.tensor(emit('pe'))
            block.scalar(emit('act'))
            block.vector(emit('dve'))
            block.gpsimd(emit('pool'))
            block.sync(emit('sp'))


def build(debug=None, stages=99):
    nc = bass.Bass("TRN2", target_bir_lowering=False)
    dbg = {}
    with ExitStack() as top:
        P = Prog(nc, top)

        def din(name, shape, dt=F32):
            return nc.dram_tensor(name, list(shape), dt, kind="ExternalInput").ap()

        def dout(name, shape, dt=F32):
            return nc.dram_tensor(name, list(shape), dt, kind="ExternalOutput").ap()

        def dscratch(name, shape, dt=F32):
            return nc.dram_tensor(name, list(shape), dt, kind="Internal").ap()

        sbn = [0]

        def sb(st, name, shape, dt=F32):
            sbn[0] += 1
            return st.enter_context(nc.sbuf_tensor('sb%d_%s' % (sbn[0], name), list(shape), dt))

        xT = din('xT', [D, T])
        x_in = din('x', [T, D])
        cT = din('cT', [128, 16])
        ada_w = din('ada_w', [24, 128, 16, 512])
        adab_col = din('adab_col', [128, 32])
        adab_row = din('adab_row', [1, 8192])
        n1g_col = din('n1g_col', [128, 16])
        w_in_l = din('w_in_l', [NCH, 128, 16, 128])
        cols_d = din('cols', [128, NCOL])
        cst_d = din('cst', [128, NCST])
        w2p_d = din('w2p', [128, 1024])
        a2p_d = din('a2p', [128, 1024])
        g2a_d = din('g2a', [128, 1024])
        g2b_d = din('g2b', [128, 1024])
        pa_l = din('pa_l', [16, 128, 8, 128])
        pb_l = din('pb_l', [16, 128, 8, 128])
        wout_l = din('wout_l', [16, 128, 16, 128])
        rows_d = din('rows', [1, 2 * D + 32])
        rw_l = din('rw_l', [128, 16, 32])
        if stages >= 5:
            wgu_l = din('wgu_l', [NE, 8, 128, 16, 512])
            wdn_l = din('wdn_l', [NE, 4, 128, 16, 512])
        bgu_rows = din('bgu_rows', [NE * 128, 32])
        bdn_rows = din('bdn_rows', [NE, D])
        out = dout('out', [T, D])
        modrow = dscratch('modrow', [1, 8192])
        x1s = dscratch('x1s', [T, D])
        Xg = dscratch('Xg', [NE * CAP, D], BF16)
        Ys = dscratch('Ys', [NE * CAP, D], BF16)

        if debug:
            for nm, (shp, dt_) in debug.items():
                dbg[nm] = dout('dbg_' + nm, shp, dt_)

        def tap(name, ap, key):
            if debug and name in debug:
                P.dma('sp', lambda e: e.dma_start(out=dbg[name], in_=ap), reads=[key])

        ps = [top.enter_context(nc.psum_tensor('ps%d' % i, [128, 512], F32)) for i in range(8)]
        psn = [0]

        def nps():
            i = psn[0]
            psn[0] = (i + 1) % 8
            return i

        ones_bf = sb(top, 'ones_bf', [128, 128], BF16)
        P.op('pool', lambda e: e.memset(ones_bf[:], 1.0), writes=['ones_bf'])
        A1 = sb(top, 'A1', [128, 16])
        SH1 = sb(top, 'SH1', [128, 16])
        cst = sb(top, 'cst', [128, NCST])
        cols = sb(top, 'colsb', [128, NCOL])
        P.dma('sp', lambda e: e.dma_start(out=cst[:], in_=cst_d), writes=['cst'])
        P.dma('sp', lambda e: e.dma_start(out=cols[:], in_=cols_d), writes=['cols'])
        mask4 = cst[:, 0:512]
        ident = cst[:, 512:640]
        strictL = cst[:, 640:768]
        inclU = cst[:, 768:896]
        bd64 = cst[:, 896:1024]
        zeros = cst[:, 1024:1152]
        ecap1 = cst[:, 1152:1184]
        strictU = cst[:, 1184:1312]
        erow = cst[:, 1312:1344]
        iota_aq = cst[:, 1344:1348]
        iota_s8q = cst[:, 1348:1356]
        iota_q = cst[:, 1344:1345]
        idx4_all = sb(top, 'idx4_all', [128, NT, 4], I32)
        EP = sb(top, 'EP', [128, NPASS])
        RB = sb(top, 'RB', [128, NPASS])
        g4_all = sb(top, 'g4_all', [128, NT, 4])

        def rsqrt(apf, key):
            P.op('act', lambda e: e.activation(out=apf(), in_=apf(), func=AF.Sqrt), reads=[key], writes=[key])
            P.op('dve', lambda e: e.reciprocal(out=apf(), in_=apf()), reads=[key], writes=[key])

        def col(name, j=0, n=1):
            o = COLOFF[name] + j
            return cols[:, o:o + n]

        with ExitStack() as st:
            ct = sb(st, 'ct', [128, 16])
            cact = sb(st, 'cact', [128, 16], BF16)
            abc = sb(st, 'abc', [128, 32])
            n1g = sb(st, 'n1g', [128, 16])
            abr = sb(st, 'abr', [1, 8192])
            mrow = sb(st, 'mrow', [1, 8192])
            mcol = sb(st, 'mcol', [128, 32])
            wst = [sb(st, 'wst%d' % i, [128, 16, 512]) for i in range(2)]
            wbf = [sb(st, 'wbf%d' % i, [128, 16, 512], BF16) for i in range(2)]
            P.dma('sp', lambda e: e.dma_start(out=ct[:], in_=cT), writes=['ct'])
            P.dma('sp', lambda e: e.dma_start(out=abc[:], in_=adab_col), writes=['abc'])
            P.dma('sp', lambda e: e.dma_start(out=n1g[:], in_=n1g_col), writes=['n1g'])
            P.dma('sp', lambda e: e.dma_start(out=abr[:], in_=adab_row), writes=['abr'])
            P.op('act', lambda e: e.activation(out=cact[:], in_=ct[:], func=AF.Silu), reads=['ct'], writes=['cact'])
            for s in range(24):
                b = s % 2
                P.dma('sp', lambda e, s=s, b=b: e.dma_start(out=wst[b][:], in_=ada_w[s]), writes=[('wst', b)])
                P.op('dve', lambda e, b=b: e.tensor_copy(out=wbf[b][:, 0:8, :], in_=wst[b][:, 0:8, :]),
                     reads=[('wst', b)], writes=[('wbf', b, 0)])
                P.op('act', lambda e, b=b: e.copy(out=wbf[b][:, 8:16, :], in_=wst[b][:, 8:16, :]),
                     reads=[('wst', b)], writes=[('wbf', b, 1)])
                rd = [('wbf', b, 0), ('wbf', b, 1), 'cact']
                if s < 8:
                    for j in range(4):
                        cc = 4 * s + j

                        def mmf(e, b=b, j=j, cc=cc):
                            for k in range(16):
                                r = e.matmul(ps[0][:, cc:cc + 1], lhsT=wbf[b][:, k, j * 128:(j + 1) * 128],
                                             rhs=cact[:, k:k + 1], start=(k == 0), stop=(k == 15))
                            return r
                        P.op('pe', mmf, reads=rd, writes=[('ps', 0)])
                else:
                    pb = 1 + (s % 2)

                    def mmf(e, b=b, pb=pb):
                        for k in range(16):
                            r = e.matmul(ps[pb][0:1, :], lhsT=cact[:, k:k + 1], rhs=wbf[b][:, k, :],
                                         start=(k == 0), stop=(k == 15))
                        return r
                    P.op('pe', mmf, reads=rd, writes=[('ps', pb)])
                    c0 = (s - 8) * 512
                    P.op('dve', lambda e, pb=pb, c0=c0: e.tensor_tensor(
                        out=mrow[0:1, c0:c0 + 512], in0=ps[pb][0:1, :], in1=abr[0:1, c0:c0 + 512], op=ALU.add),
                        reads=[('ps', pb), 'abr'], writes=[('mrow', s)])
            P.op('dve', lambda e: e.tensor_tensor(out=mcol[:], in0=ps[0][:, 0:32], in1=abc[:], op=ALU.add),
                 reads=[('ps', 0), 'abc'], writes=['mcol'])
            P.op('dve', lambda e: e.scalar_tensor_tensor(out=A1[:], in0=mcol[:, 16:32], scalar=1.0, in1=n1g[:],
                                                         op0=ALU.add, op1=ALU.mult),
                 reads=['mcol', 'n1g'], writes=['A1'])
            P.op('dve', lambda e: e.tensor_copy(out=SH1[:], in_=mcol[:, 0:16]), reads=['mcol'], writes=['SH1'])
            n2r = sb(st, 'n2r', [1, D])
            P.dma('sp', lambda e: e.dma_start(out=n2r[:], in_=rows_d[0:1, 0:D]), writes=['n2r'])
            P.op('dve', lambda e: e.scalar_tensor_tensor(out=mrow[0:1, 2 * D:3 * D], in0=mrow[0:1, 2 * D:3 * D], scalar=1.0, in1=n2r[:],
                                                         op0=ALU.add, op1=ALU.mult),
                 reads=[('mrow', s) for s in range(16, 20)] + ['n2r'], writes=[('mrow', s) for s in range(16, 20)])
            P.dma('sp', lambda e: e.dma_start(out=modrow, in_=mrow[:]),
                  reads=[('mrow', s) for s in range(8, 24)], writes=['modrow'])
            P.barrier()
            P.flush()

        with ExitStack() as mx:
            rbbc = sb(mx, 'rbbc', [128, 32])
            rwt = sb(mx, 'rwt', [128, 16, 32])
            P.dma('sp', lambda e: e.dma_start(out=rbbc[:], in_=rows_d[0:1, 2 * D:2 * D + 32].to_broadcast([128, 32])),
                  writes=['rbbc'])
            P.dma('sp', lambda e: e.dma_start(out=rwt[:], in_=rw_l), writes=['rwt'])
            S_rw = sb(mx, 'S_rw', [128, 8, 128])
            S_hg = sb(mx, 'S_hg', [128, 8, 128])
            carry = sb(mx, 'carry', [128, 32])
            lbc = sb(mx, 'lbc', [128, 8])
            omlc = sb(mx, 'omlc', [128, 8])
            ommc = sb(mx, 'ommc', [128, 27])
            w2p = sb(mx, 'w2p', [128, 1024])
            a2p = sb(mx, 'a2p', [128, 1024])
            g2a = sb(mx, 'g2a', [128, 1024])
            g2b = sb(mx, 'g2b', [128, 1024])
            maskb_all = sb(mx, 'maskb_all', [128, NT, 32], BF16)
            triS_bf = sb(mx, 'triS_bf', [128, 128], BF16)
            P.op('pool', lambda e: e.memset(S_rw[:], 0.0), writes=['S_rw'])
            P.op('pool', lambda e: e.memset(S_hg[:], 0.0), writes=['S_hg'])
            P.op('pool', lambda e: e.memset(carry[:], 0.0), writes=['carry'])
            P.dma('sp', lambda e: e.dma_start(out=w2p[:], in_=w2p_d), writes=['w2p'])
            P.dma('sp', lambda e: e.dma_start(out=a2p[:], in_=a2p_d), writes=['a2p'])
            P.dma('sp', lambda e: e.dma_start(out=g2a[:], in_=g2a_d), writes=['g2a'])
            P.dma('sp', lambda e: e.dma_start(out=g2b[:], in_=g2b_d), writes=['g2b'])
            P.op('dve', lambda e: e.tensor_copy(out=triS_bf[:], in_=strictU), reads=['cst'], writes=['triS_bf'])
            P.op('dve', lambda e: e.tensor_tensor(out=lbc[:], in0=col('lb0', 0, 8), in1=col('lb1', 0, 8), op=ALU.subtract),
                 reads=['cols'], writes=['lbc'])
            P.op('act', lambda e: e.activation(out=lbc[:], in_=lbc[:], func=AF.Sigmoid), reads=['lbc'], writes=['lbc'])
            P.op('dve', lambda e: e.tensor_scalar(out=omlc[:], in0=lbc[:], scalar1=-1.0, scalar2=1.0, op0=ALU.mult, op1=ALU.add),
                 reads=['lbc'], writes=['omlc'])
            P.op('dve', lambda e: e.tensor_scalar(out=ommc[:], in0=col('mu', 0, 27), scalar1=-1.0, scalar2=1.0,
                                                  op0=ALU.mult, op1=ALU.add), reads=['cols'], writes=['ommc'])

            NSTG, NBFB = 2, 2
            stg = [sb(mx, 'stg%d' % i, [128, 16, 128]) for i in range(NSTG)]
            wcb = [sb(mx, 'wcb%d' % i, [128, 16, 128], BF16) for i in range(NBFB)]
            cln = [0]

            def load_chunk(src, kdim=16):
                n = cln[0]
                cln[0] += 1
                s_, b_ = n % NSTG, n % NBFB
                P.dma('sp', lambda e: e.dma_start(out=stg[s_][:, 0:kdim, :], in_=src), writes=[('stg', s_)])
                if n % 2 == 0:
                    P.op('act', lambda e: e.copy(out=wcb[b_][:, 0:kdim, :], in_=stg[s_][:, 0:kdim, :]),
                         reads=[('stg', s_)], writes=[('wcb', b_)])
                else:
                    P.op('pool', lambda e: e.tensor_copy(out=wcb[b_][:, 0:kdim, :], in_=stg[s_][:, 0:kdim, :]),
                         reads=[('stg', s_)], writes=[('wcb', b_)])
                return wcb[b_], ('wcb', b_)

            for tb in range(min(NB, NBLK_DBG[0]) if stages >= 1.5 else 0):
                t0_ = tb * 512
                with ExitStack() as bk:
                    hT = sb(bk, 'hT', [128, 16, 512], BF16)
                    yaT = sb(bk, 'yaT', [128, 8, 512], BF16)
                    ybT = sb(bk, 'ybT', [128, 8, 512], BF16)
                    praw = [sb(bk, 'praw%d' % i, [128, 513]) for i in range(2)]
                    shtmp = sb(bk, 'shtmp', [128, 512])
                    prn = [0]

                    with ExitStack() as sB:
                        xt = sb(sB, 'xt', [128, 16, 512])
                        sq = sb(sB, 'sq', [128, 16, 512], BF16)
                        rstd = sb(sB, 'rstd', [128, 1, 512])
                        P.dma('sp', lambda e: e.dma_start(
                            out=xt[:], in_=xT.rearrange("(k p) t -> p k t", p=128)[:, :, t0_:t0_ + 512]), writes=['xt'])
                        P.op('act', lambda e: e.activation(out=sq[:], in_=xt[:], func=AF.Square), reads=['xt'], writes=['sq'])
                        pb = nps()

                        def mmf(e, pb=pb):
                            for k in range(16):
                                r = e.matmul(ps[pb][:, :], lhsT=ones_bf[:], rhs=sq[:, k, :], start=(k == 0), stop=(k == 15))
                            return r
                        P.op('pe', mmf, reads=['sq', 'ones_bf'], writes=[('ps', pb)])
                        P.op('dve', lambda e, pb=pb: e.tensor_scalar(out=rstd[:, 0, :], in0=ps[pb][:, :], scalar1=1.0 / D, scalar2=1e-5,
                                                                     op0=ALU.mult, op1=ALU.add), reads=[('ps', pb)], writes=['rstd'])
                        rsqrt(lambda: rstd[:, 0, :], 'rstd')
                        P.op('dve', lambda e: e.tensor_tensor(out=xt[:], in0=xt[:], in1=rstd[:].to_broadcast([128, 16, 512]),
                                                              op=ALU.mult), reads=['xt', 'rstd'], writes=['xt'])
                        for k in range(16):
                            P.op('act', lambda e, k=k: e.activation(out=hT[:, k, :], in_=xt[:, k, :], func=AF.Identity,
                                                                    scale=A1[:, k:k + 1], bias=SH1[:, k:k + 1]),
                                 reads=['xt', 'A1', 'SH1'], writes=['hT'])
                        if tb == 0:
                            tap('hT', hT[:], 'hT')
                        P.barrier()
                        P.flush()

                    if stages < 2:
                        continue

                    def proj_fm(ci, M=128):
                        w, wk = load_chunk(w_in_l[ci])
                        pb = nps()

                        def f(e):
                            for k in range(16):
                                r = e.matmul(ps[pb][0:M, :], lhsT=w[:, k, 0:M], rhs=hT[:, k, :], start=(k == 0), stop=(k == 15))
                            return r
                        P.op('pe', f, reads=[wk, 'hT'], writes=[('ps', pb)])
                        return pb

                    def shift(pb, ci, out_ap, okey, M=128):
                        i = prn[0] % 2
                        prn[0] += 1
                        pr, pk = praw[i], ('praw', i)
                        P.op('act', lambda e: e.copy(out=pr[0:M, 1:513], in_=ps[pb][0:M, :]), reads=[('ps', pb)], writes=[pk])
                        P.op('dve', lambda e: e.tensor_copy(out=pr[0:M, 0:1], in_=carry[0:M, ci:ci + 1]), reads=['carry'], writes=[pk])
                        P.op('pool', lambda e: e.tensor_scalar(out=shtmp[0:M, :], in0=pr[0:M, 0:512], scalar1=col('mu', ci)[0:M, :],
                                                               scalar2=None, op0=ALU.mult), reads=[pk, 'cols'], writes=['shtmp'])
                        P.op('dve', lambda e: e.scalar_tensor_tensor(out=out_ap, in0=pr[0:M, 1:513], scalar=ommc[0:M, ci:ci + 1],
                                                                     in1=shtmp[0:M, :], op0=ALU.mult, op1=ALU.add),
                             reads=[pk, 'shtmp', 'ommc'], writes=[okey])
                        P.op('dve', lambda e: e.tensor_copy(out=carry[0:M, ci:ci + 1], in_=pr[0:M, 512:513]), reads=[pk], writes=['carry'])

                    with ExitStack() as sC:
                        was = sb(sC, 'was', [128, 512])
                        thw = sb(sC, 'thw', [128, 512])
                        sg0 = sb(sC, 'sg0', [128, 512])
                        sg1 = sb(sC, 'sg1', [128, 512])
                        P.op('pool', lambda e: e.memset(sg1[:], 0.0), writes=['sg1'])
                        pb = proj_fm(CI_WA)
                        shift(pb, 24, was[:], 'was')
                        P.op('act', lambda e: e.activation(out=thw[:], in_=was[:, :], func=AF.Tanh), reads=['was'], writes=['thw'])
                        pb = proj_fm(CI_GD0)
                        shift(pb, 25, sg0[:], 'sg0')
                        P.op('act', lambda e: e.activation(out=sg0[:], in_=sg0[:], func=AF.Sigmoid), reads=['sg0'], writes=['sg0'])
                        pb = proj_fm(CI_GD1, M=32)
                        shift(pb, 26, sg1[0:32, :], 'sg1', M=32)
                        P.op('act', lambda e: e.activation(out=sg1[0:32, :], in_=sg1[0:32, :], func=AF.Sigmoid), reads=['sg1'], writes=['sg1'])

                        names = ['r_t', 'k_t', 'v_t', 'a_t', 'lw', 'g_t', 'cum', 'c_t', 'E1', 'E2', 'kk', 'km', 'b_t', 'bon', 'Bt', 'Kt', 'tmpA', 'tmpB', 'Bt0', 'Bt1', 'Kt0', 'Kt1']
                        Tt = {n: sb(sC, n, [128, 4, 128]) for n in names}
                        KR = sb(sC, 'KR', [128, 4, 2, 128])
                        BKV = sb(sC, 'BKV', [128, 4, 3, 128])
                        eref = sb(sC, 'eref', [128, 4])
                        ecl = sb(sC, 'ecl', [128, 4])
                        Msc = [sb(sC, 'Msc%d' % u, [128, 512]) for u in range(8)]
                        Xb = [[sb(sC, 'X%d_%d' % (u, v), [128, 128]) for v in range(2)] for u in range(8)]
                        XTb = [[sb(sC, 'XT%d_%d' % (u, v), [128, 128]) for v in range(2)] for u in range(8)]
                        Tm = [sb(sC, 'Tm%d' % u, [128, 128]) for u in range(8)]
                        S0p = sb(sC, 'S0p', [128, 128])
                        nG = sb(sC, 'nG', [128, 128])
                        Ut = sb(sC, 'Ut', [128, 128])
                        ytm = sb(sC, 'ytm', [128, 4, 128])
                        ysq = Tt['tmpA']
                        st8 = sb(sC, 'st8', [128, 4, 8])

                        def F2(n):
                            return Tt[n][:].rearrange("p a b -> p (a b)")

                        for j in range(8):
                            jc = slice(j * 128, (j + 1) * 128)
                            pb = nps()
                            P.op('pe', lambda e, pb=pb, jc=jc: e.matmul(ps[pb][:, :], lhsT=a2p[:, jc], rhs=was[:, :], start=True, stop=True),
                                 reads=['a2p', 'was'], writes=[('ps', pb)])
                            P.op('act', lambda e, pb=pb, j=j: e.activation(out=F2('a_t'), in_=ps[pb][:, :], func=AF.Sigmoid, bias=col('a0', j)),
                                 reads=[('ps', pb), 'cols'], writes=['a_t'])
                            pb = nps()
                            P.op('pe', lambda e, pb=pb, jc=jc: e.matmul(ps[pb][:, :], lhsT=w2p[:, jc], rhs=thw[:, :], start=True, stop=True),
                                 reads=['w2p', 'thw'], writes=[('ps', pb)])
                            P.op('act', lambda e, pb=pb, j=j: e.activation(out=F2('lw'), in_=ps[pb][:, :], func=AF.Sigmoid, bias=col('w0', j)),
                                 reads=[('ps', pb), 'cols'], writes=['lw'])
                            P.op('pool', lambda e: e.tensor_scalar(out=F2('lw'), in0=F2('lw'), scalar1=-0.6065306597126334, scalar2=None, op0=ALU.mult),
                                 reads=['lw'], writes=['lw'])
                            pb = nps()

                            def gmm(e, pb=pb, jc=jc):
                                e.matmul(ps[pb][:, :], lhsT=g2a[:, jc], rhs=sg0[:, :], start=True, stop=False)
                                return e.matmul(ps[pb][:, :], lhsT=g2b[:, jc], rhs=sg1[:, :], start=False, stop=True)
                            P.op('pe', gmm, reads=['g2a', 'g2b', 'sg0', 'sg1'], writes=[('ps', pb)])
                            P.op('act', lambda e, pb=pb: e.copy(out=F2('g_t'), in_=ps[pb][:, :]), reads=[('ps', pb)], writes=['g_t'])
                            for nm, ci in (('r_t', j), ('k_t', 8 + j), ('v_t', 16 + j)):
                                pb = proj_fm(ci)
                                shift(pb, ci, F2(nm), nm)
                            if tb == 0 and j == 0:
                                tap('r0', F2('r_t'), 'r_t')
                                tap('a0', F2('a_t'), 'a_t')
                                tap('lw0', F2('lw'), 'lw')
                            for i in range(4):
                                P.op('dve', lambda e, i=i: e.tensor_tensor_scan(out=Tt['cum'][:, i, :], data0=Tt['lw'][:, i, :], data1=zeros,
                                                                               initial=0.0, op0=ALU.add, op1=ALU.add),
                                     reads=['lw', 'cst'], writes=['cum'])
                            for i in range(4):
                                P.op('dve', lambda e, i=i: e.tensor_scalar(out=Tt['c_t'][:, i, :], in0=Tt['cum'][:, i, :], scalar1=Tt['cum'][:, i, 63:64],
                                                                          scalar2=None, op0=ALU.subtract), reads=['cum'], writes=['c_t'])
                            P.op('act', lambda e: e.activation(out=F2('E1'), in_=F2('c_t'), func=AF.Exp, scale=-1.0), reads=['c_t'], writes=['E1'])
                            P.op('act', lambda e: e.activation(out=F2('E2'), in_=F2('c_t'), func=AF.Exp), reads=['c_t'], writes=['E2'])
                            P.op('pool', lambda e: e.tensor_tensor(out=F2('tmpA'), in0=F2('c_t'), in1=F2('lw'), op=ALU.subtract),
                                 reads=['c_t', 'lw'], writes=['tmpA'])
                            P.op('act', lambda e: e.activation(out=F2('tmpA'), in_=F2('tmpA'), func=AF.Exp), reads=['tmpA'], writes=['tmpA'])
                            P.op('act', lambda e: e.activation(out=eref[:], in_=Tt['cum'][:, :, 63], func=AF.Exp), reads=['cum'], writes=['eref'])
                            P.op('act', lambda e: e.activation(out=ecl[:], in_=Tt['c_t'][:, :, 127], func=AF.Exp), reads=['c_t'], writes=['ecl'])
                            P.op('dve', lambda e, j=j: e.tensor_scalar(out=F2('kk'), in0=F2('k_t'), scalar1=col('k_k', j), scalar2=None, op0=ALU.mult),
                                 reads=['k_t', 'cols'], writes=['kk'])
                            P.op('pool', lambda e: e.tensor_tensor(out=F2('tmpB'), in0=F2('kk'), in1=F2('kk'), op=ALU.mult), reads=['kk'], writes=['tmpB'])
                            pb = nps()
                            P.op('pe', lambda e, pb=pb: e.matmul(ps[pb][:, :], lhsT=bd64, rhs=F2('tmpB'), start=True, stop=True),
                                 reads=['cst', 'tmpB'], writes=[('ps', pb)])
                            P.op('dve', lambda e, pb=pb: e.tensor_scalar(out=F2('tmpB'), in0=ps[pb][:, :], scalar1=1e-24, scalar2=None, op0=ALU.max),
                                 reads=[('ps', pb)], writes=['tmpB'])
                            rsqrt(lambda: F2('tmpB'), 'tmpB')
                            P.op('dve', lambda e: e.tensor_tensor(out=F2('kk'), in0=F2('kk'), in1=F2('tmpB'), op=ALU.mult), reads=['kk', 'tmpB'], writes=['kk'])
                            P.op('dve', lambda e, j=j: e.tensor_scalar(out=F2('km'), in0=F2('a_t'), scalar1=-1.0, scalar2=col('k_a', j), op0=ALU.add, op1=ALU.mult),
                                 reads=['a_t', 'cols'], writes=['km'])
                            P.op('dve', lambda e: e.scalar_tensor_tensor(out=F2('km'), in0=F2('km'), scalar=1.0, in1=F2('k_t'), op0=ALU.add, op1=ALU.mult),
                                 reads=['km', 'k_t'], writes=['km'])
                            P.op('pool', lambda e: e.tensor_tensor(out=F2('b_t'), in0=F2('kk'), in1=F2('a_t'), op=ALU.mult), reads=['kk', 'a_t'], writes=['b_t'])
                            P.op('dve', lambda e, j=j: e.scalar_tensor_tensor(out=F2('tmpB'), in0=F2('r_t'), scalar=col('r_k', j), in1=F2('km'), op0=ALU.mult, op1=ALU.mult),
                                 reads=['r_t', 'km', 'cols'], writes=['tmpB'])
                            pb = nps()
                            P.op('pe', lambda e, pb=pb: e.matmul(ps[pb][:, :], lhsT=bd64, rhs=F2('tmpB'), start=True, stop=True),
                                 reads=['cst', 'tmpB'], writes=[('ps', pb)])
                            P.op('dve', lambda e, pb=pb: e.tensor_tensor(out=F2('bon'), in0=ps[pb][:, :], in1=F2('v_t'), op=ALU.mult),
                                 reads=[('ps', pb), 'v_t'], writes=['bon'])
                            P.op('dve', lambda e: e.tensor_tensor(out=F2('Bt'), in0=F2('b_t'), in1=F2('E1'), op=ALU.mult), reads=['b_t', 'E1'], writes=['Bt'])
                            P.op('pool', lambda e: e.tensor_tensor(out=F2('Kt'), in0=F2('km'), in1=F2('E1'), op=ALU.mult), reads=['km', 'E1'], writes=['Kt'])
                            for h2 in range(2):
                                hm = bd64[:, h2 * 64:h2 * 64 + 1]
                                P.op('dve', lambda e, h2=h2, hm=hm: e.tensor_scalar(out=F2('Bt%d' % h2), in0=F2('Bt'), scalar1=hm, scalar2=None, op0=ALU.mult),
                                     reads=['Bt', 'cst'], writes=['Bt%d' % h2])
                                P.op('pool', lambda e, h2=h2, hm=hm: e.tensor_scalar(out=F2('Kt%d' % h2), in0=F2('Kt'), scalar1=hm, scalar2=None, op0=ALU.mult),
                                     reads=['Kt', 'cst'], writes=['Kt%d' % h2])
                            P.op('dve', lambda e: e.tensor_tensor(out=KR[:, :, 0, :], in0=Tt['kk'][:], in1=Tt['tmpA'][:], op=ALU.mult), reads=['kk', 'tmpA'], writes=['KR'])
                            P.op('pool', lambda e: e.tensor_tensor(out=KR[:, :, 1, :], in0=Tt['r_t'][:], in1=Tt['E2'][:], op=ALU.mult), reads=['r_t', 'E2'], writes=['KR'])
                            for i in range(4):
                                pb = nps()

                                def trf(e, pb=pb, i=i):
                                    e.transpose(ps[pb][:, 0:128], Tt['Bt'][:, i, :], ident)
                                    e.transpose(ps[pb][:, 128:256], Tt['Kt'][:, i, :], ident)
                                    return e.transpose(ps[pb][:, 256:384], Tt['v_t'][:, i, :], ident)
                                P.op('pe', trf, reads=['Bt', 'Kt', 'v_t', 'cst'], writes=[('ps', pb)])
                                P.op('act', lambda e, pb=pb, i=i: e.copy(out=BKV[:, i, :, :].rearrange("p a b -> p (a b)"), in_=ps[pb][:, 0:384]),
                                     reads=[('ps', pb)], writes=[('BKV', i)])
                            for i in range(4):
                                for h2 in range(2):
                                    u = i * 2 + h2
                                    hs = slice(h2 * 64, h2 * 64 + 64)
                                    pb = nps()

                                    def scf(e, pb=pb, i=i, h2=h2):
                                        krr = KR[:, i, :, :].rearrange("p a b -> p (a b)")
                                        e.matmul(ps[pb][:, 0:256], lhsT=Tt['Bt%d' % h2][:, i, :], rhs=krr, start=True, stop=True)
                                        return e.matmul(ps[pb][:, 256:512], lhsT=Tt['Kt%d' % h2][:, i, :], rhs=krr, start=True, stop=True)
                                    P.op('pe', scf, reads=['Bt%d' % h2, 'Kt%d' % h2, 'KR'], writes=[('ps', pb)])
                                    P.op('dve', lambda e, pb=pb, u=u: e.tensor_tensor(out=Msc[u][:], in0=ps[pb][:, :], in1=mask4, op=ALU.mult),
                                         reads=[('ps', pb), 'cst'], writes=[('Msc', u)])
                                    pb = nps()
                                    P.op('pe', lambda e, pb=pb, i=i, h2=h2: e.matmul(ps[pb][:, 0:128], lhsT=KR[:, i, 0, :], rhs=Tt['Bt%d' % h2][:, i, :], start=True, stop=True),
                                         reads=['Bt%d' % h2, 'KR'], writes=[('ps', pb)])
                                    P.op('dve', lambda e, pb=pb, u=u: e.tensor_tensor(out=XTb[u][0][:], in0=ps[pb][:, 0:128], in1=strictL, op=ALU.mult),
                                         reads=[('ps', pb), 'cst'], writes=[('XT', u, 0)])
                                    P.op('pool', lambda e, u=u: e.tensor_copy(out=Xb[u][0][:], in_=Msc[u][:, 0:128]), reads=[('Msc', u)], writes=[('X', u, 0)])
                                    P.op('pool', lambda e, u=u: e.tensor_tensor(out=Tm[u][:], in0=ident, in1=Msc[u][:, 0:128], op=ALU.subtract),
                                         reads=[('Msc', u), 'cst'], writes=[('Tm', u)])
                            for lvl in range(6):
                                a_, b_ = lvl % 2, (lvl + 1) % 2
                                for u in range(8):
                                    pb = nps()

                                    def sqf(e, pb=pb, u=u, a_=a_, last=(lvl == 5)):
                                        r = e.matmul(ps[pb][:, 128:256], lhsT=Xb[u][a_][:], rhs=XTb[u][a_][:], start=True, stop=True)
                                        if not last:
                                            r = e.matmul(ps[pb][:, 0:128], lhsT=XTb[u][a_][:], rhs=Xb[u][a_][:], start=True, stop=True)
                                        return r
                                    P.op('pe', sqf, reads=[('X', u, a_), ('XT', u, a_)], writes=[('ps', pb)])
                                    P.op('dve', lambda e, pb=pb, u=u, b_=b_: e.tensor_copy(out=XTb[u][b_][:], in_=ps[pb][:, 128:256]),
                                         reads=[('ps', pb)], writes=[('XT', u, b_)])
                                    if lvl < 5:
                                        P.op('dve', lambda e, pb=pb, u=u, b_=b_: e.tensor_copy(out=Xb[u][b_][:], in_=ps[pb][:, 0:128]),
                                             reads=[('ps', pb)], writes=[('X', u, b_)])
                                for u in range(8):
                                    pb = nps()
                                    P.op('pe', lambda e, pb=pb, u=u, b_=b_: e.matmul(ps[pb][:, 0:128], lhsT=XTb[u][b_][:], rhs=Tm[u][:], start=True, stop=True),
                                         reads=[('XT', u, b_), ('Tm', u)], writes=[('ps', pb)])
                                    P.op('dve', lambda e, pb=pb, u=u: e.tensor_tensor(out=Tm[u][:], in0=ps[pb][:, 0:128], in1=Tm[u][:], op=ALU.add),
                                         reads=[('ps', pb), ('Tm', u)], writes=[('Tm', u)])
                            Sj = S_rw[:, j, :]
                            for i in range(4):
                                P.op('dve', lambda e, i=i, Sj=Sj: e.tensor_scalar(out=S0p[:], in0=Sj, scalar1=eref[:, i:i + 1], scalar2=None, op0=ALU.mult),
                                     reads=['S_rw', 'eref'], writes=['S0p'])
                                pb = nps()

                                def gf(e, pb=pb, i=i):
                                    e.matmul(ps[pb][:, 0:128], lhsT=KR[:, i, 0, :], rhs=S0p[:], start=True, stop=False)
                                    for h2 in range(2):
                                        r = e.matmul(ps[pb][:, h2 * 64:h2 * 64 + 64], lhsT=Msc[i * 2 + h2][:, 256:384],
                                                     rhs=BKV[:, i, 2, h2 * 64:h2 * 64 + 64], start=False, stop=(h2 == 1))
                                    return r
                                P.op('pe', gf, reads=['KR', 'S0p', ('Msc', 2 * i), ('Msc', 2 * i + 1), ('BKV', i)], writes=[('ps', pb)])
                                P.op('dve', lambda e, pb=pb: e.tensor_scalar(out=nG[:], in0=ps[pb][:, 0:128], scalar1=-1.0, scalar2=None, op0=ALU.mult),
                                     reads=[('ps', pb)], writes=['nG'])
                                pb = nps()

                                def uf(e, pb=pb, i=i):
                                    for h2 in range(2):
                                        r = e.matmul(ps[pb][:, h2 * 64:h2 * 64 + 64], lhsT=Tm[i * 2 + h2][:], rhs=nG[:, h2 * 64:h2 * 64 + 64], start=True, stop=True)
                                    return r
                                P.op('pe', uf, reads=[('Tm', 2 * i), ('Tm', 2 * i + 1), 'nG'], writes=[('ps', pb)])
                                P.op('act', lambda e, pb=pb: e.copy(out=Ut[:], in_=ps[pb][:, 0:128]), reads=[('ps', pb)], writes=['Ut'])
                                pb = nps()

                                def yf(e, pb=pb, i=i):
                                    e.matmul(ps[pb][:, 0:128], lhsT=KR[:, i, 1, :], rhs=S0p[:], start=True, stop=False)
                                    for h2 in range(2):
                                        cs = slice(h2 * 64, h2 * 64 + 64)
                                        e.matmul(ps[pb][:, cs], lhsT=Msc[i * 2 + h2][:, 128:256], rhs=Ut[:, cs], start=False, stop=False)
                                        r = e.matmul(ps[pb][:, cs], lhsT=Msc[i * 2 + h2][:, 384:512], rhs=BKV[:, i, 2, cs], start=False, stop=(h2 == 1))
                                    return r
                                P.op('pe', yf, reads=['KR', 'S0p', ('Msc', 2 * i), ('Msc', 2 * i + 1), 'Ut', ('BKV', i)], writes=[('ps', pb)])
                                P.op('act', lambda e, pb=pb, i=i: e.copy(out=ytm[:, i, :], in_=ps[pb][:, 0:128]), reads=[('ps', pb)], writes=[('ytm', i)])
                                pb = nps()

                                def sf(e, pb=pb, i=i):
                                    e.matmul(ps[pb][:, 0:128], lhsT=ident, rhs=S0p[:], start=True, stop=False)
                                    e.matmul(ps[pb][:, 0:128], lhsT=BKV[:, i, 0, :], rhs=Ut[:], start=False, stop=False)
                                    return e.matmul(ps[pb][:, 0:128], lhsT=BKV[:, i, 1, :], rhs=BKV[:, i, 2, :], start=False, stop=True)
                                P.op('pe', sf, reads=['cst', 'S0p', ('BKV', i), 'Ut'], writes=[('ps', pb)])
                                P.op('dve', lambda e, pb=pb, i=i, Sj=Sj: e.scalar_tensor_tensor(out=Sj, in0=ps[pb][:, 0:128], scalar=ecl[:, i:i + 1], in1=bd64,
                                                                                             op0=ALU.mult, op1=ALU.mult),
                                     reads=[('ps', pb), 'ecl', 'cst'], writes=['S_rw'])
                            if tb == 0 and j == 0:
                                tap('y0', ytm[:], ('ytm', 3))
                            yk = [('ytm', i) for i in range(4)]
                            yv = ytm[:].rearrange("p a (g n) -> p (a g) n", g=2)
                            P.op('pool', lambda e: e.tensor_tensor(out=ysq[:], in0=ytm[:], in1=ytm[:], op=ALU.mult), reads=yk, writes=['ysq'])
                            P.op('dve', lambda e: e.reduce_sum(out=st8[:, 0, :], in_=yv, axis=AX.X), reads=yk, writes=['st8'])
                            P.op('dve', lambda e: e.reduce_sum(out=st8[:, 1, :], in_=ysq[:].rearrange("p a (g n) -> p (a g) n", g=2), axis=AX.X),
                                 reads=['ysq'], writes=['st8'])
                            P.op('dve', lambda e: e.tensor_scalar(out=st8[:, 2, :], in0=st8[:, 0, :], scalar1=1.0 / 64, scalar2=None, op0=ALU.mult),
                                 reads=['st8'], writes=['st8'])
                            P.op('dve', lambda e: e.tensor_tensor(out=st8[:, 0, :], in0=st8[:, 2, :], in1=st8[:, 2, :], op=ALU.mult), reads=['st8'], writes=['st8'])
                            P.op('dve', lambda e: e.scalar_tensor_tensor(out=st8[:, 3, :], in0=st8[:, 1, :], scalar=1.0 / 64, in1=st8[:, 0, :],
                                                                         op0=ALU.mult, op1=ALU.subtract), reads=['st8'], writes=['st8'])
                            P.op('dve', lambda e: e.tensor_scalar(out=st8[:, 3, :], in0=st8[:, 3, :], scalar1=64e-5, scalar2=None, op0=ALU.add),
                                 reads=['st8'], writes=['st8'])
                            rsqrt(lambda: st8[:, 3, :], 'st8')
                            for g in range(8):
                                P.op('dve', lambda e, g=g: e.tensor_scalar(out=ytm[:, g // 2, (g % 2) * 64:(g % 2) * 64 + 64],
                                                                          in0=ytm[:, g // 2, (g % 2) * 64:(g % 2) * 64 + 64],
                                                                          scalar1=st8[:, 2, g:g + 1], scalar2=st8[:, 3, g:g + 1],
                                                                          op0=ALU.subtract, op1=ALU.mult),
                                     reads=yk + ['st8'], writes=yk)
                            pb = nps()

                            def ytr(e, pb=pb):
                                for i in range(4):
                                    r = e.transpose(ps[pb][:, i * 128:(i + 1) * 128], ytm[:, i, :], ident)
                                return r
                            P.op('pe', ytr, reads=yk + ['cst'], writes=[('ps', pb)])
                            P.op('act', lambda e, pb=pb, j=j: e.activation(out=F2('tmpB'), in_=ps[pb][:, :], func=AF.Identity,
                                                                           scale=col('gn_g', j), bias=col('gn_b', j)),
                                 reads=[('ps', pb), 'cols'], writes=['tmpB'])
                            P.op('dve', lambda e: e.tensor_tensor(out=F2('tmpB'), in0=F2('tmpB'), in1=F2('bon'), op=ALU.add), reads=['tmpB', 'bon'], writes=['tmpB'])
                            P.op('dve', lambda e, j=j: e.tensor_tensor(out=yaT[:, j, :], in0=F2('tmpB'), in1=F2('g_t'), op=ALU.mult),
                                 reads=['tmpB', 'g_t'], writes=['yaT'])
                        if tb == 0:
                            tap('yaT', yaT[:], 'yaT')
                        P.barrier()
                        P.flush()

                    if stages < 3:
                        continue
                    with ExitStack() as sD:
                        names = ['q_t', 'fg', 'lf', 'kf', 'cum', 'c_t', 'Eq', 'Ek', 'qd', 'kd', 'vtm', 'sog', 'otm', 'osq', 'kdtm']
                        Tt = {n: sb(sD, 'h_' + n, [128, 4, 128]) for n in names}
                        eref = sb(sD, 'h_eref', [128, 4])
                        ecl = sb(sD, 'h_ecl', [128, 4])
                        AT = sb(sD, 'h_AT', [128, 128])
                        S0p = sb(sD, 'h_S0p', [128, 128])
                        rs4 = sb(sD, 'h_rs4', [128, 4])

                        def F2(n):
                            return Tt[n][:].rearrange("p a b -> p (a b)")

                        for h in range(8):
                            pb = proj_fm(CI_HQ + h)
                            P.op('act', lambda e, pb=pb: e.activation(out=F2('q_t'), in_=ps[pb][:, :], func=AF.Silu), reads=[('ps', pb)], writes=['q_t'])
                            pb = proj_fm(CI_HF + h)
                            P.op('act', lambda e, pb=pb: e.activation(out=F2('fg'), in_=ps[pb][:, :], func=AF.Sigmoid), reads=[('ps', pb)], writes=['fg'])
                            P.op('dve', lambda e, h=h: e.tensor_scalar(out=F2('fg'), in0=F2('fg'), scalar1=omlc[:, h:h + 1], scalar2=lbc[:, h:h + 1],
                                                                      op0=ALU.mult, op1=ALU.add), reads=['fg', 'omlc', 'lbc'], writes=['fg'])
                            P.op('act', lambda e: e.activation(out=F2('lf'), in_=F2('fg'), func=AF.Ln), reads=['fg'], writes=['lf'])
                            P.op('pool', lambda e: e.tensor_scalar(out=F2('kf'), in0=F2('fg'), scalar1=-1.0, scalar2=1.0, op0=ALU.mult, op1=ALU.add),
                                 reads=['fg'], writes=['kf'])
                            for i in range(4):
                                P.op('dve', lambda e, i=i: e.tensor_tensor_scan(out=Tt['cum'][:, i, :], data0=Tt['lf'][:, i, :], data1=zeros,
                                                                               initial=0.0, op0=ALU.add, op1=ALU.add), reads=['lf', 'cst'], writes=['cum'])
                            for i in range(4):
                                P.op('dve', lambda e, i=i: e.tensor_scalar(out=Tt['c_t'][:, i, :], in0=Tt['cum'][:, i, :], scalar1=Tt['cum'][:, i, 63:64],
                                                                          scalar2=None, op0=ALU.subtract), reads=['cum'], writes=['c_t'])
                            P.op('act', lambda e: e.activation(out=ecl[:], in_=Tt['c_t'][:, :, 127], func=AF.Exp), reads=['c_t'], writes=['h_ecl'])
                            P.op('act', lambda e: e.activation(out=eref[:], in_=Tt['cum'][:, :, 63], func=AF.Exp), reads=['cum'], writes=['h_eref'])
                            P.op('dve', lambda e: e.tensor_scalar(out=F2('c_t'), in0=F2('c_t'), scalar1=-40.0, scalar2=40.0, op0=ALU.max, op1=ALU.min),
                                 reads=['c_t', 'h_ecl'], writes=['c_t'])
                            P.op('act', lambda e: e.activation(out=F2('Eq'), in_=F2('c_t'), func=AF.Exp), reads=['c_t'], writes=['Eq'])
                            P.op('act', lambda e: e.activation(out=F2('Ek'), in_=F2('c_t'), func=AF.Exp, scale=-1.0), reads=['c_t'], writes=['Ek'])
                            P.op('dve', lambda e: e.tensor_tensor(out=F2('qd'), in0=F2('q_t'), in1=F2('Eq'), op=ALU.mult), reads=['q_t', 'Eq'], writes=['qd'])
                            P.op('pool', lambda e: e.tensor_tensor(out=F2('kd'), in0=F2('kf'), in1=F2('Ek'), op=ALU.mult), reads=['kf', 'Ek'], writes=['kd'])
                            for nm, cbase, fn in (('vtm', CI_HI, None), ('sog', CI_HO, AF.Silu)):
                                w, wk = load_chunk(w_in_l[cbase + h])
                                pb = nps()

                                def tmf(e, pb=pb, w=w):
                                    for i in range(4):
                                        for k in range(16):
                                            r = e.matmul(ps[pb][:, i * 128:(i + 1) * 128], lhsT=hT[:, k, i * 128:(i + 1) * 128], rhs=w[:, k, :],
                                                         start=(k == 0), stop=(k == 15))
                                    return r
                                P.op('pe', tmf, reads=[wk, 'hT'], writes=[('ps', pb)])
                                if fn is None:
                                    P.op('act', lambda e, pb=pb, nm=nm: e.copy(out=F2(nm), in_=ps[pb][:, :]), reads=[('ps', pb)], writes=[nm])
                                else:
                                    P.op('act', lambda e, pb=pb, nm=nm, fn=fn: e.activation(out=F2(nm), in_=ps[pb][:, :], func=fn), reads=[('ps', pb)], writes=[nm])
                            pb = nps()

                            def ktr(e, pb=pb):
                                for i in range(4):
                                    r = e.transpose(ps[pb][:, i * 128:(i + 1) * 128], Tt['kd'][:, i, :], ident)
                                return r
                            P.op('pe', ktr, reads=['kd', 'cst'], writes=[('ps', pb)])
                            P.op('act', lambda e, pb=pb: e.copy(out=F2('kdtm'), in_=ps[pb][:, :]), reads=[('ps', pb)], writes=['kdtm'])
                            Sh = S_hg[:, h, :]
                            for i in range(4):
                                pb = nps()
                                P.op('pe', lambda e, pb=pb, i=i: e.matmul(ps[pb][:, 0:128], lhsT=Tt['kd'][:, i, :], rhs=Tt['qd'][:, i, :], start=True, stop=True),
                                     reads=['kd', 'qd'], writes=[('ps', pb)])
                                P.op('dve', lambda e, pb=pb: e.tensor_tensor(out=AT[:], in0=ps[pb][:, 0:128], in1=inclU, op=ALU.mult),
                                     reads=[('ps', pb), 'cst'], writes=['h_AT'])
                                P.op('dve', lambda e, i=i, Sh=Sh: e.tensor_scalar(out=S0p[:], in0=Sh, scalar1=eref[:, i:i + 1], scalar2=None, op0=ALU.mult),
                                     reads=['S_hg', 'h_eref'], writes=['h_S0p'])
                                pb = nps()

                                def of(e, pb=pb, i=i):
                                    e.matmul(ps[pb][:, 0:128], lhsT=AT[:], rhs=Tt['vtm'][:, i, :], start=True, stop=False)
                                    return e.matmul(ps[pb][:, 0:128], lhsT=Tt['qd'][:, i, :], rhs=S0p[:], start=False, stop=True)
                                P.op('pe', of, reads=['h_AT', 'vtm', 'qd', 'h_S0p'], writes=[('ps', pb)])
                                P.op('act', lambda e, pb=pb, i=i: e.copy(out=Tt['otm'][:, i, :], in_=ps[pb][:, 0:128]), reads=[('ps', pb)], writes=['otm'])
                                pb = nps()

                                def sf(e, pb=pb, i=i):
                                    e.matmul(ps[pb][:, 0:128], lhsT=ident, rhs=S0p[:], start=True, stop=False)
                                    return e.matmul(ps[pb][:, 0:128], lhsT=Tt['kdtm'][:, i, :], rhs=Tt['vtm'][:, i, :], start=False, stop=True)
                                P.op('pe', sf, reads=['cst', 'h_S0p', 'kdtm', 'vtm'], writes=[('ps', pb)])
                                P.op('dve', lambda e, pb=pb, i=i, Sh=Sh: e.tensor_scalar(out=Sh, in0=ps[pb][:, 0:128], scalar1=ecl[:, i:i + 1], scalar2=None, op0=ALU.mult),
                                     reads=[('ps', pb), 'h_ecl'], writes=['S_hg'])
                            if tb == 0 and h == 0:
                                tap('o0', Tt['otm'][:], 'otm')
                            P.op('pool', lambda e: e.tensor_tensor(out=F2('osq'), in0=F2('otm'), in1=F2('otm'), op=ALU.mult), reads=['otm'], writes=['osq'])
                            P.op('dve', lambda e: e.reduce_sum(out=rs4[:], in_=Tt['osq'][:], axis=AX.X), reads=['osq'], writes=['h_rs4'])
                            P.op('dve', lambda e: e.tensor_scalar(out=rs4[:], in0=rs4[:], scalar1=1.0 / 128, scalar2=1e-5, op0=ALU.mult, op1=ALU.add),
                                 reads=['h_rs4'], writes=['h_rs4'])
                            rsqrt(lambda: rs4[:], 'h_rs4')
                            for i in range(4):
                                P.op('dve', lambda e, i=i: e.scalar_tensor_tensor(out=Tt['otm'][:, i, :], in0=Tt['otm'][:, i, :], scalar=rs4[:, i:i + 1],
                                                                                 in1=Tt['sog'][:, i, :], op0=ALU.mult, op1=ALU.mult),
                                     reads=['otm', 'h_rs4', 'sog'], writes=['otm'])
                            pb = nps()

                            def otr(e, pb=pb):
                                for i in range(4):
                                    r = e.transpose(ps[pb][:, i * 128:(i + 1) * 128], Tt['otm'][:, i, :], ident)
                                return r
                            P.op('pe', otr, reads=['otm', 'cst'], writes=[('ps', pb)])
                            P.op('act', lambda e, pb=pb, h=h: e.activation(out=ybT[:, h, :], in_=ps[pb][:, :], func=AF.Identity, scale=col('hgn', 0)),
                                 reads=[('ps', pb), 'cols'], writes=['ybT'])
                        if tb == 0:
                            tap('ybT', ybT[:], 'ybT')
                        P.barrier()
                        P.flush()

                    if stages < 4:
                        continue
                    with ExitStack() as sE:
                        mgT = sb(sE, 'mgT', [128, 16, 512], BF16)
                        sga = sb(sE, 'sga', [128, 512])
                        sgb = sb(sE, 'sgb', [128, 512])
                        tE = sb(sE, 'tE', [128, 512])
                        xb = sb(sE, 'xb', [128, 4, D])
                        g1bc = sb(sE, 'g1bc', [128, D])
                        A2bc = sb(sE, 'A2bc', [128, D])
                        sh2bc = sb(sE, 'sh2bc', [128, D])
                        P.dma('sp', lambda e: e.dma_start(out=g1bc[:], in_=modrow[0:1, 0:D].to_broadcast([128, D])), reads=['modrow'], writes=['g1bc'])
                        P.dma('sp', lambda e: e.dma_start(out=sh2bc[:], in_=modrow[0:1, D:2 * D].to_broadcast([128, D])), reads=['modrow'], writes=['sh2bc'])
                        P.dma('sp', lambda e: e.dma_start(out=A2bc[:], in_=modrow[0:1, 2 * D:3 * D].to_broadcast([128, D])), reads=['modrow'], writes=['A2bc'])
                        P.dma('sp', lambda e: e.dma_start(out=xb[:], in_=x_in[t0_:t0_ + 512, :].rearrange("(a p) d -> p a d", p=128)), writes=['xb'])
                        for dc in range(16):
                            pb = proj_fm(CI_GA + dc)
                            P.op('act', lambda e, pb=pb: e.activation(out=sga[:], in_=ps[pb][:, :], func=AF.Sigmoid), reads=[('ps', pb)], writes=['sga'])
                            pb = proj_fm(CI_GB + dc)
                            P.op('act', lambda e, pb=pb: e.activation(out=sgb[:], in_=ps[pb][:, :], func=AF.Sigmoid), reads=[('ps', pb)], writes=['sgb'])
                            for src_l, yT, sgt, first in ((pa_l, yaT, sga, True), (pb_l, ybT, sgb, False)):
                                w, wk = load_chunk(src_l[dc], kdim=8)
                                pb = nps()

                                def zf(e, pb=pb, w=w, yT=yT):
                                    for k in range(8):
                                        r = e.matmul(ps[pb][:, :], lhsT=w[:, k, :], rhs=yT[:, k, :], start=(k == 0), stop=(k == 7))
                                    return r
                                P.op('pe', zf, reads=[wk, 'yaT', 'ybT'], writes=[('ps', pb)])
                                if first:
                                    P.op('dve', lambda e, pb=pb: e.tensor_tensor(out=tE[:], in0=ps[pb][:, :], in1=sga[:], op=ALU.mult),
                                         reads=[('ps', pb), 'sga'], writes=['tE'])
                                else:
                                    P.op('dve', lambda e, pb=pb: e.tensor_tensor(out=sgb[:], in0=ps[pb][:, :], in1=sgb[:], op=ALU.mult),
                                         reads=[('ps', pb), 'sgb'], writes=['sgb'])
                            P.op('pool', lambda e, dc=dc: e.tensor_tensor(out=mgT[:, dc, :], in0=tE[:], in1=sgb[:], op=ALU.add),
                                 reads=['tE', 'sgb'], writes=['mgT'])
                        if tb == 0:
                            tap('mgT', mgT[:], 'mgT')
                        for dc in range(16):
                            w, wk = load_chunk(wout_l[dc])
                            pb = nps()

                            def mf(e, pb=pb, w=w):
                                for i in range(4):
                                    for k in range(16):
                                        r = e.matmul(ps[pb][:, i * 128:(i + 1) * 128], lhsT=mgT[:, k, i * 128:(i + 1) * 128], rhs=w[:, k, :],
                                                     start=(k == 0), stop=(k == 15))
                                return r
                            P.op('pe', mf, reads=[wk, 'mgT'], writes=[('ps', pb)])
                            dsl = slice(dc * 128, (dc + 1) * 128)
                            P.op('dve', lambda e, pb=pb, dsl=dsl: e.tensor_tensor(out=tE[:].rearrange("p (a b) -> p a b", a=4),
                                                                                 in0=ps[pb][:, :].rearrange("p (a b) -> p a b", a=4),
                                                                                 in1=g1bc[:, dsl].rearrange("p (a b) -> p a b", a=1).to_broadcast([128, 4, 128]),
                                                                                 op=ALU.mult),
                                 reads=[('ps', pb), 'g1bc'], writes=['tE'])
                            P.op('pool', lambda e, dsl=dsl: e.tensor_tensor(out=xb[:, :, dsl], in0=xb[:, :, dsl], in1=tE[:].rearrange("p (a b) -> p a b", a=4), op=ALU.add),
                                 reads=['tE', 'xb'], writes=['xb'])
                        P.dma('sp', lambda e: e.dma_start(out=x1s[t0_:t0_ + 512, :].rearrange("(a p) d -> p a d", p=128), in_=xb[:]), reads=['xb'], writes=['x1s'])
                        if tb == 0:
                            tap('x1', xb[:], 'xb')
                        junk = sb(sE, 'junk', [128, D], BF16)
                        h2f = sb(sE, 'h2f', [128, D])
                        h2b = [sb(sE, 'h2b%d' % i, [128, D], BF16) for i in range(2)]
                        h2T = sb(sE, 'h2T', [128, 16, 128])
                        ss1 = sb(sE, 'ss1', [128, 1])
                        lg = sb(sE, 'lg', [128, 32])
                        mx8 = sb(sE, 'mx8', [128, 8])
                        msk = sb(sE, 'msk', [128, 32])
                        ex = sb(sE, 'ex', [128, 32])
                        den = sb(sE, 'den', [128, 1])
                        nmx = sb(sE, 'nmx', [128, 1])
                        vv = sb(sE, 'vv', [128, 32])
                        v8 = sb(sE, 'v8', [128, 8])
                        sel = sb(sE, 'sel', [128, 32])
                        for i in range(4):
                            it = tb * 4 + i
                            xt1 = xb[:, i, :]
                            P.op('act', lambda e, xt1=xt1: e.activation(out=junk[:], in_=xt1, func=AF.Square, accum_out=ss1[:]), reads=['xb'], writes=['junk', 'ss1'])
                            P.op('dve', lambda e: e.tensor_scalar(out=ss1[:], in0=ss1[:], scalar1=1.0 / D, scalar2=1e-5, op0=ALU.mult, op1=ALU.add), reads=['ss1'], writes=['ss1'])
                            rsqrt(lambda: ss1[:], 'ss1')
                            P.op('dve', lambda e, xt1=xt1: e.scalar_tensor_tensor(out=h2f[:], in0=xt1, scalar=ss1[:, 0:1], in1=A2bc[:], op0=ALU.mult, op1=ALU.mult),
                                 reads=['xb', 'ss1', 'A2bc'], writes=['h2f'])
                            P.op('pool', lambda e: e.tensor_tensor(out=h2f[:], in0=h2f[:], in1=sh2bc[:], op=ALU.add), reads=['h2f', 'sh2bc'], writes=['h2f'])
                            hb, hbk = h2b[it % 2], ('h2b', it % 2)
                            P.op('act', lambda e, hb=hb: e.copy(out=hb[:], in_=h2f[:]), reads=['h2f'], writes=[hbk])
                            if it == 0:
                                tap('h2', h2f[:], 'h2f')
                            for q4 in range(4):
                                pb = nps()

                                def htr(e, pb=pb, q4=q4):
                                    for c4 in range(4):
                                        k = q4 * 4 + c4
                                        r = e.transpose(ps[pb][:, c4 * 128:(c4 + 1) * 128], h2f[:, k * 128:(k + 1) * 128], ident)
                                    return r
                                P.op('pe', htr, reads=['h2f', 'cst'], writes=[('ps', pb)])
                                P.op('act', lambda e, pb=pb, q4=q4: e.copy(out=h2T[:, q4 * 4:(q4 + 1) * 4, :].rearrange("p a b -> p (a b)"), in_=ps[pb][:, :]),
                                     reads=[('ps', pb)], writes=[('h2T', q4)])
                            pb = nps()

                            def lgf(e, pb=pb):
                                for k in range(16):
                                    r = e.matmul(ps[pb][:, 0:32], lhsT=h2T[:, k, :], rhs=rwt[:, k, :], start=(k == 0), stop=(k == 15))
                                return r
                            P.op('pe', lgf, reads=[('h2T', q) for q in range(4)] + ['rwt'], writes=[('ps', pb)])
                            P.op('dve', lambda e, pb=pb: e.tensor_tensor(out=lg[:], in0=ps[pb][:, 0:32], in1=rbbc[:], op=ALU.add), reads=[('ps', pb), 'rbbc'], writes=['lg'])
                            if it == 0:
                                tap('lg', lg[:], 'lg')
                            P.op('dve', lambda e: e.max(out=mx8[:], in_=lg[:]), reads=['lg'], writes=['mx8'])
                            P.op('dve', lambda e: e.tensor_scalar(out=msk[:], in0=lg[:], scalar1=mx8[:, 3:4], scalar2=None, op0=ALU.is_ge), reads=['lg', 'mx8'], writes=['msk'])
                            P.op('dve', lambda e: e.tensor_scalar(out=nmx[:], in0=mx8[:, 0:1], scalar1=-1.0, scalar2=None, op0=ALU.mult), reads=['mx8'], writes=['nmx'])
                            P.op('act', lambda e: e.activation(out=ex[:], in_=lg[:], func=AF.Exp, bias=nmx[:, 0:1]), reads=['lg', 'nmx'], writes=['ex'])
                            P.op('dve', lambda e: e.tensor_tensor(out=ex[:], in0=ex[:], in1=msk[:], op=ALU.mult), reads=['ex', 'msk'], writes=['ex'])
                            P.op('dve', lambda e: e.reduce_sum(out=den[:], in_=ex[:], axis=AX.X), reads=['ex'], writes=['den'])
                            P.op('dve', lambda e: e.reciprocal(out=den[:], in_=den[:]), reads=['den'], writes=['den'])
                            P.op('dve', lambda e: e.tensor_scalar(out=ex[:], in0=ex[:], scalar1=den[:, 0:1], scalar2=None, op0=ALU.mult), reads=['ex', 'den'], writes=['ex'])
                            P.op('dve', lambda e, it=it: e.tensor_copy(out=maskb_all[:, it, :], in_=msk[:]), reads=['msk'], writes=[('maskb', it)])
                            pb = nps()

                            def pf(e, pb=pb, it=it):
                                for jt in range(it):
                                    e.matmul(ps[pb][:, 0:32], lhsT=ones_bf[:], rhs=maskb_all[:, jt, :], start=(jt == 0), stop=False)
                                return e.matmul(ps[pb][:, 0:32], lhsT=triS_bf[:], rhs=maskb_all[:, it, :], start=(it == 0), stop=True)
                            P.op('pe', pf, reads=[('maskb', jt) for jt in range(it + 1)] + ['ones_bf', 'triS_bf'], writes=[('ps', pb)])
                            P.op('dve', lambda e, pb=pb: e.tensor_tensor(out=vv[:], in0=ps[pb][:, 0:32], in1=ecap1, op=ALU.add), reads=[('ps', pb), 'cst'], writes=['vv'])
                            P.op('dve', lambda e: e.tensor_tensor(out=vv[:], in0=vv[:], in1=msk[:], op=ALU.mult), reads=['vv', 'msk'], writes=['vv'])
                            P.op('dve', lambda e: e.max(out=v8[:], in_=vv[:]), reads=['vv'], writes=['v8'])
                            for k4 in range(4):
                                P.op('dve', lambda e, k4=k4: e.tensor_scalar(out=sel[:], in0=vv[:], scalar1=v8[:, k4:k4 + 1], scalar2=None, op0=ALU.is_equal),
                                     reads=['vv', 'v8'], writes=['sel'])
                                P.op('dve', lambda e: e.tensor_tensor(out=sel[:], in0=sel[:], in1=ex[:], op=ALU.mult), reads=['sel', 'ex'], writes=['sel'])
                                P.op('dve', lambda e, k4=k4, it=it: e.reduce_sum(out=g4_all[:, it, k4:k4 + 1], in_=sel[:], axis=AX.X), reads=['sel'], writes=['g4_all'])
                            P.op('dve', lambda e: e.tensor_scalar(out=v8[:, 0:4], in0=v8[:, 0:4], scalar1=-1.0, scalar2=None, op0=ALU.add), reads=['v8'], writes=['v8'])
                            P.op('dve', lambda e, it=it: e.tensor_copy(out=idx4_all[:, it, :], in_=v8[:, 0:4]), reads=['v8'], writes=['idx4_all'])
                            for k4 in range(4):
                                P.dma('pool', lambda e, hb=hb, it=it, k4=k4: e.indirect_dma_start(
                                    out=Xg, out_offset=bass.IndirectOffsetOnAxis(ap=idx4_all[:, it, k4:k4 + 1].bitcast(U32), axis=0),
                                    in_=hb[:], in_offset=None), reads=[hbk, 'idx4_all'], writes=['Xg'])
                        if tb == 0:
                            tap('idx4', idx4_all[:, 0:4, :], 'idx4_all')
                            tap('g4', g4_all[:, 0:4, :], 'g4_all')
                        P.barrier()
                        P.flush()
            if stages >= 5:
                with ExitStack() as sT:
                    cnt = sb(sT, 'cnt', [128, 32])
                    nbt = sb(sT, 'nbt', [128, 32])
                    tq = sb(sT, 'tq', [128, 32])
                    cum = sb(sT, 'cumx', [128, 32])
                    excl = sb(sT, 'excl', [128, 32])
                    oh = sb(sT, 'oh', [128, 32])
                    r3 = sb(sT, 'r3', [128, 3])
                    SPt = sb(sT, 'SPt', [128, NPASS])
                    pb = nps()

                    def cf(e, pb=pb):
                        for jt in range(NT):
                            r = e.matmul(ps[pb][:, 0:32], lhsT=ones_bf[:], rhs=maskb_all[:, jt, :], start=(jt == 0), stop=(jt == NT - 1))
                        return r
                    P.op('pe', cf, reads=[('maskb', jt) for jt in range(NT)] + ['ones_bf'], writes=[('ps', pb)])
                    P.op('dve', lambda e, pb=pb: e.tensor_copy(out=cnt[:], in_=ps[pb][:, 0:32]), reads=[('ps', pb)], writes=['cnt'])
                    P.op('dve', lambda e: e.tensor_scalar(out=nbt[:], in0=cnt[:], scalar1=0.5, scalar2=None, op0=ALU.is_gt), reads=['cnt'], writes=['nbt'])
                    for jq in range(1, CAP // SUB):
                        P.op('dve', lambda e, jq=jq: e.tensor_scalar(out=tq[:], in0=cnt[:], scalar1=jq * SUB + 0.5, scalar2=None, op0=ALU.is_gt), reads=['cnt'], writes=['tq'])
                        P.op('dve', lambda e: e.tensor_tensor(out=nbt[:], in0=nbt[:], in1=tq[:], op=ALU.add), reads=['nbt', 'tq'], writes=['nbt'])
                    P.op('dve', lambda e: e.tensor_tensor_scan(out=cum[:], data0=nbt[:], data1=zeros[:, 0:32], initial=0.0, op0=ALU.add, op1=ALU.add),
                         reads=['nbt', 'cst'], writes=['cumx'])
                    P.op('dve', lambda e: e.tensor_tensor(out=excl[:], in0=cum[:], in1=nbt[:], op=ALU.subtract), reads=['cumx', 'nbt'], writes=['excl'])
                    for p_ in range(NPASS):
                        P.op('dve', lambda e, p_=p_: e.tensor_scalar(out=tq[:], in0=cum[:], scalar1=p_ + 0.5, scalar2=None, op0=ALU.is_gt), reads=['cumx'], writes=['tq'])
                        P.op('dve', lambda e, p_=p_: e.scalar_tensor_tensor(out=oh[:], in0=excl[:], scalar=p_ + 0.5, in1=tq[:], op0=ALU.is_lt, op1=ALU.mult),
                             reads=['excl', 'tq'], writes=['oh'])
                        P.op('dve', lambda e: e.reduce_sum(out=r3[:, 0:1], in_=oh[:], axis=AX.X), reads=['oh'], writes=['r3'])
                        P.op('dve', lambda e: e.tensor_tensor(out=tq[:], in0=oh[:], in1=erow, op=ALU.mult), reads=['oh', 'cst'], writes=['tq'])
                        P.op('dve', lambda e, p_=p_: e.reduce_sum(out=EP[:, p_:p_ + 1], in_=tq[:], axis=AX.X), reads=['tq'], writes=['EP'])
                        P.op('dve', lambda e: e.tensor_tensor(out=tq[:], in0=oh[:], in1=excl[:], op=ALU.mult), reads=['oh', 'excl'], writes=['tq'])
                        P.op('dve', lambda e: e.reduce_sum(out=r3[:, 1:2], in_=tq[:], axis=AX.X), reads=['tq'], writes=['r3'])
                        P.op('dve', lambda e, p_=p_: e.scalar_tensor_tensor(out=SPt[:, p_:p_ + 1], in0=r3[:, 0:1], scalar=float(p_), in1=r3[:, 1:2],
                                                                          op0=ALU.mult, op1=ALU.subtract), reads=['r3'], writes=['SPt'])
                    P.op('dve', lambda e: e.tensor_scalar(out=RB[:], in0=EP[:], scalar1=float(CAP), scalar2=None, op0=ALU.mult), reads=['EP'], writes=['RB'])
                    P.op('dve', lambda e: e.scalar_tensor_tensor(out=RB[:], in0=SPt[:], scalar=float(SUB), in1=RB[:], op0=ALU.mult, op1=ALU.add),
                         reads=['SPt', 'RB'], writes=['RB'])
                    if debug and 'EP' in debug:
                        P.dma('sp', lambda e: e.dma_start(out=dbg['EP'], in_=EP[:]), reads=['EP'])
                        P.dma('sp', lambda e: e.dma_start(out=dbg['RB'], in_=RB[:]), reads=['RB'])
                    P.barrier()
                    P.flush()
            P.barrier()
            P.flush()

        if stages >= 5:
            wgu_rows = wgu_l.rearrange("e s p k n -> (e s p) (k n)")
            wdn_rows = wdn_l.rearrange("e s p k n -> (e s p) (k n)")
            with ExitStack() as sG:
                bgu_t = sb(sG, 'bgu_t', [128, 32])
                bdn_t = sb(sG, 'bdn_t', [128, D])
                bdrb = sb(sG, 'bdrb', [1, D], BF16)
                ones1 = sb(sG, 'ones1', [1, 128], BF16)
                identb = sb(sG, 'identb', [128, 128], BF16)
                xg = sb(sG, 'xg', [128, NA, D], BF16)
                XeT = sb(sG, 'XeT', [128, 16, SUB], BF16)
                actT = sb(sG, 'actT', [128, 16, SUB], BF16)
                glu = sb(sG, 'glu', [128, 4, SUB])
                sgl = sb(sG, 'sgl', [128, 4, SUB])
                lin = sb(sG, 'lin', [128, SUB])
                ysb = [sb(sG, 'ysb%d' % i, [128, D], BF16) for i in range(NA)]
                wst = [sb(sG, 'gwst%d' % i, [128, 16 * 512]) for i in range(2)]
                wbf = [sb(sG, 'gwbf%d' % i, [128, 16, 512], BF16) for i in range(2)]
                fidx = sb(sG, 'fidx', [128, 20])
                iidx = [sb(sG, 'iidx%d' % i, [128, 20], I32) for i in range(2)]
                P.op('pool', lambda e: e.memset(ones1[:], 1.0), writes=['ones1'])
                P.op('dve', lambda e: e.tensor_copy(out=identb[:], in_=ident), reads=['cst'], writes=['identb'])
                sln = [0]

                def load_slab(rows_ap, idx_ap, ik):
                    n = sln[0]
                    sln[0] += 1
                    b = n % 2
                    P.dma('pool', lambda e: e.indirect_dma_start(out=wst[b][:], out_offset=None, in_=rows_ap,
                                                                 in_offset=bass.IndirectOffsetOnAxis(ap=idx_ap, axis=0)),
                          reads=[ik], writes=[('gwst', b)])
                    wv = wst[b][:].rearrange("p (k n) -> p k n", k=16)
                    P.op('dve', lambda e: e.tensor_copy(out=wbf[b][:, 0:8, :], in_=wv[:, 0:8, :]), reads=[('gwst', b)], writes=[('gwbf', b, 0)])
                    P.op('act', lambda e: e.copy(out=wbf[b][:, 8:16, :], in_=wv[:, 8:16, :]), reads=[('gwst', b)], writes=[('gwbf', b, 1)])
                    return wbf[b], [('gwbf', b, 0), ('gwbf', b, 1)]

                for pas in range(NPASS):
                    ii, ik = iidx[pas % 2], ('iidx', pas % 2)
                    epc = EP[:, pas:pas + 1]
                    P.op('dve', lambda e, pas=pas: e.tensor_scalar(out=fidx[:, 0:4], in0=iota_aq, scalar1=RB[:, pas:pas + 1], scalar2=None, op0=ALU.add),
                         reads=['RB', 'cst'], writes=['fidx'])
                    P.op('dve', lambda e, epc=epc: e.scalar_tensor_tensor(out=fidx[:, 4:12], in0=iota_s8q, scalar=0.0, in1=epc.to_broadcast([128, 8]), op0=ALU.add, op1=ALU.add),
                         reads=['EP', 'cst'], writes=['fidx'])
                    P.op('dve', lambda e, epc=epc: e.scalar_tensor_tensor(out=fidx[:, 4:12], in0=epc.to_broadcast([128, 8]), scalar=1023.0, in1=fidx[:, 4:12], op0=ALU.mult, op1=ALU.add),
                         reads=['EP', 'fidx'], writes=['fidx'])
                    P.op('dve', lambda e, epc=epc: e.scalar_tensor_tensor(out=fidx[:, 12:16], in0=epc.to_broadcast([128, 4]), scalar=512.0, in1=iota_aq, op0=ALU.mult, op1=ALU.add),
                         reads=['EP', 'cst'], writes=['fidx'])
                    P.op('dve', lambda e, epc=epc: e.scalar_tensor_tensor(out=fidx[:, 16:17], in0=epc, scalar=128.0, in1=iota_q, op0=ALU.mult, op1=ALU.add),
                         reads=['EP', 'cst'], writes=['fidx'])
                    P.op('dve', lambda e, epc=epc: e.tensor_copy(out=fidx[:, 17:18], in_=epc), reads=['EP'], writes=['fidx'])
                    P.op('dve', lambda e, ii=ii: e.tensor_copy(out=ii[:, 0:18], in_=fidx[:, 0:18]), reads=['fidx'], writes=[ik])

                    def ix(c, ii=ii):
                        return ii[:, c:c + 1].bitcast(U32)
                    for a in range(NA):
                        P.dma('pool', lambda e, a=a, ix=ix: e.indirect_dma_start(out=xg[:, a, :], out_offset=None, in_=Xg,
                                                                              in_offset=bass.IndirectOffsetOnAxis(ap=ix(a), axis=0)),
                              reads=[ik], writes=['xg'])
                    P.dma('pool', lambda e, ix=ix: e.indirect_dma_start(out=bgu_t[:], out_offset=None, in_=bgu_rows,
                                                                       in_offset=bass.IndirectOffsetOnAxis(ap=ix(16), axis=0)), reads=[ik], writes=['bgu_t'])
                    P.dma('pool', lambda e, ix=ix: e.indirect_dma_start(out=bdn_t[:], out_offset=None, in_=bdn_rows,
                                                                       in_offset=bass.IndirectOffsetOnAxis(ap=ix(17), axis=0)), reads=[ik], writes=['bdn_t'])
                    P.op('dve', lambda e: e.tensor_copy(out=bdrb[:], in_=bdn_t[0:1, :]), reads=['bdn_t'], writes=['bdrb'])
                    for k in range(16):
                        pb = nps()

                        def xtr(e, pb=pb, k=k):
                            pv = ps[pb][:, 0:SUB // 2].bitcast(BF16)
                            for a in range(NA):
                                r = e.transpose(pv[:, a * 128:(a + 1) * 128], xg[:, a, k * 128:(k + 1) * 128], identb[:])
                            return r
                        P.op('pe', xtr, reads=['xg', 'identb'], writes=[('ps', pb)])
                        if k % 2 == 0:
                            P.op('act', lambda e, pb=pb, k=k: e.copy(out=XeT[:, k, :], in_=ps[pb][:, 0:SUB // 2].bitcast(BF16)), reads=[('ps', pb)], writes=['XeT'])
                        else:
                            P.op('dve', lambda e, pb=pb, k=k: e.tensor_copy(out=XeT[:, k, :], in_=ps[pb][:, 0:SUB // 2].bitcast(BF16)), reads=[('ps', pb)], writes=['XeT'])
                    for gs in range(4):
                        for half in range(2):
                            w, wks = load_slab(wgu_rows, ix(4 + half * 4 + gs), ik)
                            for c4 in range(4):
                                pb = nps()

                                def guf(e, pb=pb, w=w, c4=c4):
                                    for k in range(16):
                                        r = e.matmul(ps[pb][:, 0:SUB], lhsT=w[:, k, c4 * 128:(c4 + 1) * 128], rhs=XeT[:, k, :], start=(k == 0), stop=(k == 15))
                                    return r
                                P.op('pe', guf, reads=wks + ['XeT'], writes=[('ps', pb)])
                                bc_ = half * 16 + gs * 4 + c4
                                bcol = bgu_t[:, bc_:bc_ + 1]
                                if half == 0:
                                    P.op('dve', lambda e, pb=pb, c4=c4, bcol=bcol: e.tensor_scalar(out=glu[:, c4, :], in0=ps[pb][:, 0:SUB], scalar1=bcol, scalar2=7.0,
                                                                                                   op0=ALU.add, op1=ALU.min), reads=[('ps', pb), 'bgu_t'], writes=[('glu', c4)])
                                    P.op('act', lambda e, c4=c4: e.activation(out=sgl[:, c4, :], in_=glu[:, c4, :], func=AF.Sigmoid, scale=1.702), reads=[('glu', c4)], writes=[('sgl', c4)])
                                    P.op('dve', lambda e, c4=c4: e.tensor_tensor(out=glu[:, c4, :], in0=glu[:, c4, :], in1=sgl[:, c4, :], op=ALU.mult),
                                         reads=[('glu', c4), ('sgl', c4)], writes=[('glu', c4)])
                                else:
                                    P.op('dve', lambda e, pb=pb, bcol=bcol: e.tensor_scalar(out=lin[:], in0=ps[pb][:, 0:SUB], scalar1=bcol, scalar2=7.0,
                                                                                            op0=ALU.add, op1=ALU.min), reads=[('ps', pb), 'bgu_t'], writes=['lin'])
                                    P.op('dve', lambda e: e.tensor_scalar(out=lin[:], in0=lin[:], scalar1=-7.0, scalar2=1.0, op0=ALU.max, op1=ALU.add), reads=['lin'], writes=['lin'])
                                    P.op('dve', lambda e, c4=c4, gs=gs: e.tensor_tensor(out=actT[:, gs * 4 + c4, :], in0=lin[:], in1=glu[:, c4, :], op=ALU.mult),
                                         reads=['lin', ('glu', c4)], writes=['actT'])
                    for db in range(4):
                        w, wks = load_slab(wdn_rows, ix(12 + db), ik)
                        for a in range(NA):
                            pb = nps()

                            def dnf(e, pb=pb, w=w, a=a, db=db):
                                for k in range(16):
                                    e.matmul(ps[pb][:, :], lhsT=actT[:, k, a * 128:(a + 1) * 128], rhs=w[:, k, :], start=(k == 0), stop=False)
                                return e.matmul(ps[pb][:, :], lhsT=ones1[:], rhs=bdrb[0:1, db * 512:(db + 1) * 512], start=False, stop=True)
                            P.op('pe', dnf, reads=wks + ['actT', 'ones1', 'bdrb'], writes=[('ps', pb)])
                            P.op('act', lambda e, pb=pb, a=a, db=db: e.copy(out=ysb[a][:, db * 512:(db + 1) * 512], in_=ps[pb][:, :]), reads=[('ps', pb)], writes=[('ysb', a)])
                    for a in range(NA):
                        P.dma('pool', lambda e, a=a, ix=ix: e.indirect_dma_start(out=Ys, out_offset=bass.IndirectOffsetOnAxis(ap=ix(a), axis=0),
                                                                              in_=ysb[a][:], in_offset=None), reads=[('ysb', a), ik], writes=[('Ys', pas, a)])
                    if pas % 4 == 3:
                        P.barrier()
                        P.flush()
                P.barrier()
                P.flush()

        if debug and 'idx4full' in debug:
            P.dma('sp', lambda e: e.dma_start(out=dbg['idx4full'], in_=idx4_all[:]), reads=['idx4_all'])
            P.dma('sp', lambda e: e.dma_start(out=dbg['g4full'], in_=g4_all[:]), reads=['g4_all'])
            P.barrier()
            P.flush()
        if debug and 'Xg0' in debug:
            P.dma('sp', lambda e: e.dma_start(out=dbg['Xg0'], in_=Xg[0:128, :]), reads=['Xg'])
            P.dma('sp', lambda e: e.dma_start(out=dbg['Ys0'], in_=Ys[0:128, :]), reads=['Ys'])
            P.dma('sp', lambda e: e.dma_start(out=dbg['x1s'], in_=x1s[0:128, :]), reads=['x1s'])
            P.barrier()
            P.flush()
        if stages >= 6:
            with ExitStack() as sH:
                g2bc = sb(sH, 'g2bc', [128, D])
                fnbc = sb(sH, 'fnbc', [128, D])
                x1t = [sb(sH, 'x1t%d' % i, [128, D]) for i in range(2)]
                yg = [sb(sH, 'yg%d' % i, [128, D], BF16) for i in range(4)]
                acc = sb(sH, 'acc', [128, D])
                junk = sb(sH, 'junkH', [128, D], BF16)
                ssH = sb(sH, 'ssH', [128, 1])
                P.dma('sp', lambda e: e.dma_start(out=g2bc[:], in_=modrow[0:1, 3 * D:4 * D].to_broadcast([128, D])), reads=['modrow'], writes=['g2bc'])
                P.dma('sp', lambda e: e.dma_start(out=fnbc[:], in_=rows_d[0:1, D:2 * D].to_broadcast([128, D])), writes=['fnbc'])
                for it in range(NT):
                    xt_, xk = x1t[it % 2], ('x1t', it % 2)
                    P.dma('sp', lambda e, xt_=xt_, it=it: e.dma_start(out=xt_[:], in_=x1s[it * 128:(it + 1) * 128, :]), reads=['x1s'], writes=[xk])
                    for k4 in range(4):
                        P.dma('pool', lambda e, it=it, k4=k4: e.indirect_dma_start(
                            out=yg[k4][:], out_offset=None, in_=Ys,
                            in_offset=bass.IndirectOffsetOnAxis(ap=idx4_all[:, it, k4:k4 + 1].bitcast(U32), axis=0)),
                            reads=['Ys', 'idx4_all'], writes=[('yg', k4)])
                    P.op('dve', lambda e, it=it: e.tensor_scalar(out=acc[:], in0=yg[0][:], scalar1=g4_all[:, it, 0:1], scalar2=None, op0=ALU.mult),
                         reads=[('yg', 0), 'g4_all'], writes=['acc'])
                    for k4 in range(1, 4):
                        P.op('dve', lambda e, it=it, k4=k4: e.scalar_tensor_tensor(out=acc[:], in0=yg[k4][:], scalar=g4_all[:, it, k4:k4 + 1], in1=acc[:],
                                                                                   op0=ALU.mult, op1=ALU.add), reads=[('yg', k4), 'g4_all', 'acc'], writes=['acc'])
                    P.op('pool', lambda e: e.tensor_tensor(out=acc[:], in0=acc[:], in1=g2bc[:], op=ALU.mult), reads=['acc', 'g2bc'], writes=['acc'])
                    P.op('dve', lambda e, xt_=xt_: e.tensor_tensor(out=xt_[:], in0=xt_[:], in1=acc[:], op=ALU.add), reads=[xk, 'acc'], writes=[xk])
                    P.op('act', lambda e, xt_=xt_: e.activation(out=junk[:], in_=xt_[:], func=AF.Square, accum_out=ssH[:]), reads=[xk], writes=['junkH', 'ssH'])
                    P.op('dve', lambda e: e.tensor_scalar(out=ssH[:], in0=ssH[:], scalar1=1.0 / D, scalar2=1e-5, op0=ALU.mult, op1=ALU.add), reads=['ssH'], writes=['ssH'])
                    rsqrt(lambda: ssH[:], 'ssH')
                    P.op('dve', lambda e, xt_=xt_: e.scalar_tensor_tensor(out=xt_[:], in0=xt_[:], scalar=ssH[:, 0:1], in1=fnbc[:], op0=ALU.mult, op1=ALU.mult),
                         reads=[xk, 'ssH', 'fnbc'], writes=[xk])
                    P.dma('sp', lambda e, xt_=xt_, it=it: e.dma_start(out=out[it * 128:(it + 1) * 128, :], in_=xt_[:]), reads=[xk], writes=['out'])
                P.barrier()
                P.flush()

        P.barrier()
        P.flush()
    return nc


NCH = 91
NBLK_DBG = [4]
OPLIMIT = [10 ** 9]
CI_WA, CI_GD0, CI_GD1 = 24, 25, 26
CI_HQ, CI_HF, CI_HI, CI_HO = 27, 35, 43, 51
CI_GA, CI_GB = 59, 75
COLOFF = {'mu': 0, 'w0': 27, 'a0': 35, 'k_k': 43, 'k_a': 51, 'r_k': 59, 'gn_g': 67, 'gn_b': 75, 'lb0': 83, 'lb1': 91, 'hgn': 99}
NCOL = 100
NCST = 1356


def _f(a):
    return np.ascontiguousarray(np.asarray(a, dtype=np.float32))


def _chunks(w, starts, width=128):
    outl = []
    for c0 in starts:
        blk = np.zeros((2048, 128), np.float32)
        n = min(width, w.shape[1] - c0)
        blk[:, :n] = w[:, c0:c0 + n]
        outl.append(blk.reshape(16, 128, 128).transpose(1, 0, 2))
    return np.ascontiguousarray(np.stack(outl, 0))


def _colv(v, n):
    buf = np.zeros(n * 128, np.float32)
    buf[:v.size] = v.reshape(-1)
    return buf.reshape(n, 128).T


def prep_shared(inputs, experts=True):
    sh = {}
    aw = _f(inputs['ada_w'])[0]
    sh['ada_w'] = np.ascontiguousarray(aw.reshape(16, 128, 24, 512).transpose(2, 1, 0, 3))
    ab = _f(inputs['ada_b'])[0]
    sh['adab_col'] = np.ascontiguousarray(ab[:4096].reshape(32, 128).T)
    sh['adab_row'] = np.ascontiguousarray(ab[4096:].reshape(1, 8192))
    sh['n1g_col'] = np.ascontiguousarray(_f(inputs['norm1_g'])[0].reshape(16, 128).T)
    w_in = _f(inputs['w_in'])[0]
    starts = [i * 128 for i in range(27)] + [3360 + i * 128 for i in range(32)] + [7456 + i * 128 for i in range(32)]
    wl = _chunks(w_in, starts)
    wl[26, :, :, 32:] = 0.0
    sh['w_in_l'] = wl
    cols = np.zeros((128, NCOL), np.float32)
    cols[:, 0:27] = _colv(_f(inputs['rwkv_mu'])[0], 27)
    for nm, key in (('w0', 'rwkv_w0'), ('a0', 'rwkv_a0'), ('k_k', 'rwkv_k_k'), ('k_a', 'rwkv_k_a'), ('r_k', 'rwkv_r_k'),
                    ('gn_g', 'rwkv_gn_g'), ('gn_b', 'rwkv_gn_b')):
        cols[:, COLOFF[nm]:COLOFF[nm] + 8] = _colv(_f(inputs[key])[0], 8)
    lbl = _f(inputs['hgrn_lb_logits'])
    cols[:, 83:91] = _colv(lbl[0], 8)
    cols[:, 91:99] = _colv(lbl[1], 8)
    cols[:, 99:100] = _f(inputs['hgrn_gn_g'])[0].reshape(128, 1)
    sh['cols'] = cols
    p = np.arange(128)[:, None]
    fcol = np.arange(128)[None, :]
    sU = (p < fcol).astype(np.float32)
    iU = (p <= fcol).astype(np.float32)
    sL = (p > fcol).astype(np.float32)
    cst = np.zeros((128, NCST), np.float32)
    cst[:, 0:512] = np.concatenate([sU, iU, sU, iU], 1)
    cst[:, 512:640] = np.eye(128, dtype=np.float32)
    cst[:, 640:768] = sL
    cst[:, 768:896] = iU
    cst[:, 896:1024] = ((p // 64) == (fcol // 64)).astype(np.float32)
    cst[:, 1152:1184] = (np.arange(32) * CAP + 1)[None, :].astype(np.float32)
    cst[:, 1184:1312] = sU
    cst[:, 1312:1344] = np.arange(32)[None, :].astype(np.float32)
    cst[:, 1344:1348] = (np.arange(4)[None, :] * 128 + p).astype(np.float32)
    cst[:, 1348:1356] = (np.arange(8)[None, :] * 128 + p).astype(np.float32)
    sh['cst'] = cst
    z64 = np.zeros((64, 1024), np.float32)
    sh['w2p'] = np.ascontiguousarray(np.concatenate([_f(inputs['rwkv_w2'])[0], z64], 0))
    sh['a2p'] = np.ascontiguousarray(np.concatenate([z64, _f(inputs['rwkv_a2'])[0]], 0))
    g2 = _f(inputs['rwkv_g2'])[0]
    sh['g2a'] = np.ascontiguousarray(g2[:128])
    sh['g2b'] = np.ascontiguousarray(np.concatenate([g2[128:160], np.zeros((96, 1024), np.float32)], 0))
    for nm, key in (('pa_l', 'proj_a'), ('pb_l', 'proj_b')):
        w = _f(inputs[key])[0]
        sh[nm] = np.ascontiguousarray(w.reshape(8, 128, 16, 128).transpose(2, 1, 0, 3))
    wo = _f(inputs['w_out'])[0]
    sh['wout_l'] = np.ascontiguousarray(wo.reshape(16, 128, 16, 128).transpose(2, 1, 0, 3))
    sh['rows'] = np.ascontiguousarray(np.concatenate([_f(inputs['norm2_g'])[0], _f(inputs['final_norm_g']),
                                                      _f(inputs['router_b'])[0]]).reshape(1, -1))
    rw = _f(inputs['router_w'])[0]
    sh['rw_l'] = np.ascontiguousarray(rw.reshape(16, 128, 32).transpose(1, 0, 2))
    if experts:
        wgu = np.asarray(inputs['exp_w_gate_up'], dtype=np.float32)[0]
        sh['wgu_l'] = np.ascontiguousarray(wgu.reshape(NE, 16, 128, 8, 512).transpose(0, 3, 2, 1, 4))
        wdn = np.asarray(inputs['exp_w_down'], dtype=np.float32)[0]
        sh['wdn_l'] = np.ascontiguousarray(wdn.reshape(NE, 16, 128, 4, 512).transpose(0, 3, 2, 1, 4))
    bgu = _f(inputs['exp_b_gate_up'])[0]
    sh['bgu_rows'] = np.ascontiguousarray(bgu.reshape(NE, 32, 128).transpose(0, 2, 1).reshape(NE * 128, 32))
    sh['bdn_rows'] = np.ascontiguousarray(_f(inputs['exp_b_down'])[0])
    return sh


def prep_core(inputs, b):
    x = np.asarray(inputs['x'], dtype=np.float32)[b]
    c = np.asarray(inputs['c'], dtype=np.float32)[b]
    return {
        'x': np.ascontiguousarray(x),
        'xT': np.ascontiguousarray(x.T),
        'cT': np.ascontiguousarray(c.reshape(16, 128).T),
    }


def kernel(**inputs):
    nc = build()
    sh = prep_shared(inputs)
    in_maps = []
    for b in range(8):
        m = dict(sh)
        m.update(prep_core(inputs, b))
        in_maps.append(m)
    res = run_bass_kernel_spmd(nc, in_maps, core_ids=list(range(8)))
    return np.stack([np.asarray(r['out'], dtype=np.float32) for r in res.results], axis=0)
```

```python
from contextlib import ExitStack
import numpy as np
import concourse.bass as bass
import concourse.mybir as mybir
from concourse.bass_utils import run_bass_kernel_spmd

F32 = mybir.dt.float32
BF16 = mybir.dt.bfloat16
I32 = mybir.dt.int32
U32 = mybir.dt.uint32
AF = mybir.ActivationFunctionType
ALU = mybir.AluOpType
AX = mybir.AxisListType

D = 2048
T = 2048
NT = 16
NB = 4
NE = 32
NPASS = 48
CAP = 2048
SUB = 512
NA = SUB // 128
RW = 1024
RWKV_COLS = 3360
HG0 = 3360
GT0 = 3360 + 4096
NDS = 32


class Prog:
    CE = ('pe', 'act', 'dve', 'pool')

    def __init__(self, nc, stack):
        self.nc = nc
        self.eng = {'pe': nc.tensor, 'act': nc.scalar, 'dve': nc.vector, 'pool': nc.gpsimd, 'sp': nc.sync}
        self.esem = {e: stack.enter_context(nc.semaphore('se_' + e)) for e in self.CE}
        self.dsem = [stack.enter_context(nc.semaphore('sd%d' % i)) for i in range(NDS)]
        self.duse = [0] * NDS
        self.dnext = 0
        self.seq = {e: 0 for e in self.CE}
        self.seen = {e: {} for e in self.eng}
        self.res = {}
        self.ops = {e: [] for e in self.eng}
        self.nops = 0

    def _need(self, e, ev, waits):
        if ev is None:
            return
        sem, val, src = ev
        if src == e and e == 'pe':
            return
        k = id(sem)
        if self.seen[e].get(k, 0) >= val:
            return
        self.seen[e][k] = val
        waits.append((sem, val))

    def _deps(self, e, reads, writes):
        waits = []
        for r in reads:
            st = self.res.get(r)
            if st:
                self._need(e, st['w'], waits)
        for w in writes:
            st = self.res.get(w)
            if st:
                self._need(e, st['w'], waits)
                for ev in st['r'].values():
                    self._need(e, ev, waits)
        return waits

    def _commit(self, e, ev, reads, writes):
        for r in reads:
            st = self.res.setdefault(r, {'w': None, 'r': {}})
            st['r'][(e, id(ev[0]))] = ev
        for w in writes:
            self.res[w] = {'w': ev, 'r': {}}

    def op(self, e, fn, reads=(), writes=()):
        if self.nops >= OPLIMIT[0]:
            return
        waits = self._deps(e, reads, writes)
        self.seq[e] += 1
        ev = (self.esem[e], self.seq[e], e)
        self.ops[e].append((waits, fn, (self.esem[e], 1)))
        self._commit(e, ev, reads, writes)
        self.nops += 1

    def dma(self, q, fn, reads=(), writes=()):
        if self.nops >= OPLIMIT[0]:
            return
        waits = self._deps(q, reads, writes)
        i = self.dnext
        self.dnext = (self.dnext + 1) % NDS
        sem = self.dsem[i]
        if self.duse[i] > 0:
            self._need(q, (sem, 16 * self.duse[i], 'dma'), waits)
        self.duse[i] += 1
        ev = (sem, 16 * self.duse[i], 'dma')
        self.ops[q].append((waits, fn, (sem, 16)))
        self._commit(q, ev, reads, writes)
        self.nops += 1

    def barrier(self):
        for e in self.eng:
            waits = []
            for c in self.CE:
                if self.seq[c] > 0:
                    self._need(e, (self.esem[c], self.seq[c], 'x'), waits)
            for i in range(NDS):
                if self.duse[i] > 0:
                    self._need(e, (self.dsem[i], 16 * self.duse[i], 'dma'), waits)
            if waits:
                self.ops[e].append((waits, None, None))
        self.res = {}

    def flush(self):
        nc = self.nc
        ops = self.ops
        self.ops = {e: [] for e in self.eng}
        with nc.Block() as block:
            def emit(name):
                def body(engine):
                    for waits, fn, inc in ops[name]:
                        for sem, val in waits:
                            engine.wait_ge(sem, val)
                        if fn is not None:
                            fn(engine).then_inc(inc[0], inc[1])
                return body
            block.tensor(emit('pe'))
            block.scalar(emit('act'))
            block.vector(emit('dve'))
            block.gpsimd(emit('pool'))
            block.sync(emit('sp'))


def build(debug=None, stages=99):
    nc = bass.Bass("TRN2", target_bir_lowering=False)
    dbg = {}
    with ExitStack() as top:
        P = Prog(nc, top)

        def din(name, shape, dt=F32):
            return nc.dram_tensor(name, list(shape), dt, kind="ExternalInput").ap()

        def dout(name, shape, dt=F32):
            return nc.dram_tensor(name, list(shape), dt, kind="ExternalOutput").ap()

        def dscratch(name, shape, dt=F32):
            return nc.dram_tensor(name, list(shape), dt, kind="Internal").ap()

        sbn = [0]

        def sb(st, name, shape, dt=F32):
            sbn[0] += 1
            return st.enter_context(nc.sbuf_tensor('sb%d_%s' % (sbn[0], name), list(shape), dt))

        xT = din('xT', [D, T])
        x_in = din('x', [T, D])
        cT = din('cT', [128, 16])
        ada_w = din('ada_w', [24, 128, 16, 512])
        adab_col = din('adab_col', [128, 32])
        adab_row = din('adab_row', [1, 8192])
        n1g_col = din('n1g_col', [128, 16])
        w_in_l = din('w_in_l', [NCH, 128, 16, 128])
        cols_d = din('cols', [128, NCOL])
        cst_d = din('cst', [128, NCST])
        w2p_d = din('w2p', [128, 1024])
        a2p_d = din('a2p', [128, 1024])
        g2a_d = din('g2a', [128, 1024])
        g2b_d = din('g2b', [128, 1024])
        pa_l = din('pa_l', [16, 128, 8, 128])
        pb_l = din('pb_l', [16, 128, 8, 128])
        wout_l = din('wout_l', [16, 128, 16, 128])
        rows_d = din('rows', [1, 2 * D + 32])
        rw_l = din('rw_l', [128, 16, 32])
        if stages >= 5:
            wgu_l = din('wgu_l', [NE, 8, 128, 16, 512])
            wdn_l = din('wdn_l', [NE, 4, 128, 16, 512])
        bgu_rows = din('bgu_rows', [NE * 128, 32])
        bdn_rows = din('bdn_rows', [NE, D])
        out = dout('out', [T, D])
        modrow = dscratch('modrow', [1, 8192])
        x1s = dscratch('x1s', [T, D])
        Xg = dscratch('Xg', [NE * CAP, D], BF16)
        Ys = dscratch('Ys', [NE * CAP, D], BF16)

        if debug:
            for nm, (shp, dt_) in debug.items():
                dbg[nm] = dout('dbg_' + nm, shp, dt_)

        def tap(name, ap, key):
            if debug and name in debug:
                P.dma('sp', lambda e: e.dma_start(out=dbg[name], in_=ap), reads=[key])

        ps = [top.enter_context(nc.psum_tensor('ps%d' % i, [128, 512], F32)) for i in range(8)]
        psn = [0]

        def nps():
            i = psn[0]
            psn[0] = (i + 1) % 8
            return i

        ones_bf = sb(top, 'ones_bf', [128, 128], BF16)
        P.op('pool', lambda e: e.memset(ones_bf[:], 1.0), writes=['ones_bf'])
        A1 = sb(top, 'A1', [128, 16])
        SH1 = sb(top, 'SH1', [128, 16])
        cst = sb(top, 'cst', [128, NCST])
        cols = sb(top, 'colsb', [128, NCOL])
        P.dma('sp', lambda e: e.dma_start(out=cst[:], in_=cst_d), writes=['cst'])
        P.dma('sp', lambda e: e.dma_start(out=cols[:], in_=cols_d), writes=['cols'])
        mask4 = cst[:, 0:512]
        ident = cst[:, 512:640]
        strictL = cst[:, 640:768]
        inclU = cst[:, 768:896]
        bd64 = cst[:, 896:1024]
        zeros = cst[:, 1024:1152]
        ecap1 = cst[:, 1152:1184]
        strictU = cst[:, 1184:1312]
        erow = cst[:, 1312:1344]
        iota_aq = cst[:, 1344:1348]
        iota_s8q = cst[:, 1348:1356]
        iota_q = cst[:, 1344:1345]
        idx4_all = sb(top, 'idx4_all', [128, NT, 4], I32)
        EP = sb(top, 'EP', [128, NPASS])
        RB = sb(top, 'RB', [128, NPASS])
        g4_all = sb(top, 'g4_all', [128, NT, 4])

        def rsqrt(apf, key):
            P.op('act', lambda e: e.activation(out=apf(), in_=apf(), func=AF.Sqrt), reads=[key], writes=[key])
            P.op('dve', lambda e: e.reciprocal(out=apf(), in_=apf()), reads=[key], writes=[key])

        def col(name, j=0, n=1):
            o = COLOFF[name] + j
            return cols[:, o:o + n]

        with ExitStack() as st:
            ct = sb(st, 'ct', [128, 16])
            cact = sb(st, 'cact', [128, 16], BF16)
            abc = sb(st, 'abc', [128, 32])
            n1g = sb(st, 'n1g', [128, 16])
            abr = sb(st, 'abr', [1, 8192])
            mrow = sb(st, 'mrow', [1, 8192])
            mcol = sb(st, 'mcol', [128, 32])
            wst = [sb(st, 'wst%d' % i, [128, 16, 512]) for i in range(2)]
            wbf = [sb(st, 'wbf%d' % i, [128, 16, 512], BF16) for i in range(2)]
            P.dma('sp', lambda e: e.dma_start(out=ct[:], in_=cT), writes=['ct'])
            P.dma('sp', lambda e: e.dma_start(out=abc[:], in_=adab_col), writes=['abc'])
            P.dma('sp', lambda e: e.dma_start(out=n1g[:], in_=n1g_col), writes=['n1g'])
            P.dma('sp', lambda e: e.dma_start(out=abr[:], in_=adab_row), writes=['abr'])
            P.op('act', lambda e: e.activation(out=cact[:], in_=ct[:], func=AF.Silu), reads=['ct'], writes=['cact'])
            for s in range(24):
                b = s % 2
                P.dma('sp', lambda e, s=s, b=b: e.dma_start(out=wst[b][:], in_=ada_w[s]), writes=[('wst', b)])
                P.op('dve', lambda e, b=b: e.tensor_copy(out=wbf[b][:, 0:8, :], in_=wst[b][:, 0:8, :]),
                     reads=[('wst', b)], writes=[('wbf', b, 0)])
                P.op('act', lambda e, b=b: e.copy(out=wbf[b][:, 8:16, :], in_=wst[b][:, 8:16, :]),
                     reads=[('wst', b)], writes=[('wbf', b, 1)])
                rd = [('wbf', b, 0), ('wbf', b, 1), 'cact']
                if s < 8:
                    for j in range(4):
                        cc = 4 * s + j

                        def mmf(e, b=b, j=j, cc=cc):
                            for k in range(16):
                                r = e.matmul(ps[0][:, cc:cc + 1], lhsT=wbf[b][:, k, j * 128:(j + 1) * 128],
                                             rhs=cact[:, k:k + 1], start=(k == 0), stop=(k == 15))
                            return r
                        P.op('pe', mmf, reads=rd, writes=[('ps', 0)])
                else:
                    pb = 1 + (s % 2)

                    def mmf(e, b=b, pb=pb):
                        for k in range(16):
                            r = e.matmul(ps[pb][0:1, :], lhsT=cact[:, k:k + 1], rhs=wbf[b][:, k, :],
                                         start=(k == 0), stop=(k == 15))
                        return r
                    P.op('pe', mmf, reads=rd, writes=[('ps', pb)])
                    c0 = (s - 8) * 512
                    P.op('dve', lambda e, pb=pb, c0=c0: e.tensor_tensor(
                        out=mrow[0:1, c0:c0 + 512], in0=ps[pb][0:1, :], in1=abr[0:1, c0:c0 + 512], op=ALU.add),
                        reads=[('ps', pb), 'abr'], writes=[('mrow', s)])
            P.op('dve', lambda e: e.tensor_tensor(out=mcol[:], in0=ps[0][:, 0:32], in1=abc[:], op=ALU.add),
                 reads=[('ps', 0), 'abc'], writes=['mcol'])
            P.op('dve', lambda e: e.scalar_tensor_tensor(out=A1[:], in0=mcol[:, 16:32], scalar=1.0, in1=n1g[:],
                                                         op0=ALU.add, op1=ALU.mult),
                 reads=['mcol', 'n1g'], writes=['A1'])
            P.op('dve', lambda e: e.tensor_copy(out=SH1[:], in_=mcol[:, 0:16]), reads=['mcol'], writes=['SH1'])
            n2r = sb(st, 'n2r', [1, D])
            P.dma('sp', lambda e: e.dma_start(out=n2r[:], in_=rows_d[0:1, 0:D]), writes=['n2r'])
            P.op('dve', lambda e: e.scalar_tensor_tensor(out=mrow[0:1, 2 * D:3 * D], in0=mrow[0:1, 2 * D:3 * D], scalar=1.0, in1=n2r[:],
                                                         op0=ALU.add, op1=ALU.mult),
                 reads=[('mrow', s) for s in range(16, 20)] + ['n2r'], writes=[('mrow', s) for s in range(16, 20)])
            P.dma('sp', lambda e: e.dma_start(out=modrow, in_=mrow[:]),
                  reads=[('mrow', s) for s in range(8, 24)], writes=['modrow'])
            P.barrier()
            P.flush()

        with ExitStack() as mx:
            rbbc = sb(mx, 'rbbc', [128, 32])
            rwt = sb(mx, 'rwt', [128, 16, 32])
            P.dma('sp', lambda e: e.dma_start(out=rbbc[:], in_=rows_d[0:1, 2 * D:2 * D + 32].to_broadcast([128, 32])),
                  writes=['rbbc'])
            P.dma('sp', lambda e: e.dma_start(out=rwt[:], in_=rw_l), writes=['rwt'])
            S_rw = sb(mx, 'S_rw', [128, 8, 128])
            S_hg = sb(mx, 'S_hg', [128, 8, 128])
            carry = sb(mx, 'carry', [128, 32])
            lbc = sb(mx, 'lbc', [128, 8])
            omlc = sb(mx, 'omlc', [128, 8])
            ommc = sb(mx, 'ommc', [128, 27])
            w2p = sb(mx, 'w2p', [128, 1024])
            a2p = sb(mx, 'a2p', [128, 1024])
            g2a = sb(mx, 'g2a', [128, 1024])
            g2b = sb(mx, 'g2b', [128, 1024])
            maskb_all = sb(mx, 'maskb_all', [128, NT, 32], BF16)
            triS_bf = sb(mx, 'triS_bf', [128, 128], BF16)
            P.op('pool', lambda e: e.memset(S_rw[:], 0.0), writes=['S_rw'])
            P.op('pool', lambda e: e.memset(S_hg[:], 0.0), writes=['S_hg'])
            P.op('pool', lambda e: e.memset(carry[:], 0.0), writes=['carry'])
            P.dma('sp', lambda e: e.dma_start(out=w2p[:], in_=w2p_d), writes=['w2p'])
            P.dma('sp', lambda e: e.dma_start(out=a2p[:], in_=a2p_d), writes=['a2p'])
            P.dma('sp', lambda e: e.dma_start(out=g2a[:], in_=g2a_d), writes=['g2a'])
            P.dma('sp', lambda e: e.dma_start(out=g2b[:], in_=g2b_d), writes=['g2b'])
            P.op('dve', lambda e: e.tensor_copy(out=triS_bf[:], in_=strictU), reads=['cst'], writes=['triS_bf'])
            P.op('dve', lambda e: e.tensor_tensor(out=lbc[:], in0=col('lb0', 0, 8), in1=col('lb1', 0, 8), op=ALU.subtract),
                 reads=['cols'], writes=['lbc'])
            P.op('act', lambda e: e.activation(out=lbc[:], in_=lbc[:], func=AF.Sigmoid), reads=['lbc'], writes=['lbc'])
            P.op('dve', lambda e: e.tensor_scalar(out=omlc[:], in0=lbc[:], scalar1=-1.0, scalar2=1.0, op0=ALU.mult, op1=ALU.add),
                 reads=['lbc'], writes=['omlc'])
            P.op('dve', lambda e: e.tensor_scalar(out=ommc[:], in0=col('mu', 0, 27), scalar1=-1.0, scalar2=1.0,
                                                  op0=ALU.mult, op1=ALU.add), reads=['cols'], writes=['ommc'])

            NSTG, NBFB = 2, 2
            stg = [sb(mx, 'stg%d' % i, [128, 16, 128]) for i in range(NSTG)]
            wcb = [sb(mx, 'wcb%d' % i, [128, 16, 128], BF16) for i in range(NBFB)]
            cln = [0]

            def load_chunk(src, kdim=16):
                n = cln[0]
                cln[0] += 1
                s_, b_ = n % NSTG, n % NBFB
                P.dma('sp', lambda e: e.dma_start(out=stg[s_][:, 0:kdim, :], in_=src), writes=[('stg', s_)])
                if n % 2 == 0:
                    P.op('act', lambda e: e.copy(out=wcb[b_][:, 0:kdim, :], in_=stg[s_][:, 0:kdim, :]),
                         reads=[('stg', s_)], writes=[('wcb', b_)])
                else:
                    P.op('pool', lambda e: e.tensor_copy(out=wcb[b_][:, 0:kdim, :], in_=stg[s_][:, 0:kdim, :]),
                         reads=[('stg', s_)], writes=[('wcb', b_)])
                return wcb[b_], ('wcb', b_)

            for tb in range(min(NB, NBLK_DBG[0]) if stages >= 1.5 else 0):
                t0_ = tb * 512
                with ExitStack() as bk:
                    hT = sb(bk, 'hT', [128, 16, 512], BF16)
                    yaT = sb(bk, 'yaT', [128, 8, 512], BF16)
                    ybT = sb(bk, 'ybT', [128, 8, 512], BF16)
                    praw = [sb(bk, 'praw%d' % i, [128, 513]) for i in range(2)]
                    shtmp = sb(bk, 'shtmp', [128, 512])
                    prn = [0]

                    with ExitStack() as sB:
                        xt = sb(sB, 'xt', [128, 16, 512])
                        sq = sb(sB, 'sq', [128, 16, 512], BF16)
                        rstd = sb(sB, 'rstd', [128, 1, 512])
                        P.dma('sp', lambda e: e.dma_start(
                            out=xt[:], in_=xT.rearrange("(k p) t -> p k t", p=128)[:, :, t0_:t0_ + 512]), writes=['xt'])
                        P.op('act', lambda e: e.activation(out=sq[:], in_=xt[:], func=AF.Square), reads=['xt'], writes=['sq'])
                        pb = nps()

                        def mmf(e, pb=pb):
                            for k in range(16):
                                r = e.matmul(ps[pb][:, :], lhsT=ones_bf[:], rhs=sq[:, k, :], start=(k == 0), stop=(k == 15))
                            return r
                        P.op('pe', mmf, reads=['sq', 'ones_bf'], writes=[('ps', pb)])
                        P.op('dve', lambda e, pb=pb: e.tensor_scalar(out=rstd[:, 0, :], in0=ps[pb][:, :], scalar1=1.0 / D, scalar2=1e-5,
                                                                     op0=ALU.mult, op1=ALU.add), reads=[('ps', pb)], writes=['rstd'])
                        rsqrt(lambda: rstd[:, 0, :], 'rstd')
                        P.op('dve', lambda e: e.tensor_tensor(out=xt[:], in0=xt[:], in1=rstd[:].to_broadcast([128, 16, 512]),
                                                              op=ALU.mult), reads=['xt', 'rstd'], writes=['xt'])
                        for k in range(16):
                            P.op('act', lambda e, k=k: e.activation(out=hT[:, k, :], in_=xt[:, k, :], func=AF.Identity,
                                                                    scale=A1[:, k:k + 1], bias=SH1[:, k:k + 1]),
                                 reads=['xt', 'A1', 'SH1'], writes=['hT'])
                        if tb == 0:
                            tap('hT', hT[:], 'hT')
                        P.barrier()
                        P.flush()

                    if stages < 2:
                        continue

                    def proj_fm(ci, M=128):
                        w, wk = load_chunk(w_in_l[ci])
                        pb = nps()

                        def f(e):
                            for k in range(16):
                                r = e.matmul(ps[pb][0:M, :], lhsT=w[:, k, 0:M], rhs=hT[:, k, :], start=(k == 0), stop=(k == 15))
                            return r
                        P.op('pe', f, reads=[wk, 'hT'], writes=[('ps', pb)])
                        return pb

                    def shift(pb, ci, out_ap, okey, M=128):
                        i = prn[0] % 2
                        prn[0] += 1
                        pr, pk = praw[i], ('praw', i)
                        P.op('act', lambda e: e.copy(out=pr[0:M, 1:513], in_=ps[pb][0:M, :]), reads=[('ps', pb)], writes=[pk])
                        P.op('dve', lambda e: e.tensor_copy(out=pr[0:M, 0:1], in_=carry[0:M, ci:ci + 1]), reads=['carry'], writes=[pk])
                        P.op('pool', lambda e: e.tensor_scalar(out=shtmp[0:M, :], in0=pr[0:M, 0:512], scalar1=col('mu', ci)[0:M, :],
                                                               scalar2=None, op0=ALU.mult), reads=[pk, 'cols'], writes=['shtmp'])
                        P.op('dve', lambda e: e.scalar_tensor_tensor(out=out_ap, in0=pr[0:M, 1:513], scalar=ommc[0:M, ci:ci + 1],
                                                                     in1=shtmp[0:M, :], op0=ALU.mult, op1=ALU.add),
                             reads=[pk, 'shtmp', 'ommc'], writes=[okey])
                        P.op('dve', lambda e: e.tensor_copy(out=carry[0:M, ci:ci + 1], in_=pr[0:M, 512:513]), reads=[pk], writes=['carry'])

                    with ExitStack() as sC:
                        was = sb(sC, 'was', [128, 512])
                        thw = sb(sC, 'thw', [128, 512])
                        sg0 = sb(sC, 'sg0', [128, 512])
                        sg1 = sb(sC, 'sg1', [128, 512])
                        P.op('pool', lambda e: e.memset(sg1[:], 0.0), writes=['sg1'])
                        pb = proj_fm(CI_WA)
                        shift(pb, 24, was[:], 'was')
                        P.op('act', lambda e: e.activation(out=thw[:], in_=was[:, :], func=AF.Tanh), reads=['was'], writes=['thw'])
                        pb = proj_fm(CI_GD0)
                        shift(pb, 25, sg0[:], 'sg0')
                        P.op('act', lambda e: e.activation(out=sg0[:], in_=sg0[:], func=AF.Sigmoid), reads=['sg0'], writes=['sg0'])
                        pb = proj_fm(CI_GD1, M=32)
                        shift(pb, 26, sg1[0:32, :], 'sg1', M=32)
                        P.op('act', lambda e: e.activation(out=sg1[0:32, :], in_=sg1[0:32, :], func=AF.Sigmoid), reads=['sg1'], writes=['sg1'])

                        names = ['r_t', 'k_t', 'v_t', 'a_t', 'lw', 'g_t', 'cum', 'c_t', 'E1', 'E2', 'kk', 'km', 'b_t', 'bon', 'Bt', 'Kt', 'tmpA', 'tmpB', 'Bt0', 'Bt1', 'Kt0', 'Kt1']
                        Tt = {n: sb(sC, n, [128, 4, 128]) for n in names}
                        KR = sb(sC, 'KR', [128, 4, 2, 128])
                        BKV = sb(sC, 'BKV', [128, 4, 3, 128])
                        eref = sb(sC, 'eref', [128, 4])
                        ecl = sb(sC, 'ecl', [128, 4])
                        Msc = [sb(sC, 'Msc%d' % u, [128, 512]) for u in range(8)]
                        Xb = [[sb(sC, 'X%d_%d' % (u, v), [128, 128]) for v in range(2)] for u in range(8)]
                        XTb = [[sb(sC, 'XT%d_%d' % (u, v), [128, 128]) for v in range(2)] for u in range(8)]
                        Tm = [sb(sC, 'Tm%d' % u, [128, 128]) for u in range(8)]
                        S0p = sb(sC, 'S0p', [128, 128])
                        nG = sb(sC, 'nG', [128, 128])
                        Ut = sb(sC, 'Ut', [128, 128])
                        ytm = sb(sC, 'ytm', [128, 4, 128])
                        ysq = Tt['tmpA']
                        st8 = sb(sC, 'st8', [128, 4, 8])

                        def F2(n):
                            return Tt[n][:].rearrange("p a b -> p (a b)")

                        for j in range(8):
                            jc = slice(j * 128, (j + 1) * 128)
                            pb = nps()
                            P.op('pe', lambda e, pb=pb, jc=jc: e.matmul(ps[pb][:, :], lhsT=a2p[:, jc], rhs=was[:, :], start=True, stop=True),
                                 reads=['a2p', 'was'], writes=[('ps', pb)])
                            P.op('act', lambda e, pb=pb, j=j: e.activation(out=F2('a_t'), in_=ps[pb][:, :], func=AF.Sigmoid, bias=col('a0', j)),
                                 reads=[('ps', pb), 'cols'], writes=['a_t'])
                            pb = nps()
                            P.op('pe', lambda e, pb=pb, jc=jc: e.matmul(ps[pb][:, :], lhsT=w2p[:, jc], rhs=thw[:, :], start=True, stop=True),
                                 reads=['w2p', 'thw'], writes=[('ps', pb)])
                            P.op('act', lambda e, pb=pb, j=j: e.activation(out=F2('lw'), in_=ps[pb][:, :], func=AF.Sigmoid, bias=col('w0', j)),
                                 reads=[('ps', pb), 'cols'], writes=['lw'])
                            P.op('pool', lambda e: e.tensor_scalar(out=F2('lw'), in0=F2('lw'), scalar1=-0.6065306597126334, scalar2=None, op0=ALU.mult),
                                 reads=['lw'], writes=['lw'])
                            pb = nps()

                            def gmm(e, pb=pb, jc=jc):
                                e.matmul(ps[pb][:, :], lhsT=g2a[:, jc], rhs=sg0[:, :], start=True, stop=False)
                                return e.matmul(ps[pb][:, :], lhsT=g2b[:, jc], rhs=sg1[:, :], start=False, stop=True)
                            P.op('pe', gmm, reads=['g2a', 'g2b', 'sg0', 'sg1'], writes=[('ps', pb)])
                            P.op('act', lambda e, pb=pb: e.copy(out=F2('g_t'), in_=ps[pb][:, :]), reads=[('ps', pb)], writes=['g_t'])
                            for nm, ci in (('r_t', j), ('k_t', 8 + j), ('v_t', 16 + j)):
                                pb = proj_fm(ci)
                                shift(pb, ci, F2(nm), nm)
                            if tb == 0 and j == 0:
                                tap('r0', F2('r_t'), 'r_t')
                                tap('a0', F2('a_t'), 'a_t')
                                tap('lw0', F2('lw'), 'lw')
                            for i in range(4):
                                P.op('dve', lambda e, i=i: e.tensor_tensor_scan(out=Tt['cum'][:, i, :], data0=Tt['lw'][:, i, :], data1=zeros,
                                                                               initial=0.0, op0=ALU.add, op1=ALU.add),
                                     reads=['lw', 'cst'], writes=['cum'])
                            for i in range(4):
                                P.op('dve', lambda e, i=i: e.tensor_scalar(out=Tt['c_t'][:, i, :], in0=Tt['cum'][:, i, :], scalar1=Tt['cum'][:, i, 63:64],
                                                                          scalar2=None, op0=ALU.subtract), reads=['cum'], writes=['c_t'])
                            P.op('act', lambda e: e.activation(out=F2('E1'), in_=F2('c_t'), func=AF.Exp, scale=-1.0), reads=['c_t'], writes=['E1'])
                            P.op('act', lambda e: e.activation(out=F2('E2'), in_=F2('c_t'), func=AF.Exp), reads=['c_t'], writes=['E2'])
                            P.op('pool', lambda e: e.tensor_tensor(out=F2('tmpA'), in0=F2('c_t'), in1=F2('lw'), op=ALU.subtract),
                                 reads=['c_t', 'lw'], writes=['tmpA'])
                            P.op('act', lambda e: e.activation(out=F2('tmpA'), in_=F2('tmpA'), func=AF.Exp), reads=['tmpA'], writes=['tmpA'])
                            P.op('act', lambda e: e.activation(out=eref[:], in_=Tt['cum'][:, :, 63], func=AF.Exp), reads=['cum'], writes=['eref'])
                            P.op('act', lambda e: e.activation(out=ecl[:], in_=Tt['c_t'][:, :, 127], func=AF.Exp), reads=['c_t'], writes=['ecl'])
                            P.op('dve', lambda e, j=j: e.tensor_scalar(out=F2('kk'), in0=F2('k_t'), scalar1=col('k_k', j), scalar2=None, op0=ALU.mult),
                                 reads=['k_t', 'cols'], writes=['kk'])
                            P.op('pool', lambda e: e.tensor_tensor(out=F2('tmpB'), in0=F2('kk'), in1=F2('kk'), op=ALU.mult), reads=['kk'], writes=['tmpB'])
                            pb = nps()
                            P.op('pe', lambda e, pb=pb: e.matmul(ps[pb][:, :], lhsT=bd64, rhs=F2('tmpB'), start=True, stop=True),
                                 reads=['cst', 'tmpB'], writes=[('ps', pb)])
                            P.op('dve', lambda e, pb=pb: e.tensor_scalar(out=F2('tmpB'), in0=ps[pb][:, :], scalar1=1e-24, scalar2=None, op0=ALU.max),
                                 reads=[('ps', pb)], writes=['tmpB'])
                            rsqrt(lambda: F2('tmpB'), 'tmpB')
                            P.op('dve', lambda e: e.tensor_tensor(out=F2('kk'), in0=F2('kk'), in1=F2('tmpB'), op=ALU.mult), reads=['kk', 'tmpB'], writes=['kk'])
                            P.op('dve', lambda e, j=j: e.tensor_scalar(out=F2('km'), in0=F2('a_t'), scalar1=-1.0, scalar2=col('k_a', j), op0=ALU.add, op1=ALU.mult),
                                 reads=['a_t', 'cols'], writes=['km'])
                            P.op('dve', lambda e: e.scalar_tensor_tensor(out=F2('km'), in0=F2('km'), scalar=1.0, in1=F2('k_t'), op0=ALU.add, op1=ALU.mult),
                                 reads=['km', 'k_t'], writes=['km'])
                            P.op('pool', lambda e: e.tensor_tensor(out=F2('b_t'), in0=F2('kk'), in1=F2('a_t'), op=ALU.mult), reads=['kk', 'a_t'], writes=['b_t'])
                            P.op('dve', lambda e, j=j: e.scalar_tensor_tensor(out=F2('tmpB'), in0=F2('r_t'), scalar=col('r_k', j), in1=F2('km'), op0=ALU.mult, op1=ALU.mult),
                                 reads=['r_t', 'km', 'cols'], writes=['tmpB'])
                            pb = nps()
                            P.op('pe', lambda e, pb=pb: e.matmul(ps[pb][:, :], lhsT=bd64, rhs=F2('tmpB'), start=True, stop=True),
                                 reads=['cst', 'tmpB'], writes=[('ps', pb)])
                            P.op('dve', lambda e, pb=pb: e.tensor_tensor(out=F2('bon'), in0=ps[pb][:, :], in1=F2('v_t'), op=ALU.mult),
                                 reads=[('ps', pb), 'v_t'], writes=['bon'])
                            P.op('dve', lambda e: e.tensor_tensor(out=F2('Bt'), in0=F2('b_t'), in1=F2('E1'), op=ALU.mult), reads=['b_t', 'E1'], writes=['Bt'])
                            P.op('pool', lambda e: e.tensor_tensor(out=F2('Kt'), in0=F2('km'), in1=F2('E1'), op=ALU.mult), reads=['km', 'E1'], writes=['Kt'])
                            for h2 in range(2):
                                hm = bd64[:, h2 * 64:h2 * 64 + 1]
                                P.op('dve', lambda e, h2=h2, hm=hm: e.tensor_scalar(out=F2('Bt%d' % h2), in0=F2('Bt'), scalar1=hm, scalar2=None, op0=ALU.mult),
                                     reads=['Bt', 'cst'], writes=['Bt%d' % h2])
                                P.op('pool', lambda e, h2=h2, hm=hm: e.tensor_scalar(out=F2('Kt%d' % h2), in0=F2('Kt'), scalar1=hm, scalar2=None, op0=ALU.mult),
                                     reads=['Kt', 'cst'], writes=['Kt%d' % h2])
                            P.op('dve', lambda e: e.tensor_tensor(out=KR[:, :, 0, :], in0=Tt['kk'][:], in1=Tt['tmpA'][:], op=ALU.mult), reads=['kk', 'tmpA'], writes=['KR'])
                            P.op('pool', lambda e: e.tensor_tensor(out=KR[:, :, 1, :], in0=Tt['r_t'][:], in1=Tt['E2'][:], op=ALU.mult), reads=['r_t', 'E2'], writes=['KR'])
                            for i in range(4):
                                pb = nps()

                                def trf(e, pb=pb, i=i):
                                    e.transpose(ps[pb][:, 0:128], Tt['Bt'][:, i, :], ident)
                                    e.transpose(ps[pb][:, 128:256], Tt['Kt'][:, i, :], ident)
                                    return e.transpose(ps[pb][:, 256:384], Tt['v_t'][:, i, :], ident)
                                P.op('pe', trf, reads=['Bt', 'Kt', 'v_t', 'cst'], writes=[('ps', pb)])
                                P.op('act', lambda e, pb=pb, i=i: e.copy(out=BKV[:, i, :, :].rearrange("p a b -> p (a b)"), in_=ps[pb][:, 0:384]),
                                     reads=[('ps', pb)], writes=[('BKV', i)])
                            for i in range(4):
                                for h2 in range(2):
                                    u = i * 2 + h2
                                    hs = slice(h2 * 64, h2 * 64 + 64)
                                    pb = nps()

                                    def scf(e, pb=pb, i=i, h2=h2):
                                        krr = KR[:, i, :, :].rearrange("p a b -> p (a b)")
                                        e.matmul(ps[pb][:, 0:256], lhsT=Tt['Bt%d' % h2][:, i, :], rhs=krr, start=True, stop=True)
                                        return e.matmul(ps[pb][:, 256:512], lhsT=Tt['Kt%d' % h2][:, i, :], rhs=krr, start=True, stop=True)
                                    P.op('pe', scf, reads=['Bt%d' % h2, 'Kt%d' % h2, 'KR'], writes=[('ps', pb)])
                                    P.op('dve', lambda e, pb=pb, u=u: e.tensor_tensor(out=Msc[u][:], in0=ps[pb][:, :], in1=mask4, op=ALU.mult),
                                         reads=[('ps', pb), 'cst'], writes=[('Msc', u)])
                                    pb = nps()
                                    P.op('pe', lambda e, pb=pb, i=i, h2=h2: e.matmul(ps[pb][:, 0:128], lhsT=KR[:, i, 0, :], rhs=Tt['Bt%d' % h2][:, i, :], start=True, stop=True),
                                         reads=['Bt%d' % h2, 'KR'], writes=[('ps', pb)])
                                    P.op('dve', lambda e, pb=pb, u=u: e.tensor_tensor(out=XTb[u][0][:], in0=ps[pb][:, 0:128], in1=strictL, op=ALU.mult),
                                         reads=[('ps', pb), 'cst'], writes=[('XT', u, 0)])
                                    P.op('pool', lambda e, u=u: e.tensor_copy(out=Xb[u][0][:], in_=Msc[u][:, 0:128]), reads=[('Msc', u)], writes=[('X', u, 0)])
                                    P.op('pool', lambda e, u=u: e.tensor_tensor(out=Tm[u][:], in0=ident, in1=Msc[u][:, 0:128], op=ALU.subtract),
                                         reads=[('Msc', u), 'cst'], writes=[('Tm', u)])
                            for lvl in range(6):
                                a_, b_ = lvl % 2, (lvl + 1) % 2
                                for u in range(8):
                                    pbB = nps()
                                    P.op('pe', lambda e, pbB=pbB, u=u, a_=a_: e.matmul(ps[pbB][:, 0:128], lhsT=Xb[u][a_][:], rhs=XTb[u][a_][:], start=True, stop=True),
                                         reads=[('X', u, a_), ('XT', u, a_)], writes=[('ps', pbB)])
                                    P.op('act', lambda e, pbB=pbB, u=u, b_=b_: e.copy(out=XTb[u][b_][:], in_=ps[pbB][:, 0:128]),
                                         reads=[('ps', pbB)], writes=[('XT', u, b_)])
                                    if lvl < 5:
                                        pbA = nps()
                                        P.op('pe', lambda e, pbA=pbA, u=u, a_=a_: e.matmul(ps[pbA][:, 0:128], lhsT=XTb[u][a_][:], rhs=Xb[u][a_][:], start=True, stop=True),
                                             reads=[('X', u, a_), ('XT', u, a_)], writes=[('ps', pbA)])
                                        P.op('act', lambda e, pbA=pbA, u=u, b_=b_: e.copy(out=Xb[u][b_][:], in_=ps[pbA][:, 0:128]),
                                             reads=[('ps', pbA)], writes=[('X', u, b_)])
                                for u in range(8):
                                    pb = nps()
                                    P.op('pe', lambda e, pb=pb, u=u, b_=b_: e.matmul(ps[pb][:, 0:128], lhsT=XTb[u][b_][:], rhs=Tm[u][:], start=True, stop=True),
                                         reads=[('XT', u, b_), ('Tm', u)], writes=[('ps', pb)])
                                    P.op('dve', lambda e, pb=pb, u=u: e.tensor_tensor(out=Tm[u][:], in0=ps[pb][:, 0:128], in1=Tm[u][:], op=ALU.add),
                                         reads=[('ps', pb), ('Tm', u)], writes=[('Tm', u)])
                            Sj = S_rw[:, j, :]
                            for i in range(4):
                                P.op('dve', lambda e, i=i, Sj=Sj: e.tensor_scalar(out=S0p[:], in0=Sj, scalar1=eref[:, i:i + 1], scalar2=None, op0=ALU.mult),
                                     reads=['S_rw', 'eref'], writes=['S0p'])
                                pb = nps()

                                def gf(e, pb=pb, i=i):
                                    e.matmul(ps[pb][:, 0:128], lhsT=KR[:, i, 0, :], rhs=S0p[:], start=True, stop=False)
                                    for h2 in range(2):
                                        r = e.matmul(ps[pb][:, h2 * 64:h2 * 64 + 64], lhsT=Msc[i * 2 + h2][:, 256:384],
                                                     rhs=BKV[:, i, 2, h2 * 64:h2 * 64 + 64], start=False, stop=(h2 == 1))
                                    return r
                                P.op('pe', gf, reads=['KR', 'S0p', ('Msc', 2 * i), ('Msc', 2 * i + 1), ('BKV', i)], writes=[('ps', pb)])
                                P.op('dve', lambda e, pb=pb: e.tensor_scalar(out=nG[:], in0=ps[pb][:, 0:128], scalar1=-1.0, scalar2=None, op0=ALU.mult),
                                     reads=[('ps', pb)], writes=['nG'])
                                pb = nps()

                                def uf(e, pb=pb, i=i):
                                    for h2 in range(2):
                                        r = e.matmul(ps[pb][:, h2 * 64:h2 * 64 + 64], lhsT=Tm[i * 2 + h2][:], rhs=nG[:, h2 * 64:h2 * 64 + 64], start=True, stop=True)
                                    return r
                                P.op('pe', uf, reads=[('Tm', 2 * i), ('Tm', 2 * i + 1), 'nG'], writes=[('ps', pb)])
                                P.op('act', lambda e, pb=pb: e.copy(out=Ut[:], in_=ps[pb][:, 0:128]), reads=[('ps', pb)], writes=['Ut'])
                                pb = nps()

                                def yf(e, pb=pb, i=i):
                                    e.matmul(ps[pb][:, 0:128], lhsT=KR[:, i, 1, :], rhs=S0p[:], start=True, stop=False)
                                    for h2 in range(2):
                                        cs = slice(h2 * 64, h2 * 64 + 64)
                                        e.matmul(ps[pb][:, cs], lhsT=Msc[i * 2 + h2][:, 128:256], rhs=Ut[:, cs], start=False, stop=False)
                                        r = e.matmul(ps[pb][:, cs], lhsT=Msc[i * 2 + h2][:, 384:512], rhs=BKV[:, i, 2, cs], start=False, stop=(h2 == 1))
                                    return r
                                P.op('pe', yf, reads=['KR', 'S0p', ('Msc', 2 * i), ('Msc', 2 * i + 1), 'Ut', ('BKV', i)], writes=[('ps', pb)])
                                P.op('act', lambda e, pb=pb, i=i: e.copy(out=ytm[:, i, :], in_=ps[pb][:, 0:128]), reads=[('ps', pb)], writes=[('ytm', i)])
                                pb = nps()

                                def sf(e, pb=pb, i=i):
                                    e.matmul(ps[pb][:, 0:128], lhsT=ident, rhs=S0p[:], start=True, stop=False)
                                    e.matmul(ps[pb][:, 0:128], lhsT=BKV[:, i, 0, :], rhs=Ut[:], start=False, stop=False)
                                    return e.matmul(ps[pb][:, 0:128], lhsT=BKV[:, i, 1, :], rhs=BKV[:, i, 2, :], start=False, stop=True)
                                P.op('pe', sf, reads=['cst', 'S0p', ('BKV', i), 'Ut'], writes=[('ps', pb)])
                                P.op('dve', lambda e, pb=pb, i=i, Sj=Sj: e.scalar_tensor_tensor(out=Sj, in0=ps[pb][:, 0:128], scalar=ecl[:, i:i + 1], in1=bd64,
                                                                                             op0=ALU.mult, op1=ALU.mult),
                                     reads=[('ps', pb), 'ecl', 'cst'], writes=['S_rw'])
                            if tb == 0 and j == 0:
                                tap('y0', ytm[:], ('ytm', 3))
                            yk = [('ytm', i) for i in range(4)]
                            yv = ytm[:].rearrange("p a (g n) -> p (a g) n", g=2)
                            P.op('pool', lambda e: e.tensor_tensor(out=ysq[:], in0=ytm[:], in1=ytm[:], op=ALU.mult), reads=yk, writes=['ysq'])
                            P.op('dve', lambda e: e.reduce_sum(out=st8[:, 0, :], in_=yv, axis=AX.X), reads=yk, writes=['st8'])
                            P.op('dve', lambda e: e.reduce_sum(out=st8[:, 1, :], in_=ysq[:].rearrange("p a (g n) -> p (a g) n", g=2), axis=AX.X),
                                 reads=['ysq'], writes=['st8'])
                            P.op('dve', lambda e: e.tensor_scalar(out=st8[:, 2, :], in0=st8[:, 0, :], scalar1=1.0 / 64, scalar2=None, op0=ALU.mult),
                                 reads=['st8'], writes=['st8'])
                            P.op('dve', lambda e: e.tensor_tensor(out=st8[:, 0, :], in0=st8[:, 2, :], in1=st8[:, 2, :], op=ALU.mult), reads=['st8'], writes=['st8'])
                            P.op('dve', lambda e: e.scalar_tensor_tensor(out=st8[:, 3, :], in0=st8[:, 1, :], scalar=1.0 / 64, in1=st8[:, 0, :],
                                                                         op0=ALU.mult, op1=ALU.subtract), reads=['st8'], writes=['st8'])
                            P.op('dve', lambda e: e.tensor_scalar(out=st8[:, 3, :], in0=st8[:, 3, :], scalar1=64e-5, scalar2=None, op0=ALU.add),
                                 reads=['st8'], writes=['st8'])
                            rsqrt(lambda: st8[:, 3, :], 'st8')
                            for g in range(8):
                                P.op('dve', lambda e, g=g: e.tensor_scalar(out=ytm[:, g // 2, (g % 2) * 64:(g % 2) * 64 + 64],
                                                                          in0=ytm[:, g // 2, (g % 2) * 64:(g % 2) * 64 + 64],
                                                                          scalar1=st8[:, 2, g:g + 1], scalar2=st8[:, 3, g:g + 1],
                                                                          op0=ALU.subtract, op1=ALU.mult),
                                     reads=yk + ['st8'], writes=yk)
                            pb = nps()

                            def ytr(e, pb=pb):
                                for i in range(4):
                                    r = e.transpose(ps[pb][:, i * 128:(i + 1) * 128], ytm[:, i, :], ident)
                                return r
                            P.op('pe', ytr, reads=yk + ['cst'], writes=[('ps', pb)])
                            P.op('act', lambda e, pb=pb, j=j: e.activation(out=F2('tmpB'), in_=ps[pb][:, :], func=AF.Identity,
                                                                           scale=col('gn_g', j), bias=col('gn_b', j)),
                                 reads=[('ps', pb), 'cols'], writes=['tmpB'])
                            P.op('dve', lambda e: e.tensor_tensor(out=F2('tmpB'), in0=F2('tmpB'), in1=F2('bon'), op=ALU.add), reads=['tmpB', 'bon'], writes=['tmpB'])
                            P.op('dve', lambda e, j=j: e.tensor_tensor(out=yaT[:, j, :], in0=F2('tmpB'), in1=F2('g_t'), op=ALU.mult),
                                 reads=['tmpB', 'g_t'], writes=['yaT'])
                        if tb == 0:
                            tap('yaT', yaT[:], 'yaT')
                        P.barrier()
                        P.flush()

                    if stages < 3:
                        continue
                    with ExitStack() as sD:
                        names = ['q_t', 'fg', 'lf', 'kf', 'cum', 'c_t', 'Eq', 'Ek', 'qd', 'kd', 'vtm', 'sog', 'otm', 'osq', 'kdtm']
                        Tt = {n: sb(sD, 'h_' + n, [128, 4, 128]) for n in names}
                        eref = sb(sD, 'h_eref', [128, 4])
                        ecl = sb(sD, 'h_ecl', [128, 4])
                        AT = sb(sD, 'h_AT', [128, 128])
                        S0p = sb(sD, 'h_S0p', [128, 128])
                        rs4 = sb(sD, 'h_rs4', [128, 4])

                        def F2(n):
                            return Tt[n][:].rearrange("p a b -> p (a b)")

                        for h in range(8):
                            pb = proj_fm(CI_HQ + h)
                            P.op('act', lambda e, pb=pb: e.activation(out=F2('q_t'), in_=ps[pb][:, :], func=AF.Silu), reads=[('ps', pb)], writes=['q_t'])
                            pb = proj_fm(CI_HF + h)
                            P.op('act', lambda e, pb=pb: e.activation(out=F2('fg'), in_=ps[pb][:, :], func=AF.Sigmoid), reads=[('ps', pb)], writes=['fg'])
                            P.op('dve', lambda e, h=h: e.tensor_scalar(out=F2('fg'), in0=F2('fg'), scalar1=omlc[:, h:h + 1], scalar2=lbc[:, h:h + 1],
                                                                      op0=ALU.mult, op1=ALU.add), reads=['fg', 'omlc', 'lbc'], writes=['fg'])
                            P.op('act', lambda e: e.activation(out=F2('lf'), in_=F2('fg'), func=AF.Ln), reads=['fg'], writes=['lf'])
                            P.op('pool', lambda e: e.tensor_scalar(out=F2('kf'), in0=F2('fg'), scalar1=-1.0, scalar2=1.0, op0=ALU.mult, op1=ALU.add),
                                 reads=['fg'], writes=['kf'])
                            for i in range(4):
                                P.op('dve', lambda e, i=i: e.tensor_tensor_scan(out=Tt['cum'][:, i, :], data0=Tt['lf'][:, i, :], data1=zeros,
                                                                               initial=0.0, op0=ALU.add, op1=ALU.add), reads=['lf', 'cst'], writes=['cum'])
                            for i in range(4):
                                P.op('dve', lambda e, i=i: e.tensor_scalar(out=Tt['c_t'][:, i, :], in0=Tt['cum'][:, i, :], scalar1=Tt['cum'][:, i, 63:64],
                                                                          scalar2=None, op0=ALU.subtract), reads=['cum'], writes=['c_t'])
                            P.op('act', lambda e: e.activation(out=ecl[:], in_=Tt['c_t'][:, :, 127], func=AF.Exp), reads=['c_t'], writes=['h_ecl'])
                            P.op('act', lambda e: e.activation(out=eref[:], in_=Tt['cum'][:, :, 63], func=AF.Exp), reads=['cum'], writes=['h_eref'])
                            P.op('dve', lambda e: e.tensor_scalar(out=F2('c_t'), in0=F2('c_t'), scalar1=-40.0, scalar2=40.0, op0=ALU.max, op1=ALU.min),
                                 reads=['c_t', 'h_ecl'], writes=['c_t'])
                            P.op('act', lambda e: e.activation(out=F2('Eq'), in_=F2('c_t'), func=AF.Exp), reads=['c_t'], writes=['Eq'])
                            P.op('act', lambda e: e.activation(out=F2('Ek'), in_=F2('c_t'), func=AF.Exp, scale=-1.0), reads=['c_t'], writes=['Ek'])
                            P.op('dve', lambda e: e.tensor_tensor(out=F2('qd'), in0=F2('q_t'), in1=F2('Eq'), op=ALU.mult), reads=['q_t', 'Eq'], writes=['qd'])
                            P.op('pool', lambda e: e.tensor_tensor(out=F2('kd'), in0=F2('kf'), in1=F2('Ek'), op=ALU.mult), reads=['kf', 'Ek'], writes=['kd'])
                            for nm, cbase, fn in (('vtm', CI_HI, None), ('sog', CI_HO, AF.Silu)):
                                w, wk = load_chunk(w_in_l[cbase + h])
                                pb = nps()

                                def tmf(e, pb=pb, w=w):
                                    for i in range(4):
                                        for k in range(16):
                                            r = e.matmul(ps[pb][:, i * 128:(i + 1) * 128], lhsT=hT[:, k, i * 128:(i + 1) * 128], rhs=w[:, k, :],
                                                         start=(k == 0), stop=(k == 15))
                                    return r
                                P.op('pe', tmf, reads=[wk, 'hT'], writes=[('ps', pb)])
                                if fn is None:
                                    P.op('act', lambda e, pb=pb, nm=nm: e.copy(out=F2(nm), in_=ps[pb][:, :]), reads=[('ps', pb)], writes=[nm])
                                else:
                                    P.op('act', lambda e, pb=pb, nm=nm, fn=fn: e.activation(out=F2(nm), in_=ps[pb][:, :], func=fn), reads=[('ps', pb)], writes=[nm])
                            pb = nps()

                            def ktr(e, pb=pb):
                                for i in range(4):
                                    r = e.transpose(ps[pb][:, i * 128:(i + 1) * 128], Tt['kd'][:, i, :], ident)
                                return r
                            P.op('pe', ktr, reads=['kd', 'cst'], writes=[('ps', pb)])
                            P.op('act', lambda e, pb=pb: e.copy(out=F2('kdtm'), in_=ps[pb][:, :]), reads=[('ps', pb)], writes=['kdtm'])
                            Sh = S_hg[:, h, :]
                            for i in range(4):
                                pb = nps()
                                P.op('pe', lambda e, pb=pb, i=i: e.matmul(ps[pb][:, 0:128], lhsT=Tt['kd'][:, i, :], rhs=Tt['qd'][:, i, :], start=True, stop=True),
                                     reads=['kd', 'qd'], writes=[('ps', pb)])
                                P.op('dve', lambda e, pb=pb: e.tensor_tensor(out=AT[:], in0=ps[pb][:, 0:128], in1=inclU, op=ALU.mult),
                                     reads=[('ps', pb), 'cst'], writes=['h_AT'])
                                P.op('dve', lambda e, i=i, Sh=Sh: e.tensor_scalar(out=S0p[:], in0=Sh, scalar1=eref[:, i:i + 1], scalar2=None, op0=ALU.mult),
                                     reads=['S_hg', 'h_eref'], writes=['h_S0p'])
                                pb = nps()

                                def of(e, pb=pb, i=i):
                                    e.matmul(ps[pb][:, 0:128], lhsT=AT[:], rhs=Tt['vtm'][:, i, :], start=True, stop=False)
                                    return e.matmul(ps[pb][:, 0:128], lhsT=Tt['qd'][:, i, :], rhs=S0p[:], start=False, stop=True)
                                P.op('pe', of, reads=['h_AT', 'vtm', 'qd', 'h_S0p'], writes=[('ps', pb)])
                                P.op('act', lambda e, pb=pb, i=i: e.copy(out=Tt['otm'][:, i, :], in_=ps[pb][:, 0:128]), reads=[('ps', pb)], writes=['otm'])
                                pb = nps()

                                def sf(e, pb=pb, i=i):
                                    e.matmul(ps[pb][:, 0:128], lhsT=ident, rhs=S0p[:], start=True, stop=False)
                                    return e.matmul(ps[pb][:, 0:128], lhsT=Tt['kdtm'][:, i, :], rhs=Tt['vtm'][:, i, :], start=False, stop=True)
                                P.op('pe', sf, reads=['cst', 'h_S0p', 'kdtm', 'vtm'], writes=[('ps', pb)])
                                P.op('dve', lambda e, pb=pb, i=i, Sh=Sh: e.tensor_scalar(out=Sh, in0=ps[pb][:, 0:128], scalar1=ecl[:, i:i + 1], scalar2=None, op0=ALU.mult),
                                     reads=[('ps', pb), 'h_ecl'], writes=['S_hg'])
                            if tb == 0 and h == 0:
                                tap('o0', Tt['otm'][:], 'otm')
                            P.op('pool', lambda e: e.tensor_tensor(out=F2('osq'), in0=F2('otm'), in1=F2('otm'), op=ALU.mult), reads=['otm'], writes=['osq'])
                            P.op('dve', lambda e: e.reduce_sum(out=rs4[:], in_=Tt['osq'][:], axis=AX.X), reads=['osq'], writes=['h_rs4'])
                            P.op('dve', lambda e: e.tensor_scalar(out=rs4[:], in0=rs4[:], scalar1=1.0 / 128, scalar2=1e-5, op0=ALU.mult, op1=ALU.add),
                                 reads=['h_rs4'], writes=['h_rs4'])
                            rsqrt(lambda: rs4[:], 'h_rs4')
                            for i in range(4):
                                P.op('dve', lambda e, i=i: e.scalar_tensor_tensor(out=Tt['otm'][:, i, :], in0=Tt['otm'][:, i, :], scalar=rs4[:, i:i + 1],
                                                                                 in1=Tt['sog'][:, i, :], op0=ALU.mult, op1=ALU.mult),
                                     reads=['otm', 'h_rs4', 'sog'], writes=['otm'])
                            pb = nps()

                            def otr(e, pb=pb):
                                for i in range(4):
                                    r = e.transpose(ps[pb][:, i * 128:(i + 1) * 128], Tt['otm'][:, i, :], ident)
                                return r
                            P.op('pe', otr, reads=['otm', 'cst'], writes=[('ps', pb)])
                            P.op('act', lambda e, pb=pb, h=h: e.activation(out=ybT[:, h, :], in_=ps[pb][:, :], func=AF.Identity, scale=col('hgn', 0)),
                                 reads=[('ps', pb), 'cols'], writes=['ybT'])
                        if tb == 0:
                            tap('ybT', ybT[:], 'ybT')
                        P.barrier()
                        P.flush()

                    if stages < 4:
                        continue
                    with ExitStack() as sE:
                        mgT = sb(sE, 'mgT', [128, 16, 512], BF16)
                        sga = sb(sE, 'sga', [128, 512])
                        sgb = sb(sE, 'sgb', [128, 512])
                        tE = sb(sE, 'tE', [128, 512])
                        xb = sb(sE, 'xb', [128, 4, D])
                        g1bc = sb(sE, 'g1bc', [128, D])
                        A2bc = sb(sE, 'A2bc', [128, D])
                        sh2bc = sb(sE, 'sh2bc', [128, D])
                        P.dma('sp', lambda e: e.dma_start(out=g1bc[:], in_=modrow[0:1, 0:D].to_broadcast([128, D])), reads=['modrow'], writes=['g1bc'])
                        P.dma('sp', lambda e: e.dma_start(out=sh2bc[:], in_=modrow[0:1, D:2 * D].to_broadcast([128, D])), reads=['modrow'], writes=['sh2bc'])
                        P.dma('sp', lambda e: e.dma_start(out=A2bc[:], in_=modrow[0:1, 2 * D:3 * D].to_broadcast([128, D])), reads=['modrow'], writes=['A2bc'])
                        P.dma('sp', lambda e: e.dma_start(out=xb[:], in_=x_in[t0_:t0_ + 512, :].rearrange("(a p) d -> p a d", p=128)), writes=['xb'])
                        for dc in range(16):
                            pb = proj_fm(CI_GA + dc)
                            P.op('act', lambda e, pb=pb: e.activation(out=sga[:], in_=ps[pb][:, :], func=AF.Sigmoid), reads=[('ps', pb)], writes=['sga'])
                            pb = proj_fm(CI_GB + dc)
                            P.op('act', lambda e, pb=pb: e.activation(out=sgb[:], in_=ps[pb][:, :], func=AF.Sigmoid), reads=[('ps', pb)], writes=['sgb'])
                            for src_l, yT, sgt, first in ((pa_l, yaT, sga, True), (pb_l, ybT, sgb, False)):
                                w, wk = load_chunk(src_l[dc], kdim=8)
                                pb = nps()

                                def zf(e, pb=pb, w=w, yT=yT):
                                    for k in range(8):
                                        r = e.matmul(ps[pb][:, :], lhsT=w[:, k, :], rhs=yT[:, k, :], start=(k == 0), stop=(k == 7))
                                    return r
                                P.op('pe', zf, reads=[wk, 'yaT', 'ybT'], writes=[('ps', pb)])
                                if first:
                                    P.op('dve', lambda e, pb=pb: e.tensor_tensor(out=tE[:], in0=ps[pb][:, :], in1=sga[:], op=ALU.mult),
                                         reads=[('ps', pb), 'sga'], writes=['tE'])
                                else:
                                    P.op('dve', lambda e, pb=pb: e.tensor_tensor(out=sgb[:], in0=ps[pb][:, :], in1=sgb[:], op=ALU.mult),
                                         reads=[('ps', pb), 'sgb'], writes=['sgb'])
                            P.op('pool', lambda e, dc=dc: e.tensor_tensor(out=mgT[:, dc, :], in0=tE[:], in1=sgb[:], op=ALU.add),
                                 reads=['tE', 'sgb'], writes=['mgT'])
                        if tb == 0:
                            tap('mgT', mgT[:], 'mgT')
                        for dc in range(16):
                            w, wk = load_chunk(wout_l[dc])
                            pb = nps()

                            def mf(e, pb=pb, w=w):
                                for i in range(4):
                                    for k in range(16):
                                        r = e.matmul(ps[pb][:, i * 128:(i + 1) * 128], lhsT=mgT[:, k, i * 128:(i + 1) * 128], rhs=w[:, k, :],
                                                     start=(k == 0), stop=(k == 15))
                                return r
                            P.op('pe', mf, reads=[wk, 'mgT'], writes=[('ps', pb)])
                            dsl = slice(dc * 128, (dc + 1) * 128)
                            P.op('dve', lambda e, pb=pb, dsl=dsl: e.tensor_tensor(out=tE[:].rearrange("p (a b) -> p a b", a=4),
                                                                                 in0=ps[pb][:, :].rearrange("p (a b) -> p a b", a=4),
                                                                                 in1=g1bc[:, dsl].rearrange("p (a b) -> p a b", a=1).to_broadcast([128, 4, 128]),
                                                                                 op=ALU.mult),
                                 reads=[('ps', pb), 'g1bc'], writes=['tE'])
                            P.op('pool', lambda e, dsl=dsl: e.tensor_tensor(out=xb[:, :, dsl], in0=xb[:, :, dsl], in1=tE[:].rearrange("p (a b) -> p a b", a=4), op=ALU.add),
                                 reads=['tE', 'xb'], writes=['xb'])
                        P.dma('sp', lambda e: e.dma_start(out=x1s[t0_:t0_ + 512, :].rearrange("(a p) d -> p a d", p=128), in_=xb[:]), reads=['xb'], writes=['x1s'])
                        if tb == 0:
                            tap('x1', xb[:], 'xb')
                        junk = sb(sE, 'junk', [128, D], BF16)
                        h2f = sb(sE, 'h2f', [128, D])
                        h2b = [sb(sE, 'h2b%d' % i, [128, D], BF16) for i in range(2)]
                        h2T = sb(sE, 'h2T', [128, 16, 128])
                        ss1 = sb(sE, 'ss1', [128, 1])
                        lg = sb(sE, 'lg', [128, 32])
                        mx8 = sb(sE, 'mx8', [128, 8])
                        msk = sb(sE, 'msk', [128, 32])
                        ex = sb(sE, 'ex', [128, 32])
                        den = sb(sE, 'den', [128, 1])
                        nmx = sb(sE, 'nmx', [128, 1])
                        vv = sb(sE, 'vv', [128, 32])
                        v8 = sb(sE, 'v8', [128, 8])
                        sel = sb(sE, 'sel', [128, 32])
                        for i in range(4):
                            it = tb * 4 + i
                            xt1 = xb[:, i, :]
                            P.op('act', lambda e, xt1=xt1: e.activation(out=junk[:], in_=xt1, func=AF.Square, accum_out=ss1[:]), reads=['xb'], writes=['junk', 'ss1'])
                            P.op('dve', lambda e: e.tensor_scalar(out=ss1[:], in0=ss1[:], scalar1=1.0 / D, scalar2=1e-5, op0=ALU.mult, op1=ALU.add), reads=['ss1'], writes=['ss1'])
                            rsqrt(lambda: ss1[:], 'ss1')
                            P.op('dve', lambda e, xt1=xt1: e.scalar_tensor_tensor(out=h2f[:], in0=xt1, scalar=ss1[:, 0:1], in1=A2bc[:], op0=ALU.mult, op1=ALU.mult),
                                 reads=['xb', 'ss1', 'A2bc'], writes=['h2f'])
                            P.op('pool', lambda e: e.tensor_tensor(out=h2f[:], in0=h2f[:], in1=sh2bc[:], op=ALU.add), reads=['h2f', 'sh2bc'], writes=['h2f'])
                            hb, hbk = h2b[it % 2], ('h2b', it % 2)
                            P.op('act', lambda e, hb=hb: e.copy(out=hb[:], in_=h2f[:]), reads=['h2f'], writes=[hbk])
                            if it == 0:
                                tap('h2', h2f[:], 'h2f')
                            for q4 in range(4):
                                pb = nps()

                                def htr(e, pb=pb, q4=q4):
                                    for c4 in range(4):
                                        k = q4 * 4 + c4
                                        r = e.transpose(ps[pb][:, c4 * 128:(c4 + 1) * 128], h2f[:, k * 128:(k + 1) * 128], ident)
                                    return r
                                P.op('pe', htr, reads=['h2f', 'cst'], writes=[('ps', pb)])
                                P.op('act', lambda e, pb=pb, q4=q4: e.copy(out=h2T[:, q4 * 4:(q4 + 1) * 4, :].rearrange("p a b -> p (a b)"), in_=ps[pb][:, :]),
                                     reads=[('ps', pb)], writes=[('h2T', q4)])
                            pb = nps()

                            def lgf(e, pb=pb):
                                for k in range(16):
                                    r = e.matmul(ps[pb][:, 0:32], lhsT=h2T[:, k, :], rhs=rwt[:, k, :], start=(k == 0), stop=(k == 15))
                                return r
                            P.op('pe', lgf, reads=[('h2T', q) for q in range(4)] + ['rwt'], writes=[('ps', pb)])
                            P.op('dve', lambda e, pb=pb: e.tensor_tensor(out=lg[:], in0=ps[pb][:, 0:32], in1=rbbc[:], op=ALU.add), reads=[('ps', pb), 'rbbc'], writes=['lg'])
                            if it == 0:
                                tap('lg', lg[:], 'lg')
                            P.op('dve', lambda e: e.max(out=mx8[:], in_=lg[:]), reads=['lg'], writes=['mx8'])
                            P.op('dve', lambda e: e.tensor_scalar(out=msk[:], in0=lg[:], scalar1=mx8[:, 3:4], scalar2=None, op0=ALU.is_ge), reads=['lg', 'mx8'], writes=['msk'])
                            P.op('dve', lambda e: e.tensor_scalar(out=nmx[:], in0=mx8[:, 0:1], scalar1=-1.0, scalar2=None, op0=ALU.mult), reads=['mx8'], writes=['nmx'])
                            P.op('act', lambda e: e.activation(out=ex[:], in_=lg[:], func=AF.Exp, bias=nmx[:, 0:1]), reads=['lg', 'nmx'], writes=['ex'])
                            P.op('dve', lambda e: e.tensor_tensor(out=ex[:], in0=ex[:], in1=msk[:], op=ALU.mult), reads=['ex', 'msk'], writes=['ex'])
                            P.op('dve', lambda e: e.reduce_sum(out=den[:], in_=ex[:], axis=AX.X), reads=['ex'], writes=['den'])
                            P.op('dve', lambda e: e.reciprocal(out=den[:], in_=den[:]), reads=['den'], writes=['den'])
                            P.op('dve', lambda e: e.tensor_scalar(out=ex[:], in0=ex[:], scalar1=den[:, 0:1], scalar2=None, op0=ALU.mult), reads=['ex', 'den'], writes=['ex'])
                            P.op('dve', lambda e, it=it: e.tensor_copy(out=maskb_all[:, it, :], in_=msk[:]), reads=['msk'], writes=[('maskb', it)])
                            pb = nps()

                            def pf(e, pb=pb, it=it):
                                for jt in range(it):
                                    e.matmul(ps[pb][:, 0:32], lhsT=ones_bf[:], rhs=maskb_all[:, jt, :], start=(jt == 0), stop=False)
                                return e.matmul(ps[pb][:, 0:32], lhsT=triS_bf[:], rhs=maskb_all[:, it, :], start=(it == 0), stop=True)
                            P.op('pe', pf, reads=[('maskb', jt) for jt in range(it + 1)] + ['ones_bf', 'triS_bf'], writes=[('ps', pb)])
                            P.op('dve', lambda e, pb=pb: e.tensor_tensor(out=vv[:], in0=ps[pb][:, 0:32], in1=ecap1, op=ALU.add), reads=[('ps', pb), 'cst'], writes=['vv'])
                            P.op('dve', lambda e: e.tensor_tensor(out=vv[:], in0=vv[:], in1=msk[:], op=ALU.mult), reads=['vv', 'msk'], writes=['vv'])
                            P.op('dve', lambda e: e.max(out=v8[:], in_=vv[:]), reads=['vv'], writes=['v8'])
                            for k4 in range(4):
                                P.op('dve', lambda e, k4=k4: e.tensor_scalar(out=sel[:], in0=vv[:], scalar1=v8[:, k4:k4 + 1], scalar2=None, op0=ALU.is_equal),
                                     reads=['vv', 'v8'], writes=['sel'])
                                P.op('dve', lambda e: e.tensor_tensor(out=sel[:], in0=sel[:], in1=ex[:], op=ALU.mult), reads=['sel', 'ex'], writes=['sel'])
                                P.op('dve', lambda e, k4=k4, it=it: e.reduce_sum(out=g4_all[:, it, k4:k4 + 1], in_=sel[:], axis=AX.X), reads=['sel'], writes=['g4_all'])
                            P.op('dve', lambda e: e.tensor_scalar(out=v8[:, 0:4], in0=v8[:, 0:4], scalar1=-1.0, scalar2=None, op0=ALU.add), reads=['v8'], writes=['v8'])
                            P.op('dve', lambda e, it=it: e.tensor_copy(out=idx4_all[:, it, :], in_=v8[:, 0:4]), reads=['v8'], writes=['idx4_all'])
                            for k4 in range(4):
                                P.dma('pool', lambda e, hb=hb, it=it, k4=k4: e.indirect_dma_start(
                                    out=Xg, out_offset=bass.IndirectOffsetOnAxis(ap=idx4_all[:, it, k4:k4 + 1].bitcast(U32), axis=0),
                                    in_=hb[:], in_offset=None), reads=[hbk, 'idx4_all'], writes=['Xg'])
                        if tb == 0:
                            tap('idx4', idx4_all[:, 0:4, :], 'idx4_all')
                            tap('g4', g4_all[:, 0:4, :], 'g4_all')
                        P.barrier()
                        P.flush()
            if stages >= 5:
                with ExitStack() as sT:
                    cnt = sb(sT, 'cnt', [128, 32])
                    nbt = sb(sT, 'nbt', [128, 32])
                    tq = sb(sT, 'tq', [128, 32])
                    cum = sb(sT, 'cumx', [128, 32])
                    excl = sb(sT, 'excl', [128, 32])
                    oh = sb(sT, 'oh', [128, 32])
                    r3 = sb(sT, 'r3', [128, 3])
                    SPt = sb(sT, 'SPt', [128, NPASS])
                    pb = nps()

                    def cf(e, pb=pb):
                        for jt in range(NT):
                            r = e.matmul(ps[pb][:, 0:32], lhsT=ones_bf[:], rhs=maskb_all[:, jt, :], start=(jt == 0), stop=(jt == NT - 1))
                        return r
                    P.op('pe', cf, reads=[('maskb', jt) for jt in range(NT)] + ['ones_bf'], writes=[('ps', pb)])
                    P.op('dve', lambda e, pb=pb: e.tensor_copy(out=cnt[:], in_=ps[pb][:, 0:32]), reads=[('ps', pb)], writes=['cnt'])
                    P.op('dve', lambda e: e.tensor_scalar(out=nbt[:], in0=cnt[:], scalar1=0.5, scalar2=None, op0=ALU.is_gt), reads=['cnt'], writes=['nbt'])
                    for jq in range(1, CAP // SUB):
                        P.op('dve', lambda e, jq=jq: e.tensor_scalar(out=tq[:], in0=cnt[:], scalar1=jq * SUB + 0.5, scalar2=None, op0=ALU.is_gt), reads=['cnt'], writes=['tq'])
                        P.op('dve', lambda e: e.tensor_tensor(out=nbt[:], in0=nbt[:], in1=tq[:], op=ALU.add), reads=['nbt', 'tq'], writes=['nbt'])
                    P.op('dve', lambda e: e.tensor_tensor_scan(out=cum[:], data0=nbt[:], data1=zeros[:, 0:32], initial=0.0, op0=ALU.add, op1=ALU.add),
                         reads=['nbt', 'cst'], writes=['cumx'])
                    P.op('dve', lambda e: e.tensor_tensor(out=excl[:], in0=cum[:], in1=nbt[:], op=ALU.subtract), reads=['cumx', 'nbt'], writes=['excl'])
                    for p_ in range(NPASS):
                        P.op('dve', lambda e, p_=p_: e.tensor_scalar(out=tq[:], in0=cum[:], scalar1=p_ + 0.5, scalar2=None, op0=ALU.is_gt), reads=['cumx'], writes=['tq'])
                        P.op('dve', lambda e, p_=p_: e.scalar_tensor_tensor(out=oh[:], in0=excl[:], scalar=p_ + 0.5, in1=tq[:], op0=ALU.is_lt, op1=ALU.mult),
                             reads=['excl', 'tq'], writes=['oh'])
                        P.op('dve', lambda e: e.reduce_sum(out=r3[:, 0:1], in_=oh[:], axis=AX.X), reads=['oh'], writes=['r3'])
                        P.op('dve', lambda e: e.tensor_tensor(out=tq[:], in0=oh[:], in1=erow, op=ALU.mult), reads=['oh', 'cst'], writes=['tq'])
                        P.op('dve', lambda e, p_=p_: e.reduce_sum(out=EP[:, p_:p_ + 1], in_=tq[:], axis=AX.X), reads=['tq'], writes=['EP'])
                        P.op('dve', lambda e: e.tensor_tensor(out=tq[:], in0=oh[:], in1=excl[:], op=ALU.mult), reads=['oh', 'excl'], writes=['tq'])
                        P.op('dve', lambda e: e.reduce_sum(out=r3[:, 1:2], in_=tq[:], axis=AX.X), reads=['tq'], writes=['r3'])
                        P.op('dve', lambda e, p_=p_: e.scalar_tensor_tensor(out=SPt[:, p_:p_ + 1], in0=r3[:, 0:1], scalar=float(p_), in1=r3[:, 1:2],
                                                                          op0=ALU.mult, op1=ALU.subtract), reads=['r3'], writes=['SPt'])
                    P.op('dve', lambda e: e.tensor_scalar(out=RB[:], in0=EP[:], scalar1=float(CAP), scalar2=None, op0=ALU.mult), reads=['EP'], writes=['RB'])
                    P.op('dve', lambda e: e.scalar_tensor_tensor(out=RB[:], in0=SPt[:], scalar=float(SUB), in1=RB[:], op0=ALU.mult, op1=ALU.add),
                         reads=['SPt', 'RB'], writes=['RB'])
                    if debug and 'EP' in debug:
                        P.dma('sp', lambda e: e.dma_start(out=dbg['EP'], in_=EP[:]), reads=['EP'])
                        P.dma('sp', lambda e: e.dma_start(out=dbg['RB'], in_=RB[:]), reads=['RB'])
                    P.barrier()
                    P.flush()
            P.barrier()
            P.flush()

        if stages >= 5:
            wgu_rows = wgu_l.rearrange("e s p k n -> (e s p) (k n)")
            wdn_rows = wdn_l.rearrange("e s p k n -> (e s p) (k n)")
            with ExitStack() as sG:
                bgu_t = sb(sG, 'bgu_t', [128, 32])
                bdn_t = sb(sG, 'bdn_t', [128, D])
                bdrb = sb(sG, 'bdrb', [1, D], BF16)
                ones1 = sb(sG, 'ones1', [1, 128], BF16)
                identb = sb(sG, 'identb', [128, 128], BF16)
                xg = sb(sG, 'xg', [128, NA, D], BF16)
                XeT = sb(sG, 'XeT', [128, 16, SUB], BF16)
                actT = sb(sG, 'actT', [128, 16, SUB], BF16)
                glu = sb(sG, 'glu', [128, 4, SUB])
                sgl = sb(sG, 'sgl', [128, 4, SUB])
                lin = sb(sG, 'lin', [128, SUB])
                ysb = [sb(sG, 'ysb%d' % i, [128, D], BF16) for i in range(NA)]
                wst = [sb(sG, 'gwst%d' % i, [128, 16 * 512]) for i in range(2)]
                wbf = [sb(sG, 'gwbf%d' % i, [128, 16, 512], BF16) for i in range(2)]
                fidx = sb(sG, 'fidx', [128, 20])
                iidx = [sb(sG, 'iidx%d' % i, [128, 20], I32) for i in range(2)]
                P.op('pool', lambda e: e.memset(ones1[:], 1.0), writes=['ones1'])
                P.op('dve', lambda e: e.tensor_copy(out=identb[:], in_=ident), reads=['cst'], writes=['identb'])
                sln = [0]
                pend = []

                def load_slab(rows_ap, idx_ap, ik):
                    n = sln[0]
                    sln[0] += 1
                    b = n % 2
                    P.dma('pool', lambda e: e.indirect_dma_start(out=wst[b][:], out_offset=None, in_=rows_ap,
                                                                 in_offset=bass.IndirectOffsetOnAxis(ap=idx_ap, axis=0)),
                          reads=[ik], writes=[('gwst', b)])
                    wv = wst[b][:].rearrange("p (k n) -> p k n", k=16)
                    P.op('dve', lambda e: e.tensor_copy(out=wbf[b][:, 0:8, :], in_=wv[:, 0:8, :]), reads=[('gwst', b)], writes=[('gwbf', b, 0)])
                    P.op('act', lambda e: e.copy(out=wbf[b][:, 8:16, :], in_=wv[:, 8:16, :]), reads=[('gwst', b)], writes=[('gwbf', b, 1)])
                    return wbf[b], [('gwbf', b, 0), ('gwbf', b, 1)]

                for pas in range(NPASS):
                    ii, ik = iidx[pas % 2], ('iidx', pas % 2)
                    epc = EP[:, pas:pas + 1]
                    P.op('dve', lambda e, pas=pas: e.tensor_scalar(out=fidx[:, 0:4], in0=iota_aq, scalar1=RB[:, pas:pas + 1], scalar2=None, op0=ALU.add),
                         reads=['RB', 'cst'], writes=['fidx'])
                    P.op('dve', lambda e, epc=epc: e.scalar_tensor_tensor(out=fidx[:, 4:12], in0=iota_s8q, scalar=0.0, in1=epc.to_broadcast([128, 8]), op0=ALU.add, op1=ALU.add),
                         reads=['EP', 'cst'], writes=['fidx'])
                    P.op('dve', lambda e, epc=epc: e.scalar_tensor_tensor(out=fidx[:, 4:12], in0=epc.to_broadcast([128, 8]), scalar=1023.0, in1=fidx[:, 4:12], op0=ALU.mult, op1=ALU.add),
                         reads=['EP', 'fidx'], writes=['fidx'])
                    P.op('dve', lambda e, epc=epc: e.scalar_tensor_tensor(out=fidx[:, 12:16], in0=epc.to_broadcast([128, 4]), scalar=512.0, in1=iota_aq, op0=ALU.mult, op1=ALU.add),
                         reads=['EP', 'cst'], writes=['fidx'])
                    P.op('dve', lambda e, epc=epc: e.scalar_tensor_tensor(out=fidx[:, 16:17], in0=epc, scalar=128.0, in1=iota_q, op0=ALU.mult, op1=ALU.add),
                         reads=['EP', 'cst'], writes=['fidx'])
                    P.op('dve', lambda e, epc=epc: e.tensor_copy(out=fidx[:, 17:18], in_=epc), reads=['EP'], writes=['fidx'])
                    P.op('dve', lambda e, ii=ii: e.tensor_copy(out=ii[:, 0:18], in_=fidx[:, 0:18]), reads=['fidx'], writes=[ik])

                    def ix(c, ii=ii):
                        return ii[:, c:c + 1].bitcast(U32)
                    for a in range(NA):
                        P.dma('pool', lambda e, a=a, ix=ix: e.indirect_dma_start(out=xg[:, a, :], out_offset=None, in_=Xg,
                                                                              in_offset=bass.IndirectOffsetOnAxis(ap=ix(a), axis=0)),
                              reads=[ik], writes=['xg'])
                    P.dma('pool', lambda e, ix=ix: e.indirect_dma_start(out=bgu_t[:], out_offset=None, in_=bgu_rows,
                                                                       in_offset=bass.IndirectOffsetOnAxis(ap=ix(16), axis=0)), reads=[ik], writes=['bgu_t'])
                    P.dma('pool', lambda e, ix=ix: e.indirect_dma_start(out=bdn_t[:], out_offset=None, in_=bdn_rows,
                                                                       in_offset=bass.IndirectOffsetOnAxis(ap=ix(17), axis=0)), reads=[ik], writes=['bdn_t'])
                    P.op('dve', lambda e: e.tensor_copy(out=bdrb[:], in_=bdn_t[0:1, :]), reads=['bdn_t'], writes=['bdrb'])
                    for k in range(16):
                        pb = nps()

                        def xtr(e, pb=pb, k=k):
                            pv = ps[pb][:, 0:SUB // 2].bitcast(BF16)
                            for a in range(NA):
                                r = e.transpose(pv[:, a * 128:(a + 1) * 128], xg[:, a, k * 128:(k + 1) * 128], identb[:])
                            return r
                        P.op('pe', xtr, reads=['xg', 'identb'], writes=[('ps', pb)])
                        if k % 2 == 0:
                            P.op('act', lambda e, pb=pb, k=k: e.copy(out=XeT[:, k, :], in_=ps[pb][:, 0:SUB // 2].bitcast(BF16)), reads=[('ps', pb)], writes=['XeT'])
                        else:
                            P.op('dve', lambda e, pb=pb, k=k: e.tensor_copy(out=XeT[:, k, :], in_=ps[pb][:, 0:SUB // 2].bitcast(BF16)), reads=[('ps', pb)], writes=['XeT'])
                    for gs in range(4):
                        for half in range(2):
                            w, wks = load_slab(wgu_rows, ix(4 + half * 4 + gs), ik)
                            if gs == 0 and half == 1 and pend:
                                pend.pop()()
                            for c4 in range(4):
                                pb = nps()

                                def guf(e, pb=pb, w=w, c4=c4):
                                    for k in range(16):
                                        r = e.matmul(ps[pb][:, 0:SUB], lhsT=w[:, k, c4 * 128:(c4 + 1) * 128], rhs=XeT[:, k, :], start=(k == 0), stop=(k == 15))
                                    return r
                                P.op('pe', guf, reads=wks + ['XeT'], writes=[('ps', pb)])
                                bc_ = half * 16 + gs * 4 + c4
                                bcol = bgu_t[:, bc_:bc_ + 1]
                                if half == 0:
                                    P.op('dve', lambda e, pb=pb, c4=c4, bcol=bcol: e.tensor_scalar(out=glu[:, c4, :], in0=ps[pb][:, 0:SUB], scalar1=bcol, scalar2=7.0,
                                                                                                   op0=ALU.add, op1=ALU.min), reads=[('ps', pb), 'bgu_t'], writes=[('glu', c4)])
                                    P.op('act', lambda e, c4=c4: e.activation(out=sgl[:, c4, :], in_=glu[:, c4, :], func=AF.Sigmoid, scale=1.702), reads=[('glu', c4)], writes=[('sgl', c4)])
                                    P.op('dve', lambda e, c4=c4: e.tensor_tensor(out=glu[:, c4, :], in0=glu[:, c4, :], in1=sgl[:, c4, :], op=ALU.mult),
                                         reads=[('glu', c4), ('sgl', c4)], writes=[('glu', c4)])
                                else:
                                    P.op('dve', lambda e, pb=pb, bcol=bcol: e.tensor_scalar(out=lin[:], in0=ps[pb][:, 0:SUB], scalar1=bcol, scalar2=7.0,
                                                                                            op0=ALU.add, op1=ALU.min), reads=[('ps', pb), 'bgu_t'], writes=['lin'])
                                    P.op('dve', lambda e: e.tensor_scalar(out=lin[:], in0=lin[:], scalar1=-7.0, scalar2=1.0, op0=ALU.max, op1=ALU.add), reads=['lin'], writes=['lin'])
                                    P.op('dve', lambda e, c4=c4, gs=gs: e.tensor_tensor(out=actT[:, gs * 4 + c4, :], in0=lin[:], in1=glu[:, c4, :], op=ALU.mult),
                                         reads=['lin', ('glu', c4)], writes=['actT'])
                    for db in range(4):
                        w, wks = load_slab(wdn_rows, ix(12 + db), ik)
                        for a in range(NA):
                            pb = nps()

                            def dnf(e, pb=pb, w=w, a=a, db=db):
                                for k in range(16):
                                    e.matmul(ps[pb][:, :], lhsT=actT[:, k, a * 128:(a + 1) * 128], rhs=w[:, k, :], start=(k == 0), stop=False)
                                return e.matmul(ps[pb][:, :], lhsT=ones1[:], rhs=bdrb[0:1, db * 512:(db + 1) * 512], start=False, stop=True)
                            P.op('pe', dnf, reads=wks + ['actT', 'ones1', 'bdrb'], writes=[('ps', pb)])
                            P.op('act', lambda e, pb=pb, a=a, db=db: e.copy(out=ysb[a][:, db * 512:(db + 1) * 512], in_=ps[pb][:, :]), reads=[('ps', pb)], writes=[('ysb', a)])
                    def scat(pas=pas, ix=ix, ik=ik):
                        for a in range(NA):
                            P.dma('pool', lambda e, a=a: e.indirect_dma_start(out=Ys, out_offset=bass.IndirectOffsetOnAxis(ap=ix(a), axis=0),
                                                                              in_=ysb[a][:], in_offset=None), reads=[('ysb', a), ik], writes=[('Ys', pas, a)])
                    pend.append(scat)
                    if pas % 4 == 3:
                        pend.pop()()
                        P.barrier()
                        P.flush()
                P.barrier()
                P.flush()

        if debug and 'idx4full' in debug:
            P.dma('sp', lambda e: e.dma_start(out=dbg['idx4full'], in_=idx4_all[:]), reads=['idx4_all'])
            P.dma('sp', lambda e: e.dma_start(out=dbg['g4full'], in_=g4_all[:]), reads=['g4_all'])
            P.barrier()
            P.flush()
        if debug and 'Xg0' in debug:
            P.dma('sp', lambda e: e.dma_start(out=dbg['Xg0'], in_=Xg[0:128, :]), reads=['Xg'])
            P.dma('sp', lambda e: e.dma_start(out=dbg['Ys0'], in_=Ys[0:128, :]), reads=['Ys'])
            P.dma('sp', lambda e: e.dma_start(out=dbg['x1s'], in_=x1s[0:128, :]), reads=['x1s'])
            P.barrier()
            P.flush()
        if stages >= 6:
            with ExitStack() as sH:
                g2bc = sb(sH, 'g2bc', [128, D])
                fnbc = sb(sH, 'fnbc', [128, D])
                x1t = [sb(sH, 'x1t%d' % i, [128, D]) for i in range(2)]
                yg = [sb(sH, 'yg%d' % i, [128, D], BF16) for i in range(4)]
                acc = sb(sH, 'acc', [128, D])
                junk = sb(sH, 'junkH', [128, D], BF16)
                ssH = sb(sH, 'ssH', [128, 1])
                P.dma('sp', lambda e: e.dma_start(out=g2bc[:], in_=modrow[0:1, 3 * D:4 * D].to_broadcast([128, D])), reads=['modrow'], writes=['g2bc'])
                P.dma('sp', lambda e: e.dma_start(out=fnbc[:], in_=rows_d[0:1, D:2 * D].to_broadcast([128, D])), writes=['fnbc'])
                for it in range(NT):
                    xt_, xk = x1t[it % 2], ('x1t', it % 2)
                    P.dma('sp', lambda e, xt_=xt_, it=it: e.dma_start(out=xt_[:], in_=x1s[it * 128:(it + 1) * 128, :]), reads=['x1s'], writes=[xk])
                    for k4 in range(4):
                        P.dma('pool', lambda e, it=it, k4=k4: e.indirect_dma_start(
                            out=yg[k4][:], out_offset=None, in_=Ys,
                            in_offset=bass.IndirectOffsetOnAxis(ap=idx4_all[:, it, k4:k4 + 1].bitcast(U32), axis=0)),
                            reads=['Ys', 'idx4_all'], writes=[('yg', k4)])
                    P.op('dve', lambda e, it=it: e.tensor_scalar(out=acc[:], in0=yg[0][:], scalar1=g4_all[:, it, 0:1], scalar2=None, op0=ALU.mult),
                         reads=[('yg', 0), 'g4_all'], writes=['acc'])
                    for k4 in range(1, 4):
                        P.op('dve', lambda e, it=it, k4=k4: e.scalar_tensor_tensor(out=acc[:], in0=yg[k4][:], scalar=g4_all[:, it, k4:k4 + 1], in1=acc[:],
                                                                                   op0=ALU.mult, op1=ALU.add), reads=[('yg', k4), 'g4_all', 'acc'], writes=['acc'])
                    P.op('pool', lambda e: e.tensor_tensor(out=acc[:], in0=acc[:], in1=g2bc[:], op=ALU.mult), reads=['acc', 'g2bc'], writes=['acc'])
                    P.op('dve', lambda e, xt_=xt_: e.tensor_tensor(out=xt_[:], in0=xt_[:], in1=acc[:], op=ALU.add), reads=[xk, 'acc'], writes=[xk])
                    P.op('act', lambda e, xt_=xt_: e.activation(out=junk[:], in_=xt_[:], func=AF.Square, accum_out=ssH[:]), reads=[xk], writes=['junkH', 'ssH'])
                    P.op('dve', lambda e: e.tensor_scalar(out=ssH[:], in0=ssH[:], scalar1=1.0 / D, scalar2=1e-5, op0=ALU.mult, op1=ALU.add), reads=['ssH'], writes=['ssH'])
                    rsqrt(lambda: ssH[:], 'ssH')
                    P.op('dve', lambda e, xt_=xt_: e.scalar_tensor_tensor(out=xt_[:], in0=xt_[:], scalar=ssH[:, 0:1], in1=fnbc[:], op0=ALU.mult, op1=ALU.mult),
                         reads=[xk, 'ssH', 'fnbc'], writes=[xk])
                    P.dma('sp', lambda e, xt_=xt_, it=it: e.dma_start(out=out[it * 128:(it + 1) * 128, :], in_=xt_[:]), reads=[xk], writes=['out'])
                P.barrier()
                P.flush()

        P.barrier()
        P.flush()
    return nc


NCH = 91
NBLK_DBG = [4]
OPLIMIT = [10 ** 9]
CI_WA, CI_GD0, CI_GD1 = 24, 25, 26
CI_HQ, CI_HF, CI_HI, CI_HO = 27, 35, 43, 51
CI_GA, CI_GB = 59, 75
COLOFF = {'mu': 0, 'w0': 27, 'a0': 35, 'k_k': 43, 'k_a': 51, 'r_k': 59, 'gn_g': 67, 'gn_b': 75, 'lb0': 83, 'lb1': 91, 'hgn': 99}
NCOL = 100
NCST = 1356


def _f(a):
    return np.ascontiguousarray(np.asarray(a, dtype=np.float32))


def _chunks(w, starts, width=128):
    outl = []
    for c0 in starts:
        blk = np.zeros((2048, 128), np.float32)
        n = min(width, w.shape[1] - c0)
        blk[:, :n] = w[:, c0:c0 + n]
        outl.append(blk.reshape(16, 128, 128).transpose(1, 0, 2))
    return np.ascontiguousarray(np.stack(outl, 0))


def _colv(v, n):
    buf = np.zeros(n * 128, np.float32)
    buf[:v.size] = v.reshape(-1)
    return buf.reshape(n, 128).T


def prep_shared(inputs, experts=True):
    sh = {}
    aw = _f(inputs['ada_w'])[0]
    sh['ada_w'] = np.ascontiguousarray(aw.reshape(16, 128, 24, 512).transpose(2, 1, 0, 3))
    ab = _f(inputs['ada_b'])[0]
    sh['adab_col'] = np.ascontiguousarray(ab[:4096].reshape(32, 128).T)
    sh['adab_row'] = np.ascontiguousarray(ab[4096:].reshape(1, 8192))
    sh['n1g_col'] = np.ascontiguousarray(_f(inputs['norm1_g'])[0].reshape(16, 128).T)
    w_in = _f(inputs['w_in'])[0]
    starts = [i * 128 for i in range(27)] + [3360 + i * 128 for i in range(32)] + [7456 + i * 128 for i in range(32)]
    wl = _chunks(w_in, starts)
    wl[26, :, :, 32:] = 0.0
    sh['w_in_l'] = wl
    cols = np.zeros((128, NCOL), np.float32)
    cols[:, 0:27] = _colv(_f(inputs['rwkv_mu'])[0], 27)
    for nm, key in (('w0', 'rwkv_w0'), ('a0', 'rwkv_a0'), ('k_k', 'rwkv_k_k'), ('k_a', 'rwkv_k_a'), ('r_k', 'rwkv_r_k'),
                    ('gn_g', 'rwkv_gn_g'), ('gn_b', 'rwkv_gn_b')):
        cols[:, COLOFF[nm]:COLOFF[nm] + 8] = _colv(_f(inputs[key])[0], 8)
    lbl = _f(inputs['hgrn_lb_logits'])
    cols[:, 83:91] = _colv(lbl[0], 8)
    cols[:, 91:99] = _colv(lbl[1], 8)
    cols[:, 99:100] = _f(inputs['hgrn_gn_g'])[0].reshape(128, 1)
    sh['cols'] = cols
    p = np.arange(128)[:, None]
    fcol = np.arange(128)[None, :]
    sU = (p < fcol).astype(np.float32)
    iU = (p <= fcol).astype(np.float32)
    sL = (p > fcol).astype(np.float32)
    cst = np.zeros((128, NCST), np.float32)
    cst[:, 0:512] = np.concatenate([sU, iU, sU, iU], 1)
    cst[:, 512:640] = np.eye(128, dtype=np.float32)
    cst[:, 640:768] = sL
    cst[:, 768:896] = iU
    cst[:, 896:1024] = ((p // 64) == (fcol // 64)).astype(np.float32)
    cst[:, 1152:1184] = (np.arange(32) * CAP + 1)[None, :].astype(np.float32)
    cst[:, 1184:1312] = sU
    cst[:, 1312:1344] = np.arange(32)[None, :].astype(np.float32)
    cst[:, 1344:1348] = (np.arange(4)[None, :] * 128 + p).astype(np.float32)
    cst[:, 1348:1356] = (np.arange(8)[None, :] * 128 + p).astype(np.float32)
    sh['cst'] = cst
    z64 = np.zeros((64, 1024), np.float32)
    sh['w2p'] = np.ascontiguousarray(np.concatenate([_f(inputs['rwkv_w2'])[0], z64], 0))
    sh['a2p'] = np.ascontiguousarray(np.concatenate([z64, _f(inputs['rwkv_a2'])[0]], 0))
    g2 = _f(inputs['rwkv_g2'])[0]
    sh['g2a'] = np.ascontiguousarray(g2[:128])
    sh['g2b'] = np.ascontiguousarray(np.concatenate([g2[128:160], np.zeros((96, 1024), np.float32)], 0))
    for nm, key in (('pa_l', 'proj_a'), ('pb_l', 'proj_b')):
        w = _f(inputs[key])[0]
        sh[nm] = np.ascontiguousarray(w.reshape(8, 128, 16, 128).transpose(2, 1, 0, 3))
    wo = _f(inputs['w_out'])[0]
    sh['wout_l'] = np.ascontiguousarray(wo.reshape(16, 128, 16, 128).transpose(2, 1, 0, 3))
    sh['rows'] = np.ascontiguousarray(np.concatenate([_f(inputs['norm2_g'])[0], _f(inputs['final_norm_g']),
                                                      _f(inputs['router_b'])[0]]).reshape(1, -1))
    rw = _f(inputs['router_w'])[0]
    sh['rw_l'] = np.ascontiguousarray(rw.reshape(16, 128, 32).transpose(1, 0, 2))
    if experts:
        wgu = np.asarray(inputs['exp_w_gate_up'], dtype=np.float32)[0]
        sh['wgu_l'] = np.ascontiguousarray(wgu.reshape(NE, 16, 128, 8, 512).transpose(0, 3, 2, 1, 4))
        wdn = np.asarray(inputs['exp_w_down'], dtype=np.float32)[0]
        sh['wdn_l'] = np.ascontiguousarray(wdn.reshape(NE, 16, 128, 4, 512).transpose(0, 3, 2, 1, 4))
    bgu = _f(inputs['exp_b_gate_up'])[0]
    sh['bgu_rows'] = np.ascontiguousarray(bgu.reshape(NE, 32, 128).transpose(0, 2, 1).reshape(NE * 128, 32))
    sh['bdn_rows'] = np.ascontiguousarray(_f(inputs['exp_b_down'])[0])
    return sh


def prep_core(inputs, b):
    x = np.asarray(inputs['x'], dtype=np.float32)[b]
    c = np.asarray(inputs['c'], dtype=np.float32)[b]
    return {
        'x': np.ascontiguousarray(x),
        'xT': np.ascontiguousarray(x.T),
        'cT': np.ascontiguousarray(c.reshape(16, 128).T),
    }


def kernel(**inputs):
    nc = build()
    sh = prep_shared(inputs)
    in_maps = []
    for b in range(8):
        m = dict(sh)
        m.update(prep_core(inputs, b))
        in_maps.append(m)
    res = run_bass_kernel_spmd(nc, in_maps, core_ids=list(range(8)))
    return np.stack([np.asarray(r['out'], dtype=np.float32) for r in res.results], axis=0)
```
